# Optimizing a Trainium2 kernel written in Bass

```python
import jax, jax.numpy as jnp
from jax import lax
import numpy as np

D_MODEL = 4096
BATCH = 1
SEQ = 16384
DEPTH = 2

CHUNK = 64

A_KEY = 128
A_VAL = 128
A_WIDTH = 3 * D_MODEL // 8
A_HEADS = A_WIDTH // A_VAL

B_WIDTH = D_MODEL // 4
B_HEAD_DIM = 128
B_HEADS = B_WIDTH // B_HEAD_DIM
B_ROT = B_HEAD_DIM // 4
IDX_HEADS = 16
IDX_DIM = 64
IDX_ROT = IDX_DIM // 4
IDX_TOPK_MAX = 256
Q_BLOCK = 128
ROPE_THETA = 500000.0

C_WIDTH = D_MODEL - A_WIDTH - B_WIDTH
C_VAL = 256
C_KEY = 128
C_HEADS = C_WIDTH // C_VAL
RET_THETA = 10000.0

MIX_WIDTH = A_WIDTH + B_WIDTH + C_WIDTH
PROJ_SIZES = (A_HEADS * A_KEY, A_HEADS * A_KEY, A_WIDTH, A_WIDTH,
              B_WIDTH, B_WIDTH, B_WIDTH, IDX_HEADS * IDX_DIM, IDX_DIM, IDX_HEADS,
              C_HEADS * C_KEY, C_HEADS * C_KEY, C_WIDTH, C_WIDTH)
PROJ_DIM = sum(PROJ_SIZES)

N_EXPERTS = 32
TOP_K = 4
D_EXPERT = 768
SWIGLU_LIMIT = 7.0
SWIGLU_ALPHA = 1.702
MOE_BLOCK = 256

DN_ALPHA = (2 * DEPTH) ** 0.25
DN_BETA = (8 * DEPTH) ** -0.25
LN_EPS = 1e-5
NORM_EPS = 1e-6

kernel_name = 'hybrid_hgrn2_dsa_retention_moe_deepnorm'

F32 = jnp.float32


def layer_norm(x, g, b):
    xf = x.astype(F32)
    mu = jnp.mean(xf, axis=-1, keepdims=True)
    var = jnp.mean(jnp.square(xf - mu), axis=-1, keepdims=True)
    return ((xf - mu) * lax.rsqrt(var + LN_EPS) * g.astype(F32) + b.astype(F32)).astype(x.dtype)


def rope_tables(seq, n_rot, theta):
    inv_freq = 1.0 / (theta ** (jnp.arange(0, n_rot, 2, dtype=F32) / n_rot))
    ang = jnp.arange(seq, dtype=F32)[:, None] * inv_freq[None, :]
    return jnp.cos(ang), jnp.sin(ang)


def apply_rope(x, cos, sin, n_rot):
    half = n_rot // 2
    x1, x2 = x[..., :half], x[..., half:n_rot]
    c, s = cos[:, None, :], sin[:, None, :]
    return jnp.concatenate([x1 * c - x2 * s, x2 * c + x1 * s, x[..., n_rot:]], axis=-1)


def to_chunks(t, n_heads, d):
    bsz, seq = t.shape[:2]
    return t.reshape(bsz, seq // CHUNK, CHUNK, n_heads, d).transpose(1, 0, 3, 2, 4)


def from_chunks(o):
    n, bsz, h, c, d = o.shape
    return o.transpose(1, 0, 3, 2, 4).reshape(bsz, n * c, h, d)


def hgrn2_mixer(q, f_logit, i, g, lb, norm_g):
    bsz, seq, _ = q.shape
    f = lb + (1.0 - lb) * jax.nn.sigmoid(f_logit.astype(F32))
    log_f = jnp.log(f)
    k = 1.0 - f
    qc = to_chunks(q.astype(F32) * A_KEY ** -0.5, A_HEADS, A_KEY)
    kc = to_chunks(k, A_HEADS, A_KEY)
    vc = to_chunks(i.astype(F32), A_HEADS, A_VAL)
    lfc = to_chunks(log_f, A_HEADS, A_KEY)
    causal = jnp.tril(jnp.ones((CHUNK, CHUNK), dtype=bool))[:, :, None]

    def step(state, inp):
        qb, kb, vb, lf = inp
        cum = jnp.cumsum(lf, axis=-2)
        inter = jnp.einsum('bhck,bhkv->bhcv', qb * jnp.exp(cum), state)
        diff = cum[:, :, :, None, :] - cum[:, :, None, :, :]
        dec = jnp.exp(jnp.where(causal, diff, -jnp.inf))
        att = jnp.sum(qb[:, :, :, None, :] * kb[:, :, None, :, :] * dec, axis=-1)
        intra = jnp.einsum('bhcs,bhsv->bhcv', att, vb)
        last = cum[:, :, -1:, :]
        state = (jnp.exp(last[:, :, 0, :])[..., None] * state
                 + jnp.einsum('bhsk,bhsv->bhkv', kb * jnp.exp(last - cum), vb))
        return state, inter + intra

    s0 = jnp.zeros((bsz, A_HEADS, A_KEY, A_VAL), F32)
    _, o = lax.scan(step, s0, (qc, kc, vc, lfc))
    o = from_chunks(o)
    o = o * lax.rsqrt(jnp.mean(jnp.square(o), axis=-1, keepdims=True) + NORM_EPS) * norm_g.astype(F32)
    o = o * jax.nn.silu(g.astype(F32)).reshape(bsz, seq, A_HEADS, A_VAL)
    return o.reshape(bsz, seq, A_WIDTH)


def retention_mixer(q, k, v, g, cos, sin, norm_g):
    bsz, seq, _ = q.shape
    q = apply_rope(q.astype(F32).reshape(bsz, seq, C_HEADS, C_KEY), cos, sin, C_KEY)
    k = apply_rope(k.astype(F32).reshape(bsz, seq, C_HEADS, C_KEY), cos, sin, C_KEY) * C_KEY ** -0.5
    qc = q.reshape(bsz, seq // CHUNK, CHUNK, C_HEADS, C_KEY).transpose(1, 0, 3, 2, 4)
    kc = k.reshape(bsz, seq // CHUNK, CHUNK, C_HEADS, C_KEY).transpose(1, 0, 3, 2, 4)
    vc = to_chunks(v.astype(F32), C_HEADS, C_VAL)
    log_gamma = jnp.log(1.0 - jnp.exp(jnp.linspace(jnp.log(1.0 / 32), jnp.log(1.0 / 512), C_HEADS)))
    pos = jnp.arange(CHUNK, dtype=F32)
    rel = pos[:, None] - pos[None, :]
    dmat = jnp.exp(jnp.where(rel >= 0, log_gamma[:, None, None] * rel, -jnp.inf))
    q_dec = jnp.exp(log_gamma[:, None] * (pos + 1.0))[..., None]
    k_dec = jnp.exp(log_gamma[:, None] * (CHUNK - 1.0 - pos))[..., None]
    c_dec = jnp.exp(log_gamma * CHUNK)[:, None, None]

    def step(state, inp):
        qb, kb, vb = inp
        intra = jnp.einsum('bhcs,bhsv->bhcv', jnp.einsum('bhcd,bhsd->bhcs', qb, kb) * dmat, vb)
        inter = jnp.einsum('bhcd,bhdv->bhcv', qb * q_dec, state)
        state = c_dec * state + jnp.einsum('bhsd,bhsv->bhdv', kb * k_dec, vb)
        return state, intra + inter

    s0 = jnp.zeros((bsz, C_HEADS, C_KEY, C_VAL), F32)
    _, o = lax.scan(step, s0, (qc, kc, vc))
    o = from_chunks(o)
    mu = jnp.mean(o, axis=-1, keepdims=True)
    var = jnp.mean(jnp.square(o - mu), axis=-1, keepdims=True)
    o = (o - mu) * lax.rsqrt(var + NORM_EPS) * norm_g.astype(F32)
    o = o * jax.nn.silu(g.astype(F32)).reshape(bsz, seq, C_HEADS, C_VAL)
    return o.reshape(bsz, seq, C_WIDTH)


def dsa_mixer(q, k, v, qi, ki, wi, cos_b, sin_b, cos_i, sin_i):
    bsz, seq, _ = q.shape
    q = apply_rope(q.astype(F32).reshape(bsz, seq, B_HEADS, B_HEAD_DIM), cos_b, sin_b, B_ROT)
    k = apply_rope(k.astype(F32).reshape(bsz, seq, B_HEADS, B_HEAD_DIM), cos_b, sin_b, B_ROT)
    v = v.astype(F32).reshape(bsz, seq, B_HEADS, B_HEAD_DIM)
    qi = apply_rope(qi.astype(F32).reshape(bsz, seq, IDX_HEADS, IDX_DIM), cos_i, sin_i, IDX_ROT)
    ki = apply_rope(ki.astype(F32)[:, :, None, :], cos_i, sin_i, IDX_ROT)[:, :, 0, :]
    wi = wi.astype(F32) * IDX_HEADS ** -0.5
    topk = min(IDX_TOPK_MAX, seq // 4)
    key_pos = jnp.arange(seq)

    def block(j):
        s0 = j * Q_BLOCK
        qb = lax.dynamic_slice_in_dim(q, s0, Q_BLOCK, axis=1)
        qib = lax.dynamic_slice_in_dim(qi, s0, Q_BLOCK, axis=1)
        wib = lax.dynamic_slice_in_dim(wi, s0, Q_BLOCK, axis=1)
        qpos = s0 + jnp.arange(Q_BLOCK)
        limit = (qpos // CHUNK + 1) * CHUNK
        visible = key_pos[None, :] < limit[:, None]
        idx_logits = jnp.einsum('bqhd,bsd->bqhs', qib, ki) * IDX_DIM ** -0.5
        score = jnp.einsum('bqhs,bqh->bqs', jax.nn.relu(idx_logits), wib)
        score = jnp.where(visible[None], score, -jnp.inf)
        _, sel = lax.top_k(score, topk)
        sel_ok = sel < limit[None, :, None]
        ks = jax.vmap(lambda kk, ii: kk[ii])(k, sel)
        vs = jax.vmap(lambda vv, ii: vv[ii])(v, sel)
        logits = jnp.einsum('bqhd,bqkhd->bhqk', qb, ks) * B_HEAD_DIM ** -0.5
        logits = jnp.where(sel_ok[:, None], logits, -jnp.inf)
        p = jax.nn.softmax(logits, axis=-1)
        return jnp.einsum('bhqk,bqkhd->bqhd', p, vs)

    o = lax.map(block, jnp.arange(seq // Q_BLOCK))
    return o.transpose(1, 0, 2, 3, 4).reshape(bsz, seq, B_WIDTH)


def moe_ffn(h, router_w, router_b, w_gate_up, b_gate_up, w_down, b_down):
    bsz, seq, d = h.shape
    n_tok = bsz * seq
    xt = h.reshape(n_tok, d)
    logits = (xt @ router_w).astype(F32) + router_b.astype(F32)
    top_val, top_idx = lax.top_k(logits, TOP_K)
    gates = jax.nn.softmax(top_val, axis=-1)
    flat_e = top_idx.reshape(-1)
    flat_t = jnp.repeat(jnp.arange(n_tok, dtype=jnp.int32), TOP_K)
    flat_g = gates.reshape(-1)
    order = jnp.argsort(flat_e, stable=True)
    se, st, sg = flat_e[order], flat_t[order], flat_g[order]
    counts = jnp.bincount(flat_e, length=N_EXPERTS)
    padded = (counts + MOE_BLOCK - 1) // MOE_BLOCK * MOE_BLOCK
    start = jnp.cumsum(counts) - counts
    pend = jnp.cumsum(padded)
    pstart = pend - padded
    n_assign = n_tok * TOP_K
    dest = pstart[se] + jnp.arange(n_assign, dtype=jnp.int32) - start[se]
    n_blocks = -(-n_assign // MOE_BLOCK) + N_EXPERTS
    n_rows = n_blocks * MOE_BLOCK
    row_tok = jnp.zeros((n_rows,), jnp.int32).at[dest].set(st)
    row_gate = jnp.zeros((n_rows,), F32).at[dest].set(sg)
    block_expert = jnp.minimum(
        jnp.searchsorted(pend, jnp.arange(n_blocks, dtype=pend.dtype) * MOE_BLOCK, side='right'),
        N_EXPERTS - 1)

    def expert_block(y, blk):
        tok, gate, e = blk
        gu = (xt[tok] @ w_gate_up[e] + b_gate_up[e]).astype(F32)
        g_lin, u_lin = jnp.split(gu, 2, axis=-1)
        g_lin = jnp.minimum(g_lin, SWIGLU_LIMIT)
        u_lin = jnp.clip(u_lin, -SWIGLU_LIMIT, SWIGLU_LIMIT)
        act = (u_lin + 1.0) * g_lin * jax.nn.sigmoid(SWIGLU_ALPHA * g_lin)
        out = (act.astype(xt.dtype) @ w_down[e] + b_down[e]).astype(F32)
        return y.at[tok].add(out * gate[:, None]), None

    y, _ = lax.scan(expert_block, jnp.zeros((n_tok, d), F32),
                    (row_tok.reshape(n_blocks, MOE_BLOCK), row_gate.reshape(n_blocks, MOE_BLOCK), block_expert))
    return y.reshape(bsz, seq, d).astype(h.dtype)


def setup_inputs(seed: int = 0) -> dict:
    key = jax.random.key(seed)
    ks = jax.random.split(key, 20)

    def nrm(k, shape, scale):
        return jax.random.normal(k, shape, F32) * scale

    L, D, E, F = DEPTH, D_MODEL, N_EXPERTS, D_EXPERT
    return {
        'x': nrm(ks[0], (BATCH, SEQ, D), 1.0),
        'ln_in_g': 1.0 + nrm(ks[1], (D,), 0.02),
        'ln_in_b': nrm(ks[2], (D,), 0.02),
        'w_in': nrm(ks[3], (L, D, PROJ_DIM), D ** -0.5),
        'w_out': nrm(ks[4], (L, MIX_WIDTH, D), MIX_WIDTH ** -0.5 * DN_BETA),
        'hgrn_lb': nrm(ks[5], (L, A_HEADS * A_KEY), 0.5),
        'hgrn_norm_g': 1.0 + nrm(ks[6], (L, A_HEADS, A_VAL), 0.02),
        'ret_norm_g': 1.0 + nrm(ks[7], (L, C_HEADS, C_VAL), 0.02),
        'ln1_g': 1.0 + nrm(ks[8], (L, D), 0.02),
        'ln1_b': nrm(ks[9], (L, D), 0.02),
        'router_w': nrm(ks[10], (L, D, E), D ** -0.5),
        'router_b': nrm(ks[11], (L, E), 0.01),
        'w_gate_up': nrm(ks[12], (L, E, D, 2 * F), D ** -0.5),
        'b_gate_up': nrm(ks[13], (L, E, 2 * F), 0.01),
        'w_down': nrm(ks[14], (L, E, F, D), F ** -0.5 * DN_BETA),
        'b_down': nrm(ks[15], (L, E, D), 0.01),
        'ln2_g': 1.0 + nrm(ks[16], (L, D), 0.02),
        'ln2_b': nrm(ks[17], (L, D), 0.02),
    }


def reference(x, ln_in_g, ln_in_b, w_in, w_out, hgrn_lb, hgrn_norm_g, ret_norm_g, ln1_g, ln1_b,
              router_w, router_b, w_gate_up, b_gate_up, w_down, b_down, ln2_g, ln2_b):
    seq = x.shape[1]
    cos_b, sin_b = rope_tables(seq, B_ROT, ROPE_THETA)
    cos_i, sin_i = rope_tables(seq, IDX_ROT, ROPE_THETA)
    cos_r, sin_r = rope_tables(seq, C_KEY, RET_THETA)
    lb_all = jnp.cumsum(jax.nn.softmax(hgrn_lb.astype(F32), axis=0), axis=0)
    lb_all = lb_all - lb_all[0:1]
    splits = np.cumsum(PROJ_SIZES)[:-1].tolist()
    h = layer_norm(x, ln_in_g, ln_in_b)
    for l in range(DEPTH):
        p = h @ w_in[l]
        aq, af, ai, ag, bq, bk, bv, iq, ik, iw, cq, ck, cv, cg = jnp.split(p, splits, axis=-1)
        oa = hgrn2_mixer(aq, af, ai, ag, lb_all[l], hgrn_norm_g[l])
        ob = dsa_mixer(bq, bk, bv, iq, ik, iw, cos_b, sin_b, cos_i, sin_i)
        oc = retention_mixer(cq, ck, cv, cg, cos_r, sin_r, ret_norm_g[l])
        mix = jnp.concatenate([oa, ob, oc], axis=-1).astype(h.dtype) @ w_out[l]
        h = layer_norm(DN_ALPHA * h + mix, ln1_g[l], ln1_b[l])
        ffn = moe_ffn(h, router_w[l], router_b[l], w_gate_up[l], b_gate_up[l], w_down[l], b_down[l])
        h = layer_norm(DN_ALPHA * h + ffn, ln2_g[l], ln2_b[l])
    return h
```

```python
import contextlib
import numpy as np
import concourse.bass as bass
import concourse.mybir as mybir

F32 = mybir.dt.float32
BF16 = mybir.dt.bfloat16
I32 = mybir.dt.int32
AF = mybir.ActivationFunctionType
ALU = mybir.AluOpType
AX = mybir.AxisListType


class _St:
    __slots__ = ("w", "r")

    def __init__(self):
        self.w = None
        self.r = {}


class T:
    def __init__(self, s, t, name):
        self.s = s
        self.t = t
        self.name = name
        self.st = {"*": _St()}
        self.dsem = None
        self.dcnt = 0

    def __getitem__(self, idx):
        return self.t[idx]

    def states(self, key):
        if key is None:
            return list(self.st.values())
        if key not in self.st:
            n = _St()
            n.w = self.st["*"].w
            n.r = dict(self.st["*"].r)
            self.st[key] = n
        return [self.st[key], self.st["*"]]


class Sched:
    ENG = ("pe", "act", "dve", "pool", "sp")

    def __init__(self, nc):
        self.nc = nc
        self.es = contextlib.ExitStack()
        self.prog = {e: [] for e in self.ENG}
        self.cnt = {e: 0 for e in self.ENG}
        self.sem = {}
        self.waited = {e: {} for e in self.ENG}
        self.semobj = {}
        for e in self.ENG:
            self.semobj[e] = self.es.enter_context(nc.semaphore("s_" + e))
        self.ntiles = 0
        self.downer = {}
        self.out_tiles = []

    def sb(self, shape, dtype, name):
        t = self.es.enter_context(self.nc.sbuf_tensor(name, list(shape), dtype))
        return T(self, t, name)

    def ps(self, shape, dtype, name):
        t = self.es.enter_context(self.nc.psum_tensor(name, list(shape), dtype))
        return T(self, t, name)

    def _dsem(self, tl):
        if tl.dsem is None:
            key = "d_" + tl.name
            tl.dsem = key
            self.semobj[key] = self.es.enter_context(self.nc.semaphore(key))
            self.downer[key] = tl
        return tl.dsem

    @staticmethod
    def _norm(lst):
        out = []
        for x in lst:
            if isinstance(x, tuple):
                out.append(x)
            else:
                out.append((x, None))
        return out

    def _deps(self, eng, reads, writes):
        deps = {}

        def add(tk):
            if tk is None:
                return
            k, v = tk
            if deps.get(k, 0) < v:
                deps[k] = v

        for tl, key in reads:
            for st in (tl.states(key) if key is not None else tl.states(None)):
                add(st.w)
        for tl, key in writes:
            for st in (tl.states(key) if key is not None else tl.states(None)):
                add(st.w)
                for k, v in st.r.items():
                    add((k, v))
        waits = []
        wd = self.waited[eng]
        for k, v in deps.items():
            if k == eng and eng in ("pe", "sp"):
                continue
            if k in self.downer:
                v = self.downer[k].dcnt
            if wd.get(k, 0) >= v:
                continue
            wd[k] = v
            waits.append((k, v))
        return waits

    def _commit(self, reads, writes, tk):
        k, v = tk
        for tl, key in reads:
            sts = tl.states(key) if key is not None else tl.states(None)
            if key is not None:
                sts = [tl.st[key]]
            for st in sts:
                if st.r.get(k, 0) < v:
                    st.r[k] = v
        for tl, key in writes:
            if key is None:
                tl.st = {"*": _St()}
                tl.st["*"].w = tk
            else:
                st = tl.states(key)[0]
                st.w = tk
                st.r = {}

    def op(self, eng, fn, reads=(), writes=()):
        reads = self._norm(reads)
        writes = self._norm(writes)
        waits = self._deps(eng, reads, writes)
        self.cnt[eng] += 1
        tk = (eng, self.cnt[eng])
        semobj = self.semobj
        so = semobj[eng]

        def run(e, waits=waits, fn=fn, so=so):
            for k, v in waits:
                e.wait_ge(semobj[k], v)
            fn(e).then_inc(so, 1)

        self.prog[eng].append(run)
        self._commit(reads, writes, tk)

    def dma(self, q, out, in_, reads=(), writes=(), **kw):
        reads = self._norm(reads)
        writes = self._norm(writes)
        waits = self._deps(q, reads, writes)
        owner = (writes[0][0] if writes else reads[0][0])
        dk = self._dsem(owner)
        owner.dcnt += 16
        tk = (dk, owner.dcnt)
        semobj = self.semobj

        def run(e, waits=waits):
            for k, v in waits:
                e.wait_ge(semobj[k], v)
            e.dma_start(out=out, in_=in_, **kw).then_inc(semobj[dk], 16)

        self.prog[q].append(run)
        self._commit(reads, writes, tk)
        if not writes:
            self.out_tiles.append(owner)

    def finish(self):
        waits = []
        seen = set()
        for tl in self.out_tiles:
            if tl.dsem in seen:
                continue
            seen.add(tl.dsem)
            waits.append((tl.dsem, tl.dcnt))
        semobj = self.semobj

        def run(e):
            for k, v in waits:
                e.wait_ge(semobj[k], v)

        self.prog["sp"].append(run)
        nc = self.nc
        prog = self.prog
        with nc.Block() as block:
            @block.sync
            def _(e):
                for f in prog["sp"]:
                    f(e)

            @block.tensor
            def _(e):
                for f in prog["pe"]:
                    f(e)

            @block.scalar
            def _(e):
                for f in prog["act"]:
                    f(e)

            @block.vector
            def _(e):
                for f in prog["dve"]:
                    f(e)

            @block.gpsimd
            def _(e):
                for f in prog["pool"]:
                    f(e)
        self.es.close()


LN_EPS = 1e-5

def emit_ln_rows(s, xt, P, D, eps, tmp_prefix, stats, mv, rstd, epsT):
    nch = D // 512
    for c in range(nch):
        s.op("dve", lambda e, c=c: e.bn_stats(out=stats[:, c, :], in_=xt[:, c * 512:(c + 1) * 512]),
             reads=[xt], writes=[(stats, c)])
    s.op("dve", lambda e: e.bn_aggr(out=mv[:], in_=stats[:].rearrange("p c s -> p (c s)")), reads=[stats], writes=[mv])
    s.op("act", lambda e: e.activation(out=rstd[:], in_=mv[:, 1:2], func=AF.Sqrt, bias=epsT[:, 0:1], scale=1.0),
         reads=[mv, epsT], writes=[rstd])
    s.op("dve", lambda e: e.reciprocal(out=rstd[:], in_=rstd[:]), reads=[rstd], writes=[rstd])
    s.op("dve", lambda e: e.tensor_scalar(out=xt[:], in0=xt[:], scalar1=mv[:, 0:1], scalar2=rstd[:, 0:1],
                                          op0=ALU.subtract, op1=ALU.mult), reads=[xt, mv, rstd], writes=[xt])


def build_la(Tc, K, N, do_ln):
    nc = bass.Bass("TRN2", target_bir_lowering=False)
    x = nc.dram_tensor("x", [Tc, K], F32, kind="ExternalInput").ap()
    w = nc.dram_tensor("w", [K, N], F32, kind="ExternalInput").ap()
    ident_d = nc.dram_tensor("ident", [128, 128], F32, kind="ExternalInput").ap()
    if do_ln:
        g = nc.dram_tensor("g", [K], F32, kind="ExternalInput").ap()
        b = nc.dram_tensor("b", [K], F32, kind="ExternalInput").ap()
        hout = nc.dram_tensor("hout", [Tc, K], F32, kind="ExternalOutput").ap()
    p = nc.dram_tensor("p", [Tc, N], F32, kind="ExternalOutput").ap()
    s = Sched(nc)
    KC = K // 128
    TH = min(Tc, 1024)
    NTH = TH // 128
    NB = 256
    ident = s.sb([128, 128], F32, "identT")
    s.dma("sp", ident[:], ident_d, writes=[ident])
    if do_ln:
        gB = s.sb([128, K], F32, "gB"); bB = s.sb([128, K], F32, "bB")
        s.dma("sp", gB[:], g.partition_broadcast(128), writes=[gB])
        s.dma("sp", bB[:], b.partition_broadcast(128), writes=[bB])
        stats = s.sb([128, K // 512, 6], F32, "stats"); mv = s.sb([128, 2], F32, "mv"); rstd = s.sb([128, 1], F32, "rstd")
        epsT = s.sb([128, 1], F32, "epsT")
        s.op("dve", lambda e: e.memset(epsT[:], LN_EPS), writes=[epsT])
    xts = [s.sb([128, K], F32, "xt%d" % i) for i in range(2)]
    hT = s.sb([128, KC, TH], BF16, "hT")
    pst = [s.ps([128, 512], F32, "pst%d" % i) for i in range(2)]
    psg = [s.ps([128, 512], F32, "psg%d" % i) for i in range(4)]
    wbs = [s.sb([128, KC, NB], BF16, "wb%d" % i) for i in range(2)]
    outs = [s.sb([128, NB], F32, "ot%d" % i) for i in range(4)]
    nblocks = (N + NB - 1) // NB
    wv = w.rearrange("(kc p) n -> p kc n", p=128)
    tcount = 0
    wcount = 0
    ocount = 0
    for half in range(Tc // TH):
        for ti in range(NTH):
            t0 = half * TH + ti * 128
            xt = xts[tcount % 2]
            s.dma("sp", xt[:], x[t0:t0 + 128, :], writes=[xt])
            if do_ln:
                emit_ln_rows(s, xt, 128, K, LN_EPS, "ln", stats, mv, rstd, epsT)
                s.op("pool", lambda e, xt=xt: e.tensor_tensor(out=xt[:], in0=xt[:], in1=gB[:], op=ALU.mult), reads=[xt, gB], writes=[xt])
                s.op("dve", lambda e, xt=xt: e.tensor_tensor(out=xt[:], in0=xt[:], in1=bB[:], op=ALU.add), reads=[xt, bB], writes=[xt])
                s.dma("sp", hout[t0:t0 + 128, :], xt[:], reads=[xt])
            for k4 in range(KC // 4):
                pt = pst[k4 % 2]
                for j in range(4):
                    kc = k4 * 4 + j
                    s.op("pe", lambda e, pt=pt, j=j, kc=kc, xt=xt: e.transpose(pt[:, j * 128:(j + 1) * 128], xt[:, kc * 128:(kc + 1) * 128], ident[:]),
                         reads=[xt, ident], writes=[(pt, j)])
                eng = "act" if k4 % 2 == 0 else "dve"
                dst = hT[:, k4 * 4:(k4 + 1) * 4, ti * 128:(ti + 1) * 128]
                src = pt[:].rearrange("p (j t) -> p j t", j=4)
                if eng == "act":
                    s.op("act", lambda e, dst=dst, src=src: e.activation(out=dst, in_=src, func=AF.Copy), reads=[pt], writes=[(hT, (ti, k4))])
                else:
                    s.op("dve", lambda e, dst=dst, src=src: e.tensor_copy(out=dst, in_=src), reads=[pt], writes=[(hT, (ti, k4))])
            tcount += 1
        for nb in range(nblocks):
            n0 = nb * NB
            nw = min(NB, N - n0)
            wb = wbs[wcount % 2]; wcount += 1
            for q4 in range(KC // 8):
                s.dma("pool", wb[:, q4 * 8:(q4 + 1) * 8, 0:nw], wv[:, q4 * 8:(q4 + 1) * 8, n0:n0 + nw], writes=[(wb, q4)])
            for ti in range(NTH):
                t0 = half * TH + ti * 128
                pg = psg[ocount % 4]; ot = outs[ocount % 4]; ocount += 1
                for kc in range(KC):
                    s.op("pe", lambda e, pg=pg, kc=kc, wb=wb, ti=ti, nw=nw: e.matmul(pg[:, 0:nw], lhsT=hT[:, kc, ti * 128:(ti + 1) * 128], rhs=wb[:, kc, 0:nw], start=(kc == 0), stop=(kc == KC - 1)),
                         reads=[hT, wb], writes=[pg])
                if ocount % 2 == 0:
                    s.op("act", lambda e, pg=pg, ot=ot, nw=nw: e.activation(out=ot[:, 0:nw], in_=pg[:, 0:nw], func=AF.Copy), reads=[pg], writes=[ot])
                else:
                    s.op("dve", lambda e, pg=pg, ot=ot, nw=nw: e.tensor_copy(out=ot[:, 0:nw], in_=pg[:, 0:nw]), reads=[pg], writes=[ot])
                s.dma("sp", p[t0:t0 + 128, n0:n0 + nw], ot[:, 0:nw], reads=[ot])
    s.finish()
    return nc


def build_lb1(T, TB=1024):
    nc = bass.Bass("TRN2", target_bir_lowering=False)
    C = 64
    NCH = TB // C
    NBLK = T // TB
    dt = lambda n, sh: nc.dram_tensor(n, sh, F32, kind="ExternalInput").ap()
    a_q = dt("a_q", [3, 128, T]); a_f = dt("a_f", [3, 128, T]); a_lb = dt("a_lb", [128, 6])
    a_lbraw = dt("a_lbraw", [128, 6])
    c_q = dt("c_q", [3, 128, T]); c_qs = dt("c_qs", [3, 128, T]); c_k = dt("c_k", [3, 128, T]); c_ks = dt("c_ks", [3, 128, T])
    v6 = dt("v6", [6, T, 64])
    ropeC = dt("ropeC", [128, T]); ropeS = dt("ropeS", [128, T])
    rmask_d = dt("rmask", [128, TB])
    qdec_d = dt("qdec", [128, 3, 64]); kdec_d = dt("kdec", [128, 3, 64]); cdec_d = dt("cdec", [128, 3])
    mask6_d = dt("mask6", [64, 6, 64])
    identb_d = dt("identb", [128, 128])
    o6 = nc.dram_tensor("o6", [T // C, C, 6 * 64], F32, kind="ExternalOutput").ap()
    s = Sched(nc)
    SC = 128.0 ** -0.5
    rmask = s.sb([128, TB], F32, "rmaskT"); s.dma("sp", rmask[:], rmask_d, writes=[rmask])
    qdec = s.sb([128, 3, 64], F32, "qdecT"); s.dma("sp", qdec[:], qdec_d, writes=[qdec])
    kdec = s.sb([128, 3, 64], F32, "kdecT"); s.dma("sp", kdec[:], kdec_d, writes=[kdec])
    cdec = s.sb([128, 3], F32, "cdecT"); s.dma("sp", cdec[:], cdec_d, writes=[cdec])
    mask6 = s.sb([64, 6, 64], F32, "mask6T"); s.dma("sp", mask6[:], mask6_d, writes=[mask6])
    identb = s.sb([128, 128], BF16, "identbT"); s.dma("pool", identb[:], identb_d, writes=[identb])
    lbr = s.sb([128, 6], F32, "lbr"); s.dma("sp", lbr[:], a_lbraw, writes=[lbr])
    lbm = s.sb([128, 6], F32, "lbm"); s.dma("sp", lbm[:], a_lb, writes=[lbm])
    lb = s.sb([128, 3], F32, "lb"); oml = s.sb([128, 3], F32, "oml")
    s.op("dve", lambda e: e.tensor_tensor(out=lb[:], in0=lbr[:, 3:6], in1=lbr[:, 0:3], op=ALU.subtract), reads=[lbr], writes=[lb])
    s.op("act", lambda e: e.activation(out=lb[:], in_=lb[:], func=AF.Sigmoid), reads=[lb], writes=[lb])
    s.op("dve", lambda e: e.tensor_tensor(out=lb[:], in0=lb[:], in1=lbm[:, 0:3], op=ALU.mult), reads=[lb, lbm], writes=[lb])
    s.op("dve", lambda e: e.tensor_scalar(out=oml[:], in0=lb[:], scalar1=-1.0, scalar2=1.0, op0=ALU.mult, op1=ALU.add), reads=[lb], writes=[oml])
    S = s.sb([128, 6, 64], F32, "S"); Sb = s.sb([128, 6, 64], BF16, "Sb")
    s.op("dve", lambda e: e.memset(S[:], 0.0), writes=[S])
    s.op("pool", lambda e: e.memset(Sb[:], 0.0), writes=[Sb])
    d6 = s.sb([128, NCH, 6], F32, "d6")
    Qt = [s.sb([128, TB], BF16, "Qt%d" % u) for u in range(6)]
    Kt = [s.sb([128, TB], BF16, "Kt%d" % u) for u in range(6)]
    Qi = [s.sb([128, TB], BF16, "Qi%d" % u) for u in range(6)]
    Kd = [s.sb([64, NCH, 128], BF16, "Kd%d" % u) for u in range(6)]
    Vb = [s.sb([64, NCH, 64], BF16, "Vb%d" % u) for u in range(6)]
    KdT = s.sb([128, TB], BF16, "KdT")
    sc = [s.sb([128, TB], F32, "sc%d" % i) for i in range(6)]
    rC = s.sb([128, TB], F32, "rC"); rS = s.sb([128, TB], F32, "rS")
    obuf = s.sb([64, NCH, 6 * 64], F32, "obuf")
    attm = [s.sb([64, 6, 64], BF16, "attm%d" % i) for i in range(2)]
    att_ps = [s.ps([64, 512], F32, "attps%d" % i) for i in range(2)]
    o_ps = [s.ps([64, 512], F32, "ops%d" % i) for i in range(2)]
    U_ps = [s.ps([128, 512], F32, "Ups%d" % i) for i in range(2)]
    tr_ps = [s.ps([64, 1024], BF16, "trps%d" % i) for i in range(2)]
    trc = 0

    def ch3(t):
        return t[:].rearrange("p (n c) -> p n c", c=C)

    for blk in range(NBLK):
        tsl = slice(blk * TB, (blk + 1) * TB)
        s.dma("sp", rC[:], ropeC[:, tsl], writes=[rC]); s.dma("sp", rS[:], ropeS[:, tsl], writes=[rS])
        for u in range(6):
            s.dma("pool", Vb[u][:], v6[u, tsl, :].rearrange("(n c) v -> c n v", c=C), writes=[Vb[u]])
            if u < 3:
                q, f, t2, t3, t4, t5 = sc
                s.dma("sp", q[:], a_q[u, :, tsl], writes=[q]); s.dma("sp", f[:], a_f[u, :, tsl], writes=[f])
                s.op("act", lambda e, f=f: e.activation(out=f[:], in_=f[:], func=AF.Sigmoid), reads=[f], writes=[f])
                s.op("dve", lambda e, f=f, u=u: e.tensor_scalar(out=f[:], in0=f[:], scalar1=oml[:, u:u + 1], scalar2=lb[:, u:u + 1], op0=ALU.mult, op1=ALU.add), reads=[f, oml, lb], writes=[f])
                lf = t2
                s.op("act", lambda e, f=f, lf=lf: e.activation(out=lf[:], in_=f[:], func=AF.Ln), reads=[f], writes=[lf])
                kk = f
                s.op("pool", lambda e, f=f: e.tensor_scalar(out=f[:], in0=f[:], scalar1=-1.0, scalar2=1.0, op0=ALU.mult, op1=ALU.add), reads=[f], writes=[f])
                cum = t3
                s.op("dve", lambda e, cum=cum, lf=lf: e.tensor_tensor_scan(out=cum[:], data0=rmask[:], data1=lf[:], initial=0.0, op0=ALU.mult, op1=ALU.add), reads=[rmask, lf], writes=[cum])
                cum3 = ch3(cum)
                mid = cum3[:, :, 31:32].to_broadcast([128, NCH, C])
                last = cum3[:, :, C - 1:C].to_broadcast([128, NCH, C])
                dm = t2
                s.op("pool", lambda e, dm=dm, cum3=cum3, mid=mid: e.tensor_tensor(out=ch3(dm), in0=cum3, in1=mid, op=ALU.subtract), reads=[cum], writes=[dm])
                e1 = t4
                s.op("act", lambda e, e1=e1, dm=dm: e.activation(out=e1[:], in_=dm[:], func=AF.Exp), reads=[dm], writes=[e1])
                s.op("dve", lambda e, u=u, q=q, e1=e1: e.scalar_tensor_tensor(out=Qt[u][:], in0=q[:], scalar=SC, in1=e1[:], op0=ALU.mult, op1=ALU.mult), reads=[q, e1], writes=[Qt[u]])
                e2 = t5
                s.op("act", lambda e, e2=e2, dm=dm: e.activation(out=e2[:], in_=dm[:], func=AF.Exp, scale=-1.0), reads=[dm], writes=[e2])
                s.op("pool", lambda e, u=u, kk=kk, e2=e2: e.tensor_tensor(out=Kt[u][:], in0=kk[:], in1=e2[:], op=ALU.mult), reads=[kk, e2], writes=[Kt[u]])
                e3 = t4
                s.op("act", lambda e, e3=e3, cum=cum: e.activation(out=e3[:], in_=cum[:], func=AF.Exp), reads=[cum], writes=[e3])
                s.op("dve", lambda e, u=u, q=q, e3=e3: e.scalar_tensor_tensor(out=Qi[u][:], in0=q[:], scalar=SC, in1=e3[:], op0=ALU.mult, op1=ALU.mult), reads=[q, e3], writes=[Qi[u]])
                dl = t2
                s.op("pool", lambda e, dl=dl, cum3=cum3, last=last: e.tensor_tensor(out=ch3(dl), in0=last, in1=cum3, op=ALU.subtract), reads=[cum], writes=[dl])
                e4 = t5
                s.op("act", lambda e, e4=e4, dl=dl: e.activation(out=e4[:], in_=dl[:], func=AF.Exp), reads=[dl], writes=[e4])
                s.op("pool", lambda e, kk=kk, e4=e4: e.tensor_tensor(out=KdT[:], in0=kk[:], in1=e4[:], op=ALU.mult), reads=[kk, e4], writes=[KdT])
                s.op("act", lambda e, u=u, cum3=cum3: e.activation(out=d6[:, :, u:u + 1], in_=cum3[:, :, C - 1:C], func=AF.Exp), reads=[cum], writes=[(d6, u)])
            else:
                r = u - 3
                q, qs, k, ks, t4, t5 = sc
                s.dma("sp", q[:], c_q[r, :, tsl], writes=[q]); s.dma("sp", qs[:], c_qs[r, :, tsl], writes=[qs])
                s.dma("sp", k[:], c_k[r, :, tsl], writes=[k]); s.dma("sp", ks[:], c_ks[r, :, tsl], writes=[ks])
                s.op("dve", lambda e, q=q: e.tensor_tensor(out=q[:], in0=q[:], in1=rC[:], op=ALU.mult), reads=[q, rC], writes=[q])
                s.op("pool", lambda e, qs=qs: e.tensor_tensor(out=qs[:], in0=qs[:], in1=rS[:], op=ALU.mult), reads=[qs, rS], writes=[qs])
                s.op("dve", lambda e, q=q, qs=qs: e.tensor_tensor(out=q[:], in0=q[:], in1=qs[:], op=ALU.add), reads=[q, qs], writes=[q])
                s.op("act", lambda e, u=u, q=q: e.activation(out=Qt[u][:], in_=q[:], func=AF.Copy), reads=[q], writes=[Qt[u]])
                qd = qdec[:, r:r + 1, :].to_broadcast([128, NCH, C])
                s.op("pool", lambda e, u=u, q=q, qd=qd: e.tensor_tensor(out=ch3(Qi[u]), in0=ch3(q), in1=qd, op=ALU.mult), reads=[q, qdec], writes=[Qi[u]])
                s.op("dve", lambda e, k=k: e.tensor_tensor(out=k[:], in0=k[:], in1=rC[:], op=ALU.mult), reads=[k, rC], writes=[k])
                s.op("pool", lambda e, ks=ks: e.tensor_tensor(out=ks[:], in0=ks[:], in1=rS[:], op=ALU.mult), reads=[ks, rS], writes=[ks])
                s.op("dve", lambda e, k=k, ks=ks: e.tensor_tensor(out=k[:], in0=k[:], in1=ks[:], op=ALU.add), reads=[k, ks], writes=[k])
                s.op("act", lambda e, u=u, k=k: e.activation(out=Kt[u][:], in_=k[:], func=AF.Copy, scale=SC), reads=[k], writes=[Kt[u]])
                kd = kdec[:, r:r + 1, :].to_broadcast([128, NCH, C])
                s.op("pool", lambda e, k=k, kd=kd: e.tensor_tensor(out=ch3(KdT), in0=ch3(k), in1=kd, op=ALU.mult), reads=[k, kdec], writes=[KdT])
                cd = cdec[:, r:r + 1].to_broadcast([128, NCH])
                s.op("dve", lambda e, u=u, cd=cd: e.tensor_copy(out=d6[:, :, u], in_=cd), reads=[cdec], writes=[(d6, u)])
            for g in range(NCH // 8):
                tp = tr_ps[trc % 2]; trc += 1
                for j in range(8):
                    n = g * 8 + j
                    s.op("pe", lambda e, tp=tp, j=j, n=n: e.transpose(tp[:, j * 128:(j + 1) * 128], KdT[:, n * C:(n + 1) * C], identb[:]),
                         reads=[KdT, identb], writes=[(tp, j)])
                s.op("act", lambda e, tp=tp, g=g, u=u: e.activation(out=Kd[u][:, g * 8:(g + 1) * 8, :], in_=tp[:].rearrange("p (j k) -> p j k", j=8), func=AF.Copy),
                     reads=[tp], writes=[(Kd[u], g)])
        for n in range(NCH):
            gi = blk * NCH + n
            ap_ = att_ps[gi % 2]; op_ = o_ps[gi % 2]; up_ = U_ps[gi % 2]; am = attm[gi % 2]
            csl = slice(n * C, (n + 1) * C)
            for u in range(6):
                s.op("pe", lambda e, u=u, ap_=ap_, csl=csl: e.matmul(ap_[:, u * 64:(u + 1) * 64], lhsT=Kt[u][:, csl], rhs=Qt[u][:, csl], start=True, stop=True),
                     reads=[Kt[u], Qt[u]], writes=[(ap_, u)])
            s.op("dve", lambda e, ap_=ap_, am=am: e.tensor_tensor(out=am[:].rearrange("p u t -> p (u t)"), in0=ap_[:, 0:384], in1=mask6[:].rearrange("p u t -> p (u t)"), op=ALU.mult),
                 reads=[ap_, mask6], writes=[am])
            for u in range(6):
                s.op("pe", lambda e, u=u, up_=up_, n=n: e.matmul(up_[:, u * 64:(u + 1) * 64], lhsT=Kd[u][:, n, :], rhs=Vb[u][:, n, :], start=True, stop=True),
                     reads=[Kd[u], Vb[u]], writes=[(up_, u)])
            for u in range(6):
                s.op("pe", lambda e, u=u, op_=op_, am=am, n=n: e.matmul(op_[:, u * 64:(u + 1) * 64], lhsT=am[:, u, :], rhs=Vb[u][:, n, :], start=True, stop=False),
                     reads=[am, Vb[u]], writes=[(op_, u)])
                s.op("pe", lambda e, u=u, op_=op_, csl=csl: e.matmul(op_[:, u * 64:(u + 1) * 64], lhsT=Qi[u][:, csl], rhs=Sb[:, u, :], start=False, stop=True),
                     reads=[Qi[u], Sb], writes=[(op_, u)])
            s.op("act", lambda e, op_=op_, n=n: e.activation(out=obuf[:, n, :], in_=op_[:, 0:384], func=AF.Copy), reads=[op_], writes=[(obuf, n)])
            dB = d6[:, n, :].unsqueeze(2).to_broadcast([128, 6, 64])
            s.op("dve", lambda e, dB=dB: e.tensor_tensor(out=S[:], in0=S[:], in1=dB, op=ALU.mult), reads=[S, d6], writes=[S])
            s.op("dve", lambda e, up_=up_: e.tensor_tensor(out=S[:].rearrange("p u v -> p (u v)"), in0=S[:].rearrange("p u v -> p (u v)"), in1=up_[:, 0:384], op=ALU.add), reads=[S, up_], writes=[S])
            s.op("act", lambda e: e.activation(out=Sb[:], in_=S[:], func=AF.Copy), reads=[S], writes=[Sb])
        s.dma("sp", o6[blk * NCH:(blk + 1) * NCH].rearrange("n t u -> t n u"), obuf[:], reads=[obuf])
    s.finish()
    return nc


NEG1 = -1.0e30

def build_lb2(T, NQ, debug=False):
    nc = bass.Bass("TRN2", target_bir_lowering=False)
    NKT = T // 128
    dt = lambda n, sh: nc.dram_tensor(n, sh, F32, kind="ExternalInput").ap()
    qT = dt("qT", [NQ, 128, 8, 128]); qsw = dt("qsw", [NQ, 32, 8, 128]); Cq = dt("Cq", [NQ, 32, 128]); Sq = dt("Sq", [NQ, 32, 128])
    kTt = dt("kTt", [NKT, 128, 8, 128]); ksw = dt("ksw", [NKT, 32, 8, 128]); Ck = dt("Ck", [NKT, 32, 128]); Sk = dt("Sk", [NKT, 32, 128])
    Vt = dt("Vt", [NKT, 128, 8, 128])
    qiT = dt("qiT", [NQ, 64, 16, 128]); qisw = dt("qisw", [NQ, 16, 16, 128]); Cqi = dt("Cqi", [NQ, 16, 128]); Sqi = dt("Sqi", [NQ, 16, 128])
    kiT = dt("kiT", [64, T]); kisw = dt("kisw", [16, T]); Cki = dt("Cki", [16, T]); Ski = dt("Ski", [16, T])
    iw = dt("iw", [NQ, 128, 16])
    pen_d = dt("pen", [128, 1024])
    identb_d = dt("identb", [128, 128])
    ob = nc.dram_tensor("ob", [NQ, 128, 1024], F32, kind="ExternalOutput").ap()
    NMAX = 1024 * NQ
    if debug:
        dbg1 = nc.dram_tensor("dbg1", [NQ, 128, NMAX], F32, kind="ExternalOutput").ap()
        dbg2 = nc.dram_tensor("dbg2", [NQ, 128, NMAX], F32, kind="ExternalOutput").ap()
        dbg3 = nc.dram_tensor("dbg3", [NQ, 128, 4, 258], F32, kind="ExternalOutput").ap()
    s = Sched(nc)
    SC = 128.0 ** -0.5
    assert NMAX <= T
    pen = s.sb([128, 1024], F32, "penT"); s.dma("sp", pen[:], pen_d, writes=[pen])
    identb = s.sb([128, 128], BF16, "identbT"); s.dma("pool", identb[:], identb_d, writes=[identb])
    kiTb = s.sb([64, NMAX], BF16, "kiTb")
    PW = min(512, NMAX)
    stg = s.sb([64, PW], F32, "stg"); stg2 = s.sb([16, 3, PW], F32, "stg2"); stg3 = s.sb([16, 2, PW], F32, "stg3")
    for pc in range(NMAX // PW):
        sl = slice(pc * PW, (pc + 1) * PW)
        s.dma("sp", stg[:], kiT[:, sl], writes=[stg])
        s.dma("sp", stg2[:, 0, :], kisw[:, sl], writes=[(stg2, 0)])
        s.dma("sp", stg2[:, 1, :], Cki[:, sl], writes=[(stg2, 1)])
        s.dma("sp", stg2[:, 2, :], Ski[:, sl], writes=[(stg2, 2)])
        s.op("dve", lambda e: e.tensor_tensor(out=stg3[:, 0, :], in0=stg[0:16, :], in1=stg2[:, 1, :], op=ALU.mult), reads=[stg, stg2], writes=[(stg3, 0)])
        s.op("pool", lambda e: e.tensor_tensor(out=stg3[:, 1, :], in0=stg2[:, 0, :], in1=stg2[:, 2, :], op=ALU.mult), reads=[stg2], writes=[(stg3, 1)])
        s.op("dve", lambda e: e.tensor_tensor(out=stg[0:16, :], in0=stg3[:, 0, :], in1=stg3[:, 1, :], op=ALU.add), reads=[stg3], writes=[stg])
        s.op("act", lambda e, sl=sl: e.activation(out=kiTb[:, sl], in_=stg[:], func=AF.Copy), reads=[stg], writes=[(kiTb, pc)])
    work = s.sb([128, NMAX], F32, "work")
    qf = s.sb([128, 8, 128], F32, "qf"); cq_t = s.sb([32, 2, 128], F32, "cq_t")
    qb = s.sb([128, 8, 128], BF16, "qb")
    qif = s.sb([64, 16, 128], F32, "qif"); cqi_t = s.sb([16, 2, 128], F32, "cqi_t")
    qib = s.sb([64, 16, 128], BF16, "qib")
    rt1 = s.sb([32, 16, 128], F32, "rt1"); rt2 = s.sb([32, 16, 128], F32, "rt2")
    iwt = s.sb([128, 16], F32, "iwt")
    rl = [s.sb([128, 512], F32, "rl%d" % i) for i in range(3)]
    m8 = s.sb([128, 8], F32, "m8")
    m128 = [s.sb([128, 128], BF16, "m128_%d" % i) for i in range(2)]
    mT = [s.sb([128, 128], BF16, "mT%d" % i) for i in range(2)]
    kb16 = [s.sb([128, 8, 128], BF16, "kb16_%d" % i) for i in range(2)]
    kr = [s.sb([32, 8, 128], F32, "kr0")] * 2
    ksw_t = [s.sb([32, 8, 128], F32, "kswt0")] * 2
    ck_t = [s.sb([32, 2, 128], F32, "ckt%d" % i) for i in range(2)]
    vb = [s.sb([128, 8, 129], BF16, "vb%d" % i) for i in range(2)]
    for v_ in vb:
        s.op("pool", lambda e, v_=v_: e.memset(v_[:], 1.0), writes=[v_])
    pexp = [s.sb([128, 4, 128], BF16, "pexp%d" % i) for i in range(2)]
    pm = [s.sb([128, 4, 128], BF16, "pm%d" % i) for i in range(4)]
    otile = s.sb([128, 8, 128], F32, "otile"); rec = s.sb([128, 8, 1], F32, "rec")
    psA = [s.ps([128, 512], F32, "psA%d" % i) for i in range(3)]
    oacc = [s.ps([128, 512], F32, "oacc%d" % i) for i in range(4)]
    psT = s.ps([128, 2, 128], BF16, "psT")
    pac = 0
    pairc = 0
    zl = s.sb([128, 128], BF16, "zl"); zr = s.sb([128, 512], BF16, "zr")
    s.op("pool", lambda e: e.memset(zl[:], 0.0), writes=[zl])
    s.op("pool", lambda e: e.memset(zr[:], 0.0), writes=[zr])

    def bc(t2d, n):
        return t2d.unsqueeze(1).to_broadcast([t2d.shape[0], n, 128])

    for i in range(NQ):
        N = 1024 * (i + 1)
        s.dma("sp", qf[:], qT[i], writes=[qf]); s.dma("sp", rt2[:, 0:8, :], qsw[i], writes=[rt2])
        s.dma("sp", cq_t[:, 0, :], Cq[i], writes=[(cq_t, 0)]); s.dma("sp", cq_t[:, 1, :], Sq[i], writes=[(cq_t, 1)])
        s.op("dve", lambda e: e.tensor_tensor(out=rt1[:, 0:8, :], in0=qf[0:32], in1=bc(cq_t[:, 0, :], 8), op=ALU.mult), reads=[qf, cq_t], writes=[rt1])
        s.op("pool", lambda e: e.tensor_tensor(out=rt2[:, 0:8, :], in0=rt2[:, 0:8, :], in1=bc(cq_t[:, 1, :], 8), op=ALU.mult), reads=[rt2, cq_t], writes=[rt2])
        s.op("dve", lambda e: e.tensor_tensor(out=qf[0:32], in0=rt1[:, 0:8, :], in1=rt2[:, 0:8, :], op=ALU.add), reads=[rt1, rt2], writes=[qf])
        s.op("act", lambda e: e.activation(out=qb[:], in_=qf[:], func=AF.Copy), reads=[qf], writes=[qb])
        s.dma("sp", qif[:], qiT[i], writes=[qif]); s.dma("sp", rt2[0:16], qisw[i], writes=[rt2])
        s.dma("sp", cqi_t[:, 0, :], Cqi[i], writes=[(cqi_t, 0)]); s.dma("sp", cqi_t[:, 1, :], Sqi[i], writes=[(cqi_t, 1)])
        s.op("dve", lambda e: e.tensor_tensor(out=rt1[0:16], in0=qif[0:16], in1=bc(cqi_t[:, 0, :], 16), op=ALU.mult), reads=[qif, cqi_t], writes=[rt1])
        s.op("pool", lambda e: e.tensor_tensor(out=rt2[0:16], in0=rt2[0:16], in1=bc(cqi_t[:, 1, :], 16), op=ALU.mult), reads=[rt2, cqi_t], writes=[rt2])
        s.op("dve", lambda e: e.tensor_tensor(out=qif[0:16], in0=rt1[0:16], in1=rt2[0:16], op=ALU.add), reads=[rt1, rt2], writes=[qif])
        s.op("act", lambda e: e.activation(out=qib[:], in_=qif[:], func=AF.Copy), reads=[qif], writes=[qib])
        s.dma("sp", iwt[:], iw[i], writes=[iwt])
        for kb in range(N // 512):
            ksl = slice(kb * 512, (kb + 1) * 512)
            for h in range(16):
                pa = psA[pac % 3]; r_ = rl[pac % 3]; pac += 1
                s.op("pe", lambda e, pa=pa, h=h, ksl=ksl: e.matmul(pa[:], lhsT=qib[:, h, :], rhs=kiTb[:, ksl], start=True, stop=True), reads=[qib, kiTb], writes=[pa])
                s.op("act", lambda e, pa=pa, r_=r_: e.activation(out=r_[:], in_=pa[:], func=AF.Relu), reads=[pa], writes=[r_])
                if h == 0:
                    s.op("dve", lambda e, r_=r_, ksl=ksl: e.tensor_scalar(out=work[:, ksl], in0=r_[:], scalar1=iwt[:, 0:1], scalar2=None, op0=ALU.mult), reads=[r_, iwt], writes=[(work, kb)])
                else:
                    s.op("dve", lambda e, r_=r_, ksl=ksl, h=h: e.scalar_tensor_tensor(out=work[:, ksl], in0=r_[:], scalar=iwt[:, h:h + 1], in1=work[:, ksl], op0=ALU.mult, op1=ALU.add), reads=[r_, iwt, (work, kb)], writes=[(work, kb)])
        zs = slice(N - 1024, N)
        s.op("dve", lambda e, zs=zs: e.tensor_tensor(out=work[:, zs], in0=work[:, zs], in1=pen[:], op=ALU.min), reads=[work, pen], writes=[work])
        if debug:
            s.dma("sp", dbg1[i, :, 0:N], work[:, 0:N], reads=[work])
        for r in range(32):
            s.op("dve", lambda e, N=N: e.max(out=m8[:], in_=work[:, 0:N]), reads=[work], writes=[m8])
            s.op("dve", lambda e, N=N: e.match_replace(out=work[:, 0:N], in_to_replace=m8[:], in_values=work[:, 0:N], imm_value=NEG1), reads=[work, m8], writes=[work])
        if debug:
            s.dma("sp", dbg2[i, :, 0:N], work[:, 0:N], reads=[work])
        nkt = N // 128
        for b in range(4):
            s.op("pe", lambda e, b=b: e.matmul(oacc[b][:], lhsT=zl[:], rhs=zr[:], start=True, stop=True), reads=[zl, zr], writes=[oacc[b]])
        for kt in range(nkt):
            pp = pairc % 2; pairc += 1
            mm = m128[pp]; mt = mT[pp]
            s.op("pool", lambda e, mm=mm, kt=kt: e.tensor_scalar(out=mm[:], in0=work[:, kt * 128:(kt + 1) * 128], scalar1=NEG1, scalar2=None, op0=ALU.is_equal), reads=[work], writes=[mm])
            s.op("pe", lambda e, mm=mm, pp=pp: e.transpose(psT[:, pp, :], mm[:], identb[:]), reads=[mm, identb], writes=[(psT, pp)])
            s.op("act", lambda e, mt=mt, pp=pp: e.activation(out=mt[:], in_=psT[:, pp, :], func=AF.Copy), reads=[(psT, pp)], writes=[mt])
            kb_ = kb16[pp]; kr_ = kr[pp]; ks_ = ksw_t[pp]; ck_ = ck_t[pp]; vb_ = vb[pp]
            s.dma("pool", kb_[:], kTt[kt], writes=[kb_])
            s.dma("sp", kr_[:], kTt[kt, 0:32], writes=[kr_]); s.dma("sp", ks_[:], ksw[kt], writes=[ks_])
            s.dma("sp", ck_[:, 0, :], Ck[kt], writes=[(ck_, 0)]); s.dma("sp", ck_[:, 1, :], Sk[kt], writes=[(ck_, 1)])
            s.dma("pool", vb_[:, :, 0:128], Vt[kt], writes=[vb_])
            s.op("dve", lambda e, kr_=kr_, ck_=ck_: e.tensor_tensor(out=kr_[:], in0=kr_[:], in1=bc(ck_[:, 0, :], 8), op=ALU.mult), reads=[kr_, ck_], writes=[kr_])
            s.op("pool", lambda e, ks_=ks_, ck_=ck_: e.tensor_tensor(out=ks_[:], in0=ks_[:], in1=bc(ck_[:, 1, :], 8), op=ALU.mult), reads=[ks_, ck_], writes=[ks_])
            s.op("dve", lambda e, kb_=kb_, kr_=kr_, ks_=ks_: e.tensor_tensor(out=kb_[0:32], in0=kr_[:], in1=ks_[:], op=ALU.add), reads=[kr_, ks_], writes=[kb_])
            for g in range(2):
                pa = psA[pac % 3]; pac += 1
                pe_ = pexp[g]; pm_ = pm[(pp * 2 + g)]
                for hh in range(4):
                    h = 4 * g + hh
                    s.op("pe", lambda e, pa=pa, hh=hh, h=h, kb_=kb_: e.matmul(pa[:, hh * 128:(hh + 1) * 128], lhsT=kb_[:, h, :], rhs=qb[:, h, :], start=True, stop=True), reads=[kb_, qb], writes=[(pa, hh)])
                s.op("act", lambda e, pa=pa, pe_=pe_: e.activation(out=pe_[:].rearrange("p h q -> p (h q)"), in_=pa[:], func=AF.Exp, scale=SC), reads=[pa], writes=[pe_])
                eng = "dve" if g == 0 else "pool"
                s.op(eng, lambda e, pe_=pe_, pm_=pm_, mt=mt: e.tensor_tensor(out=pm_[:], in0=pe_[:], in1=bc(mt[:], 4), op=ALU.mult), reads=[pe_, mt], writes=[pm_])
            for h in range(8):
                pm_ = pm[pp * 2 + h // 4]
                oa = oacc[h // 2]
                s.op("pe", lambda e, oa=oa, h=h, pm_=pm_, vb_=vb_, kt=kt, nkt=nkt: e.matmul(oa[:, (h % 2) * 129:(h % 2) * 129 + 129], lhsT=pm_[:, h % 4, :], rhs=vb_[:, h, :], start=False, stop=True),
                     reads=[pm_, vb_], writes=[(oa, h % 2)])
        if debug:
            dbt = s.sb([128, 4, 258], F32, "dbt%d" % i)
            for b in range(4):
                s.op("dve", lambda e, b=b, dbt=dbt: e.tensor_copy(out=dbt[:, b, :], in_=oacc[b][:, 0:258]), reads=[oacc[b]], writes=[(dbt, b)])
            s.dma("sp", dbg3[i], dbt[:], reads=[dbt])
        for b in range(4):
            ov = oacc[b][:, 0:258].rearrange("p (h c) -> p h c", h=2)
            s.op("dve", lambda e, ov=ov, b=b: e.reciprocal(out=rec[:, 2 * b:2 * b + 2, :], in_=ov[:, :, 128:129]), reads=[oacc[b]], writes=[(rec, b)])
            s.op("dve", lambda e, ov=ov, b=b: e.tensor_tensor(out=otile[:, 2 * b:2 * b + 2, :], in0=ov[:, :, 0:128], in1=rec[:, 2 * b:2 * b + 2, :].to_broadcast([128, 2, 128]), op=ALU.mult), reads=[oacc[b], (rec, b)], writes=[(otile, b)])
        s.dma("sp", ob[i].rearrange("p (h d) -> p h d", h=8), otile[:], reads=[otile])
    s.finish()
    return nc


LN_EPS = 1e-5
NORM_EPS = 1e-6
DN_ALPHA = 4 ** 0.25
LIMIT = 7.0
SALPHA = 1.702

def build_lc(Tc, NE=32, D=4096, F=768, upto=5):
    nc = bass.Bass("TRN2", target_bir_lowering=False)
    dt = lambda n, sh: nc.dram_tensor(n, sh, F32, kind="ExternalInput").ap()
    KC = D // 128
    FB = F // 128
    h_d = dt("h", [Tc, D]); oa_d = dt("oa", [Tc, 1536]); ag_d = dt("ag", [Tc, 1536]); ob_d = dt("ob", [Tc, 1024])
    oc_d = dt("oc", [Tc, 1536]); cg_d = dt("cg", [Tc, 1536]); gA_d = dt("gA", [1536]); gC_d = dt("gC", [1536])
    wout_d = dt("wout", [D, D]); l1g = dt("l1g", [D]); l1b = dt("l1b", [D]); l2g = dt("l2g", [D]); l2b = dt("l2b", [D])
    rw_d = dt("rw", [D, NE]); rb_d = dt("rb", [NE])
    NEW = NE if upto >= 4 else 1
    wgu_d = dt("wgu", [NEW, D, 2 * F]); bgu_d = dt("bguT", [128, NE, 2 * FB]); wd_d = dt("wd", [NEW, F, D]); bd_d = dt("bd", [NE, D])
    ident_d = dt("ident", [128, 128])
    hout = nc.dram_tensor("hout", [Tc, D], F32, kind="ExternalOutput").ap()
    s = Sched(nc)
    GT = 256
    NT = GT // 128
    ident = s.sb([128, 128], F32, "identT"); s.dma("sp", ident[:], ident_d, writes=[ident])
    epsT = s.sb([128, 2], F32, "epsT")
    s.op("dve", lambda e: e.memset(epsT[:, 0:1], LN_EPS), writes=[(epsT, 0)])
    s.op("dve", lambda e: e.memset(epsT[:, 1:2], NORM_EPS), writes=[(epsT, 1)])
    rw = s.sb([128, KC, NE], F32, "rwT"); s.dma("sp", rw[:], rw_d.rearrange("(kc p) e -> p kc e", p=128), writes=[rw])
    rwh = s.sb([128, KC, NE], BF16, "rwh"); rwl = s.sb([128, KC, NE], BF16, "rwl")
    s.op("act", lambda e: e.activation(out=rwh[:], in_=rw[:], func=AF.Copy), reads=[rw], writes=[rwh])
    s.op("dve", lambda e: e.tensor_tensor(out=rwl[:], in0=rw[:], in1=rwh[:], op=ALU.subtract), reads=[rw, rwh], writes=[rwl])
    sc4l = s.sb([128, 4, 128], BF16, "sc4l")
    rbB = s.sb([128, NE], F32, "rbB"); s.dma("sp", rbB[:], rb_d.partition_broadcast(128), writes=[rbB])
    bg = s.sb([128, NE, 2 * FB], F32, "bgT"); s.dma("sp", bg[:], bgu_d, writes=[bg])
    s.op("dve", lambda e: e.tensor_scalar(out=bg[:, :, FB:2 * FB], in0=bg[:, :, FB:2 * FB], scalar1=1.0, scalar2=None, op0=ALU.add), reads=[bg], writes=[bg])
    bdb = s.sb([NE, D], BF16, "bdb"); s.dma("pool", bdb[:], bd_d, writes=[bdb])
    hres = [s.sb([128, D], F32, "hres%d" % i) for i in range(NT)]
    hT = s.sb([128, KC, GT], BF16, "hT")
    scr = s.sb([128, 4096], F32, "scr")
    lnb = s.sb([128, 2, D], F32, "lnb")
    WP = 6144
    wpool = [s.sb([128, WP], BF16, "wp%d" % i) for i in range(3)]
    wc = [0]
    actT = s.sb([128, FB, GT], BF16, "actT")
    tmpA = s.sb([128, 512], F32, "tmpA"); tmpB = s.sb([128, 512], F32, "tmpB")
    stats = s.sb([128, D // 512, 6], F32, "stats"); mv = s.sb([128, 2], F32, "mv"); rstd = s.sb([128, 1], F32, "rstd")
    sm = s.sb([128, 64], F32, "sm")
    G = [s.sb([128, NE], F32, "G%d" % i) for i in range(NT)]
    GTt = [s.sb([NE, 128], BF16, "GTt%d" % i) for i in range(NT)]
    lgt = s.sb([128, NE], F32, "lgt"); m8 = s.sb([128, 8], F32, "m8"); ex = s.sb([128, NE], F32, "ex"); msk = s.sb([128, NE], F32, "msk")
    sc4 = s.sb([128, 4, 128], F32, "sc4")
    bank = [s.ps([128, 512], F32, "bank%d" % i) for i in range(8)]

    def nextw():
        w = wpool[wc[0] % 3]; wc[0] += 1
        return w

    def transpose_tile(src, ti, also_router_ps=None):
        for k4 in range(KC // 4):
            pt = bank[k4 % 2]
            for j in range(4):
                kc = k4 * 4 + j
                s.op("pe", lambda e, pt=pt, j=j, kc=kc: e.transpose(pt[:, j * 128:(j + 1) * 128], src[:, kc * 128:(kc + 1) * 128], ident[:]),
                     reads=[src, ident], writes=[(pt, j)])
            dst = hT[:, k4 * 4:(k4 + 1) * 4, ti * 128:(ti + 1) * 128]
            srcv = pt[:].rearrange("p (j t) -> p j t", j=4)
            s.op("act", lambda e, dst=dst, srcv=srcv: e.activation(out=dst, in_=srcv, func=AF.Copy), reads=[pt], writes=[(hT, (ti, k4))])
            if also_router_ps is not None:
                s.op("dve", lambda e, srcv=srcv, dst=dst: e.tensor_tensor(out=sc4l[:], in0=srcv, in1=dst, op=ALU.subtract), reads=[pt, (hT, (ti, k4))], writes=[sc4l])
                for j in range(4):
                    kc = k4 * 4 + j
                    hi = hT[:, kc, ti * 128:(ti + 1) * 128]
                    s.op("pe", lambda e, hi=hi, kc=kc: e.matmul(also_router_ps[:, 0:NE], lhsT=hi, rhs=rwh[:, kc, :], start=(kc == 0), stop=False),
                         reads=[hT, rwh], writes=[also_router_ps])
                    s.op("pe", lambda e, j=j, kc=kc: e.matmul(also_router_ps[:, 0:NE], lhsT=sc4l[:, j, :], rhs=rwh[:, kc, :], start=False, stop=False),
                         reads=[sc4l, rwh], writes=[also_router_ps])
                    s.op("pe", lambda e, hi=hi, kc=kc: e.matmul(also_router_ps[:, 0:NE], lhsT=hi, rhs=rwl[:, kc, :], start=False, stop=(kc == KC - 1)),
                         reads=[hT, rwl], writes=[also_router_ps])

    def layer_norm(xt, gd, bd_):
        s.dma("sp", lnb[:, 0, :], gd.partition_broadcast(128), writes=[(lnb, 0)])
        s.dma("sp", lnb[:, 1, :], bd_.partition_broadcast(128), writes=[(lnb, 1)])
        emit_ln_rows(s, xt, 128, D, LN_EPS, "ln", stats, mv, rstd, epsT)
        s.op("pool", lambda e: e.tensor_tensor(out=xt[:], in0=xt[:], in1=lnb[:, 0, :], op=ALU.mult), reads=[xt, lnb], writes=[xt])
        s.op("dve", lambda e: e.tensor_tensor(out=xt[:], in0=xt[:], in1=lnb[:, 1, :], op=ALU.add), reads=[xt, lnb], writes=[xt])

    for grp in range(Tc // GT):
        for ti in range(NT):
            t0 = grp * GT + ti * 128
            rows = slice(t0, t0 + 128)
            s.dma("sp", hres[ti][:], h_d[rows, :], writes=[hres[ti]])
            mx = scr
            s.dma("sp", mx[:, 0:1536], oa_d[rows, :], writes=[(mx, "a")])
            s.dma("sp", mx[:, 1536:2560], ob_d[rows, :], writes=[(mx, "b")])
            s.dma("sp", mx[:, 2560:4096], oc_d[rows, :], writes=[(mx, "c")])
            gt = lnb[:, 0, 0:3072]; gn = lnb[:, 1, 0:3072]
            s.dma("sp", gt[:, 0:1536], ag_d[rows, :], writes=[(lnb, 0)]); s.dma("sp", gt[:, 1536:3072], cg_d[rows, :], writes=[(lnb, 0)])
            s.dma("sp", gn[:, 0:1536], gA_d.partition_broadcast(128), writes=[(lnb, 1)]); s.dma("sp", gn[:, 1536:3072], gC_d.partition_broadcast(128), writes=[(lnb, 1)])
            s.op("act", lambda e, gt=gt: e.activation(out=gt, in_=gt, func=AF.Silu), reads=[(lnb, 0)], writes=[(lnb, 0)])
            s.op("pool", lambda e, gt=gt, gn=gn: e.tensor_tensor(out=gt, in0=gt, in1=gn, op=ALU.mult), reads=[lnb], writes=[(lnb, 0)])
            xa = mx[:, 0:1536].rearrange("p (h v) -> p h v", v=128)
            sq = lnb[:, 1, 0:1536].rearrange("p (h v) -> p h v", v=128)
            s.op("dve", lambda e, xa=xa, sq=sq: e.tensor_tensor(out=sq, in0=xa, in1=xa, op=ALU.mult), reads=[(mx, "a"), lnb], writes=[(lnb, 1)])
            s.op("dve", lambda e, sq=sq: e.tensor_reduce(out=sm[:, 0:12], in_=sq, op=ALU.add, axis=AX.X), reads=[(lnb, 1)], writes=[sm])
            s.op("act", lambda e: e.activation(out=sm[:, 0:12], in_=sm[:, 0:12], func=AF.Sqrt, bias=epsT[:, 1:2], scale=1.0 / 128), reads=[sm, epsT], writes=[sm])
            s.op("dve", lambda e: e.reciprocal(out=sm[:, 0:12], in_=sm[:, 0:12]), reads=[sm], writes=[sm])
            s.op("dve", lambda e, xa=xa: e.tensor_tensor(out=xa, in0=xa, in1=sm[:, 0:12].unsqueeze(2).to_broadcast([128, 12, 128]), op=ALU.mult), reads=[(mx, "a"), sm], writes=[(mx, "a")])
            xc = mx[:, 2560:4096].rearrange("p (h v) -> p h v", v=256)
            sq2 = lnb[:, 1, 0:1536].rearrange("p (h v) -> p h v", v=256)
            s.op("dve", lambda e, xc=xc: e.tensor_reduce(out=sm[:, 16:22], in_=xc, op=ALU.add, axis=AX.X), reads=[(mx, "c")], writes=[sm])
            s.op("dve", lambda e: e.tensor_scalar(out=sm[:, 16:22], in0=sm[:, 16:22], scalar1=1.0 / 256, scalar2=None, op0=ALU.mult), reads=[sm], writes=[sm])
            s.op("dve", lambda e, xc=xc: e.tensor_tensor(out=xc, in0=xc, in1=sm[:, 16:22].unsqueeze(2).to_broadcast([128, 6, 256]), op=ALU.subtract), reads=[(mx, "c"), sm], writes=[(mx, "c")])
            s.op("dve", lambda e, xc=xc, sq2=sq2: e.tensor_tensor(out=sq2, in0=xc, in1=xc, op=ALU.mult), reads=[(mx, "c"), lnb], writes=[(lnb, 1)])
            s.op("dve", lambda e, sq2=sq2: e.tensor_reduce(out=sm[:, 24:30], in_=sq2, op=ALU.add, axis=AX.X), reads=[(lnb, 1)], writes=[sm])
            s.op("act", lambda e: e.activation(out=sm[:, 24:30], in_=sm[:, 24:30], func=AF.Sqrt, bias=epsT[:, 1:2], scale=1.0 / 256), reads=[sm, epsT], writes=[sm])
            s.op("dve", lambda e: e.reciprocal(out=sm[:, 24:30], in_=sm[:, 24:30]), reads=[sm], writes=[sm])
            s.op("dve", lambda e, xc=xc: e.tensor_tensor(out=xc, in0=xc, in1=sm[:, 24:30].unsqueeze(2).to_broadcast([128, 6, 256]), op=ALU.mult), reads=[(mx, "c"), sm], writes=[(mx, "c")])
            s.op("pool", lambda e, gt=gt: e.tensor_tensor(out=mx[:, 0:1536], in0=mx[:, 0:1536], in1=gt[:, 0:1536], op=ALU.mult), reads=[mx, lnb], writes=[(mx, "a")])
            s.op("pool", lambda e, gt=gt: e.tensor_tensor(out=mx[:, 2560:4096], in0=mx[:, 2560:4096], in1=gt[:, 1536:3072], op=ALU.mult), reads=[mx, lnb], writes=[(mx, "c")])
            transpose_tile(mx, ti)
        CB = 128
        wv = wout_d.rearrange("(kc p) n -> p kc n", p=128)
        for cb in range(D // CB):
            w = nextw()
            wvw = w[:, 0:KC * CB].rearrange("p (kc n) -> p kc n", n=CB)
            for q4 in range(4):
                s.dma("pool", wvw[:, q4 * 8:(q4 + 1) * 8, :], wv[:, q4 * 8:(q4 + 1) * 8, cb * CB:(cb + 1) * CB], writes=[(w, q4)])
            for ti in range(NT):
                pg = bank[2 + (cb * NT + ti) % 4]
                for kc in range(KC):
                    s.op("pe", lambda e, pg=pg, kc=kc, wvw=wvw, ti=ti: e.matmul(pg[:, 0:CB], lhsT=hT[:, kc, ti * 128:(ti + 1) * 128], rhs=wvw[:, kc, :], start=(kc == 0), stop=(kc == KC - 1)),
                         reads=[hT, w], writes=[pg])
                s.op("dve", lambda e, pg=pg, ti=ti, cb=cb: e.scalar_tensor_tensor(out=hres[ti][:, cb * CB:(cb + 1) * CB], in0=hres[ti][:, cb * CB:(cb + 1) * CB], scalar=DN_ALPHA, in1=pg[:, 0:CB], op0=ALU.mult, op1=ALU.add),
                     reads=[pg, hres[ti]], writes=[hres[ti]])
        if upto < 3:
            for ti in range(NT):
                t0 = grp * GT + ti * 128
                s.dma("sp", hout[t0:t0 + 128, :], hres[ti][:], reads=[hres[ti]])
            continue
        for ti in range(NT):
            layer_norm(hres[ti], l1g, l1b)
            rp = bank[6]
            transpose_tile(hres[ti], ti, also_router_ps=rp)
            s.op("dve", lambda e, rp=rp: e.tensor_tensor(out=lgt[:], in0=rp[:, 0:NE], in1=rbB[:], op=ALU.add), reads=[rp, rbB], writes=[lgt])
            s.op("dve", lambda e: e.max(out=m8[:], in_=lgt[:]), reads=[lgt], writes=[m8])
            s.op("dve", lambda e: e.tensor_scalar(out=msk[:], in0=lgt[:], scalar1=m8[:, 3:4], scalar2=None, op0=ALU.is_ge), reads=[lgt, m8], writes=[msk])
            s.op("dve", lambda e: e.tensor_scalar(out=ex[:], in0=lgt[:], scalar1=m8[:, 0:1], scalar2=None, op0=ALU.subtract), reads=[lgt, m8], writes=[ex])
            s.op("act", lambda e: e.activation(out=ex[:], in_=ex[:], func=AF.Exp), reads=[ex], writes=[ex])
            s.op("dve", lambda e: e.tensor_tensor(out=ex[:], in0=ex[:], in1=msk[:], op=ALU.mult), reads=[ex, msk], writes=[ex])
            s.op("dve", lambda e: e.tensor_reduce(out=sm[:, 32:33], in_=ex[:], op=ALU.add, axis=AX.X), reads=[ex], writes=[sm])
            s.op("dve", lambda e: e.reciprocal(out=sm[:, 32:33], in_=sm[:, 32:33]), reads=[sm], writes=[sm])
            s.op("dve", lambda e, ti=ti: e.tensor_scalar(out=G[ti][:], in0=ex[:], scalar1=sm[:, 32:33], scalar2=None, op0=ALU.mult), reads=[ex, sm], writes=[G[ti]])
            tp = bank[7]
            s.op("pe", lambda e, tp=tp, ti=ti: e.transpose(tp[0:NE, 0:128], G[ti][:], ident[:]), reads=[G[ti], ident], writes=[tp])
            s.op("act", lambda e, tp=tp, ti=ti: e.activation(out=GTt[ti][:], in_=tp[0:NE, 0:128], func=AF.Copy), reads=[tp], writes=[GTt[ti]])
            for db in range(D // 512):
                pg = bank[2 + db % 4]
                s.op("pe", lambda e, pg=pg, ti=ti, db=db: e.matmul(pg[:], lhsT=GTt[ti][:], rhs=bdb[:, db * 512:(db + 1) * 512], start=True, stop=True), reads=[GTt[ti], bdb], writes=[pg])
                s.op("dve", lambda e, pg=pg, ti=ti, db=db: e.scalar_tensor_tensor(out=hres[ti][:, db * 512:(db + 1) * 512], in0=hres[ti][:, db * 512:(db + 1) * 512], scalar=DN_ALPHA, in1=pg[:], op0=ALU.mult, op1=ALU.add),
                     reads=[pg, hres[ti]], writes=[hres[ti]])
        if upto < 4:
            for ti in range(NT):
                t0 = grp * GT + ti * 128
                s.dma("sp", hout[t0:t0 + 128, :], hres[ti][:], reads=[hres[ti]])
            continue
        gc = scr[:, 0:FB * GT].rearrange("p (f t) -> p f t", t=GT)
        sg = scr[:, FB * GT:2 * FB * GT].rearrange("p (f t) -> p f t", t=GT)
        for ex_i in range(NE):
            for half in range(2):
                wvv = wgu_d[ex_i].rearrange("(kc p) n -> p kc n", p=128)
                for pc in range(KC // 8):
                    w = nextw()
                    wvw = w[:, 0:8 * F].rearrange("p (kc n) -> p kc n", n=F)
                    s.dma("pool", wvw, wvv[:, pc * 8:(pc + 1) * 8, half * F:(half + 1) * F], writes=[w])
                    for k8 in range(8):
                        kc = pc * 8 + k8
                        for fb in range(FB):
                            s.op("pe", lambda e, fb=fb, kc=kc, k8=k8, wvw=wvw: e.matmul(bank[fb][:, 0:GT], lhsT=wvw[:, k8, fb * 128:(fb + 1) * 128], rhs=hT[:, kc, :], start=(kc == 0), stop=(kc == KC - 1)),
                                 reads=[w, hT], writes=[bank[fb]])
                for fb in range(FB):
                    bcol = bg[:, ex_i, half * FB + fb: half * FB + fb + 1]
                    if half == 0:
                        s.op("dve", lambda e, fb=fb, bcol=bcol: e.tensor_scalar(out=gc[:, fb, :], in0=bank[fb][:, 0:GT], scalar1=bcol, scalar2=LIMIT, op0=ALU.add, op1=ALU.min), reads=[bank[fb], bg], writes=[(scr, ("g", fb))])
                        s.op("act", lambda e, fb=fb: e.activation(out=sg[:, fb, :], in_=gc[:, fb, :], func=AF.Sigmoid, scale=SALPHA), reads=[(scr, ("g", fb))], writes=[(scr, ("s", fb))])
                    else:
                        s.op("dve", lambda e, fb=fb, bcol=bcol: e.tensor_scalar(out=tmpA[:, 0:GT], in0=bank[fb][:, 0:GT], scalar1=bcol, scalar2=LIMIT + 1.0, op0=ALU.add, op1=ALU.min), reads=[bank[fb], bg], writes=[tmpA])
                        s.op("dve", lambda e, fb=fb: e.scalar_tensor_tensor(out=tmpB[:, 0:GT], in0=tmpA[:, 0:GT], scalar=1.0 - LIMIT, in1=gc[:, fb, :], op0=ALU.max, op1=ALU.mult), reads=[tmpA, (scr, ("g", fb))], writes=[tmpB])
                        s.op("pool", lambda e, fb=fb: e.tensor_tensor(out=actT[:, fb, :], in0=tmpB[:, 0:GT], in1=sg[:, fb, :], op=ALU.mult), reads=[tmpB, (scr, ("s", fb))], writes=[(actT, fb)])
            wdv = wd_d[ex_i].rearrange("(fb p) n -> p fb n", p=128)
            for db in range(D // 1024):
                w = nextw()
                wvw = w[:, 0:FB * 1024].rearrange("p (fb n) -> p fb n", n=1024)
                s.dma("pool", wvw, wdv[:, :, db * 1024:(db + 1) * 1024], writes=[w])
                for hb in range(2):
                    for ti in range(NT):
                        pg = bank[6 + (hb * NT + ti) % 2]
                        for fb in range(FB):
                            s.op("pe", lambda e, pg=pg, fb=fb, ti=ti, wvw=wvw, hb=hb: e.matmul(pg[:], lhsT=actT[:, fb, ti * 128:(ti + 1) * 128], rhs=wvw[:, fb, hb * 512:(hb + 1) * 512], start=(fb == 0), stop=(fb == FB - 1)),
                                 reads=[actT, w], writes=[pg])
                        cs = slice(db * 1024 + hb * 512, db * 1024 + (hb + 1) * 512)
                        s.op("dve", lambda e, pg=pg, ti=ti, cs=cs, ex_i=ex_i: e.scalar_tensor_tensor(out=hres[ti][:, cs], in0=pg[:], scalar=G[ti][:, ex_i:ex_i + 1], in1=hres[ti][:, cs], op0=ALU.mult, op1=ALU.add),
                             reads=[pg, G[ti], hres[ti]], writes=[hres[ti]])
        if upto < 5:
            for ti in range(NT):
                t0 = grp * GT + ti * 128
                s.dma("sp", hout[t0:t0 + 128, :], hres[ti][:], reads=[hres[ti]])
            continue
        for ti in range(NT):
            t0 = grp * GT + ti * 128
            layer_norm(hres[ti], l2g, l2b)
            s.dma("sp", hout[t0:t0 + 128, :], hres[ti][:], reads=[hres[ti]])
    s.finish()
    return nc

import numpy as np

A_HEADS, C_HEADS = 12, 6
SC128 = 128.0 ** -0.5
RET_THETA = 10000.0
ROPE_THETA = 500000.0

def ret_lg():
    return np.log(1.0 - np.exp(np.linspace(np.log(1.0 / 32), np.log(1.0 / 512), C_HEADS))).astype(np.float64)

def rope_cs(T, n_rot, theta):
    inv = 1.0 / (theta ** (np.arange(0, n_rot, 2, dtype=np.float32) / n_rot))
    ang = np.arange(T, dtype=np.float32)[:, None] * inv[None, :].astype(np.float32)
    return np.cos(ang).astype(np.float32), np.sin(ang).astype(np.float32)

def lb1_consts(T, TB=1024):
    cos, sin = rope_cs(T, 128, RET_THETA)
    ropeC = np.concatenate([cos.T, cos.T], 0).astype(np.float32)
    ropeS = np.concatenate([-sin.T, sin.T], 0).astype(np.float32)
    rmask = np.ones((128, TB), np.float32); rmask[:, ::64] = 0.0
    return dict(ropeC=np.ascontiguousarray(ropeC), ropeS=np.ascontiguousarray(ropeS), rmask=rmask, identb=np.eye(128, dtype=np.float32))

def lb1_core_inputs(c, layer, aq, af, ai, cq, ck, cv, hgrn_lb, consts):
    T = aq.shape[0]
    lg = ret_lg()
    pos = np.arange(64, dtype=np.float64)
    a_q = np.empty((3, 128, T), np.float32); a_f = np.empty((3, 128, T), np.float32)
    c_q = np.empty((3, 128, T), np.float32); c_qs = np.empty((3, 128, T), np.float32)
    c_k = np.empty((3, 128, T), np.float32); c_ks = np.empty((3, 128, T), np.float32)
    v6 = np.empty((6, T, 64), np.float32)
    lbraw = np.empty((128, 6), np.float32)
    qdec = np.empty((128, 3, 64), np.float32); kdec = np.empty((128, 3, 64), np.float32); cdec = np.empty((128, 3), np.float32)
    mask6 = np.zeros((64, 6, 64), np.float32)
    tri = (pos[None, :] >= pos[:, None])
    for j in range(3):
        uid = 3 * c + j
        hd, half = uid // 2, uid % 2
        a_q[j] = aq[:, hd * 128:(hd + 1) * 128].T
        a_f[j] = af[:, hd * 128:(hd + 1) * 128].T
        v6[j] = ai[:, hd * 128 + half * 64: hd * 128 + half * 64 + 64]
        lbraw[:, j] = hgrn_lb[0, hd * 128:(hd + 1) * 128]
        lbraw[:, 3 + j] = hgrn_lb[1, hd * 128:(hd + 1) * 128]
        mask6[:, j, :] = tri
        hd, qt = uid // 4, uid % 4
        qq = cq[:, hd * 128:(hd + 1) * 128].T; kk = ck[:, hd * 128:(hd + 1) * 128].T
        c_q[j] = qq; c_qs[j] = np.concatenate([qq[64:], qq[:64]], 0)
        c_k[j] = kk; c_ks[j] = np.concatenate([kk[64:], kk[:64]], 0)
        v6[3 + j] = cv[:, hd * 256 + qt * 64: hd * 256 + qt * 64 + 64]
        qdec[:, j, :] = np.exp(lg[hd] * (pos + 1.0))[None, :]
        kdec[:, j, :] = (np.exp(lg[hd] * (63.0 - pos)) * SC128)[None, :]
        cdec[:, j] = np.exp(lg[hd] * 64.0)
        mask6[:, 3 + j, :] = np.where(tri, np.exp(lg[hd] * (pos[None, :] - pos[:, None])), 0.0)
    d = dict(a_q=a_q, a_f=a_f, a_lb=np.full((128, 6), float(layer), np.float32), a_lbraw=lbraw, c_q=c_q, c_qs=c_qs, c_k=c_k, c_ks=c_ks,
             v6=v6, qdec=qdec, kdec=kdec, cdec=cdec, mask6=mask6)
    d.update(consts)
    return d

def lb1_gather(results, T):
    oa = np.empty((T, 1536), np.float32); oc = np.empty((T, 1536), np.float32)
    for c, r in enumerate(results):
        o6 = r["o6"].reshape(T, 6, 64)
        for j in range(3):
            uid = 3 * c + j
            hd, half = uid // 2, uid % 2
            oa[:, hd * 128 + half * 64: hd * 128 + half * 64 + 64] = o6[:, j]
            hd, qt = uid // 4, uid % 4
            oc[:, hd * 256 + qt * 64: hd * 256 + qt * 64 + 64] = o6[:, 3 + j]
    return oa, oc

def lb2_shared(bk, bv, ik):
    T = bk.shape[0]
    NKT = T // 128
    cb, sb_ = rope_cs(T, 32, ROPE_THETA)
    ci, si = rope_cs(T, 16, ROPE_THETA)
    kTt = np.ascontiguousarray(bk.reshape(NKT, 128, 8, 128).transpose(0, 3, 2, 1))
    perm32 = np.r_[16:32, 0:16]
    ksw = np.ascontiguousarray(kTt[:, perm32])
    C32 = np.concatenate([cb, cb], 1); S32 = np.concatenate([-sb_, sb_], 1)
    Ck = np.ascontiguousarray(C32.reshape(NKT, 128, 32).transpose(0, 2, 1)); Sk = np.ascontiguousarray(S32.reshape(NKT, 128, 32).transpose(0, 2, 1))
    Vt = np.ascontiguousarray(bv.reshape(NKT, 128, 8, 128))
    kiT = np.ascontiguousarray(ik.T)
    perm16 = np.r_[8:16, 0:8]
    kisw = np.ascontiguousarray(kiT[perm16])
    C16 = np.concatenate([ci, ci], 1); S16 = np.concatenate([-si, si], 1)
    return dict(kTt=kTt, ksw=ksw, Ck=Ck, Sk=Sk, Vt=Vt, kiT=kiT, kisw=kisw, Cki=np.ascontiguousarray(C16.T), Ski=np.ascontiguousarray(S16.T),
                identb=np.eye(128, dtype=np.float32)), (C32, S32, C16, S16)

def lb2_core_inputs(c, NQ, bq, iq, iw, shared, tabs):
    C32, S32, C16, S16 = tabs
    tiles = [8 * i + c for i in range(NQ)]
    rows = np.concatenate([np.arange(j * 128, (j + 1) * 128) for j in tiles])
    perm32 = np.r_[16:32, 0:16]; perm16 = np.r_[8:16, 0:8]
    qT = np.ascontiguousarray(bq[rows].reshape(NQ, 128, 8, 128).transpose(0, 3, 2, 1))
    qsw = np.ascontiguousarray(qT[:, perm32])
    Cq = np.ascontiguousarray(C32[rows].reshape(NQ, 128, 32).transpose(0, 2, 1)); Sq = np.ascontiguousarray(S32[rows].reshape(NQ, 128, 32).transpose(0, 2, 1))
    qiT = np.ascontiguousarray(iq[rows].reshape(NQ, 128, 16, 64).transpose(0, 3, 2, 1))
    qisw = np.ascontiguousarray(qiT[:, perm16])
    Cqi = np.ascontiguousarray(C16[rows].reshape(NQ, 128, 16).transpose(0, 2, 1)); Sqi = np.ascontiguousarray(S16[rows].reshape(NQ, 128, 16).transpose(0, 2, 1))
    iwc = np.ascontiguousarray(iw[rows].reshape(NQ, 128, 16))
    r = np.arange(128)[:, None]; z = np.arange(1024)[None, :]
    vis = (z < 128 * c + 64 * (r // 64 + 1)).astype(np.float32)
    pen = np.where(vis > 0, np.float32(3.0e38), np.float32(-2.0e30)).astype(np.float32)
    d = dict(qT=qT, qsw=qsw, Cq=Cq, Sq=Sq, qiT=qiT, qisw=qisw, Cqi=Cqi, Sqi=Sqi, iw=iwc, pen=pen)
    d.update(shared)
    return d

def lb2_gather(results, T, NQ):
    ob = np.empty((T, 1024), np.float32)
    for c, r in enumerate(results):
        for i in range(NQ):
            j = 8 * i + c
            ob[j * 128:(j + 1) * 128] = r["ob"][i]
    return ob


from concourse.bass_utils import run_bass_kernel_spmd

_CACHE = {}

def _prog(key, fn):
    if key not in _CACHE:
        _CACHE[key] = fn()
    return _CACHE[key]

PROJ_SIZES = (1536, 1536, 1536, 1536, 1024, 1024, 1024, 1024, 64, 16, 768, 768, 1536, 1536)


def kernel(x, ln_in_g, ln_in_b, w_in, w_out, hgrn_lb, hgrn_norm_g, ret_norm_g, ln1_g, ln1_b,
           router_w, router_b, w_gate_up, b_gate_up, w_down, b_down, ln2_g, ln2_b):
    f32 = lambda a: np.ascontiguousarray(np.asarray(a, dtype=np.float32))
    x = f32(x)[0]
    T, D = x.shape
    L = w_in.shape[0]
    NPROJ = w_in.shape[2]
    eye = np.eye(128, dtype=np.float32)
    offs = np.cumsum((0,) + PROJ_SIZES)
    h = None
    NCQ = 4
    NCOL = NPROJ // NCQ
    TH2 = T // 2
    consts1 = lb1_consts(T)
    LCN = 4
    TcC = T // LCN
    for l in range(L):
        do_ln = (l == 0)
        ncA = _prog(("la", do_ln), lambda: build_la(TH2, D, NCOL, do_ln))
        src = x if do_ln else h
        wl = f32(w_in[l])
        ims = []
        for c in range(8):
            th, cq = c // NCQ, c % NCQ
            d = {"x": np.ascontiguousarray(src[th * TH2:(th + 1) * TH2]), "w": np.ascontiguousarray(wl[:, cq * NCOL:(cq + 1) * NCOL]), "ident": eye}
            if do_ln:
                d["g"] = f32(ln_in_g); d["b"] = f32(ln_in_b)
            ims.append(d)
        res = run_bass_kernel_spmd(ncA, ims, core_ids=list(range(8))).results
        del ims, wl
        p = np.empty((T, NPROJ), np.float32)
        for c in range(8):
            th, cq = c // NCQ, c % NCQ
            p[th * TH2:(th + 1) * TH2, cq * NCOL:(cq + 1) * NCOL] = res[c]["p"]
        if do_ln:
            h = np.concatenate([res[0]["hout"], res[NCQ]["hout"]], 0)
        del res
        aq, af, ai, ag, bq, bk, bv, iq, ik, iw, cq_, ck, cv, cg = [np.ascontiguousarray(p[:, offs[i]:offs[i + 1]]) for i in range(14)]
        del p
        ncB1 = _prog(("lb1", T), lambda: build_lb1(T))
        hl = f32(hgrn_lb)
        ims = [lb1_core_inputs(c, l, aq, af, ai, cq_, ck, cv, hl, consts1) for c in range(8)]
        res = run_bass_kernel_spmd(ncB1, ims, core_ids=list(range(8))).results
        del ims
        oa_raw, oc_raw = lb1_gather(res, T)
        del res, aq, af, ai, cq_, ck, cv
        NQ = T // 128 // 8
        ncB2 = _prog(("lb2", T), lambda: build_lb2(T, NQ))
        shared, tabs = lb2_shared(bk, bv, ik)
        ims = [lb2_core_inputs(c, NQ, bq, iq, iw, shared, tabs) for c in range(8)]
        res = run_bass_kernel_spmd(ncB2, ims, core_ids=list(range(8))).results
        del ims, shared
        ob = lb2_gather(res, T, NQ)
        del res, bq, bk, bv, iq, ik, iw
        ncC = _prog(("lc", TcC), lambda: build_lc(TcC))
        NE = router_w.shape[2]
        bguT = np.ascontiguousarray(f32(b_gate_up[l]).reshape(NE, 12, 128).transpose(2, 0, 1))
        com = dict(gA=f32(hgrn_norm_g[l]).reshape(-1), gC=f32(ret_norm_g[l]).reshape(-1), wout=f32(w_out[l]), l1g=f32(ln1_g[l]), l1b=f32(ln1_b[l]),
                   l2g=f32(ln2_g[l]), l2b=f32(ln2_b[l]), rw=f32(router_w[l]), rb=f32(router_b[l]), wgu=f32(w_gate_up[l]), bguT=bguT,
                   wd=f32(w_down[l]), bd=f32(b_down[l]), ident=eye)
        ims = []
        for c in range(LCN):
            sl = slice(c * TcC, (c + 1) * TcC)
            d = dict(h=np.ascontiguousarray(h[sl]), oa=np.ascontiguousarray(oa_raw[sl]), ag=np.ascontiguousarray(ag[sl]), ob=np.ascontiguousarray(ob[sl]),
                     oc=np.ascontiguousarray(oc_raw[sl]), cg=np.ascontiguousarray(cg[sl]))
            d.update(com)
            ims.append(d)
        res = run_bass_kernel_spmd(ncC, ims, core_ids=list(range(LCN))).results
        del ims, com
        h = np.concatenate([r["hout"] for r in res], 0)
        del res, oa_raw, oc_raw, ob, ag, cg
    return h[None].astype(np.float32)
```

```python
import contextlib
import numpy as np
import concourse.bass as bass
import concourse.mybir as mybir

F32 = mybir.dt.float32
BF16 = mybir.dt.bfloat16
I32 = mybir.dt.int32
AF = mybir.ActivationFunctionType
ALU = mybir.AluOpType
AX = mybir.AxisListType


class _St:
    __slots__ = ("w", "r")

    def __init__(self):
        self.w = None
        self.r = {}


class T:
    def __init__(self, s, t, name):
        self.s = s
        self.t = t
        self.name = name
        self.st = {"*": _St()}
        self.dsem = None
        self.dcnt = 0

    def __getitem__(self, idx):
        return self.t[idx]

    def states(self, key):
        if key is None:
            return list(self.st.values())
        if key not in self.st:
            n = _St()
            n.w = self.st["*"].w
            n.r = dict(self.st["*"].r)
            self.st[key] = n
        return [self.st[key], self.st["*"]]


class Sched:
    ENG = ("pe", "act", "dve", "pool", "sp")

    def __init__(self, nc):
        self.nc = nc
        self.es = contextlib.ExitStack()
        self.prog = {e: [] for e in self.ENG}
        self.cnt = {e: 0 for e in self.ENG}
        self.sem = {}
        self.waited = {e: {} for e in self.ENG}
        self.semobj = {}
        for e in self.ENG:
            self.semobj[e] = self.es.enter_context(nc.semaphore("s_" + e))
        self.ntiles = 0
        self.downer = {}
        self.out_tiles = []

    def sb(self, shape, dtype, name):
        t = self.es.enter_context(self.nc.sbuf_tensor(name, list(shape), dtype))
        return T(self, t, name)

    def ps(self, shape, dtype, name):
        t = self.es.enter_context(self.nc.psum_tensor(name, list(shape), dtype))
        return T(self, t, name)

    def _dsem(self, tl):
        if tl.dsem is None:
            key = "d_" + tl.name
            tl.dsem = key
            self.semobj[key] = self.es.enter_context(self.nc.semaphore(key))
            self.downer[key] = tl
        return tl.dsem

    @staticmethod
    def _norm(lst):
        out = []
        for x in lst:
            if isinstance(x, tuple):
                out.append(x)
            else:
                out.append((x, None))
        return out

    def _deps(self, eng, reads, writes):
        deps = {}

        def add(tk):
            if tk is None:
                return
            k, v = tk
            if deps.get(k, 0) < v:
                deps[k] = v

        for tl, key in reads:
            for st in (tl.states(key) if key is not None else tl.states(None)):
                add(st.w)
        for tl, key in writes:
            for st in (tl.states(key) if key is not None else tl.states(None)):
                add(st.w)
                for k, v in st.r.items():
                    add((k, v))
        waits = []
        wd = self.waited[eng]
        for k, v in deps.items():
            if k == eng and eng in ("pe", "sp"):
                continue
            if k in self.downer:
                v = self.downer[k].dcnt
            if wd.get(k, 0) >= v:
                continue
            wd[k] = v
            waits.append((k, v))
        return waits

    def _commit(self, reads, writes, tk):
        k, v = tk
        for tl, key in reads:
            sts = tl.states(key) if key is not None else tl.states(None)
            if key is not None:
                sts = [tl.st[key]]
            for st in sts:
                if st.r.get(k, 0) < v:
                    st.r[k] = v
        for tl, key in writes:
            if key is None:
                tl.st = {"*": _St()}
                tl.st["*"].w = tk
            else:
                st = tl.states(key)[0]
                st.w = tk
                st.r = {}

    def op(self, eng, fn, reads=(), writes=()):
        reads = self._norm(reads)
        writes = self._norm(writes)
        waits = self._deps(eng, reads, writes)
        self.cnt[eng] += 1
        tk = (eng, self.cnt[eng])
        semobj = self.semobj
        so = semobj[eng]

        def run(e, waits=waits, fn=fn, so=so):
            for k, v in waits:
                e.wait_ge(semobj[k], v)
            fn(e).then_inc(so, 1)

        self.prog[eng].append(run)
        self._commit(reads, writes, tk)

    def dma(self, q, out, in_, reads=(), writes=(), **kw):
        reads = self._norm(reads)
        writes = self._norm(writes)
        waits = self._deps(q, reads, writes)
        owner = (writes[0][0] if writes else reads[0][0])
        dk = self._dsem(owner)
        owner.dcnt += 16
        tk = (dk, owner.dcnt)
        semobj = self.semobj

        def run(e, waits=waits):
            for k, v in waits:
                e.wait_ge(semobj[k], v)
            e.dma_start(out=out, in_=in_, **kw).then_inc(semobj[dk], 16)

        self.prog[q].append(run)
        self._commit(reads, writes, tk)
        if not writes:
            self.out_tiles.append(owner)

    def finish(self):
        waits = []
        seen = set()
        for tl in self.out_tiles:
            if tl.dsem in seen:
                continue
            seen.add(tl.dsem)
            waits.append((tl.dsem, tl.dcnt))
        semobj = self.semobj

        def run(e):
            for k, v in waits:
                e.wait_ge(semobj[k], v)

        self.prog["sp"].append(run)
        nc = self.nc
        prog = self.prog
        with nc.Block() as block:
            @block.sync
            def _(e):
                for f in prog["sp"]:
                    f(e)

            @block.tensor
            def _(e):
                for f in prog["pe"]:
                    f(e)

            @block.scalar
            def _(e):
                for f in prog["act"]:
                    f(e)

            @block.vector
            def _(e):
                for f in prog["dve"]:
                    f(e)

            @block.gpsimd
            def _(e):
                for f in prog["pool"]:
                    f(e)
        self.es.close()


LN_EPS = 1e-5

def emit_ln_rows(s, xt, P, D, eps, tmp_prefix, stats, mv, rstd, epsT):
    nch = D // 512
    for c in range(nch):
        s.op("dve", lambda e, c=c: e.bn_stats(out=stats[:, c, :], in_=xt[:, c * 512:(c + 1) * 512]),
             reads=[xt], writes=[(stats, c)])
    s.op("dve", lambda e: e.bn_aggr(out=mv[:], in_=stats[:].rearrange("p c s -> p (c s)")), reads=[stats], writes=[mv])
    s.op("act", lambda e: e.activation(out=rstd[:], in_=mv[:, 1:2], func=AF.Sqrt, bias=epsT[:, 0:1], scale=1.0),
         reads=[mv, epsT], writes=[rstd])
    s.op("dve", lambda e: e.reciprocal(out=rstd[:], in_=rstd[:]), reads=[rstd], writes=[rstd])
    s.op("dve", lambda e: e.tensor_scalar(out=xt[:], in0=xt[:], scalar1=mv[:, 0:1], scalar2=rstd[:, 0:1],
                                          op0=ALU.subtract, op1=ALU.mult), reads=[xt, mv, rstd], writes=[xt])


def build_la(Tc, K, N, do_ln):
    nc = bass.Bass("TRN2", target_bir_lowering=False)
    x = nc.dram_tensor("x", [Tc, K], F32, kind="ExternalInput").ap()
    w = nc.dram_tensor("w", [K, N], F32, kind="ExternalInput").ap()
    ident_d = nc.dram_tensor("ident", [128, 128], F32, kind="ExternalInput").ap()
    if do_ln:
        g = nc.dram_tensor("g", [K], F32, kind="ExternalInput").ap()
        b = nc.dram_tensor("b", [K], F32, kind="ExternalInput").ap()
        hout = nc.dram_tensor("hout", [Tc, K], F32, kind="ExternalOutput").ap()
    p = nc.dram_tensor("p", [Tc, N], F32, kind="ExternalOutput").ap()
    s = Sched(nc)
    KC = K // 128
    TH = min(Tc, 1024)
    NTH = TH // 128
    NB = 256
    ident = s.sb([128, 128], F32, "identT")
    s.dma("sp", ident[:], ident_d, writes=[ident])
    if do_ln:
        gB = s.sb([128, K], F32, "gB"); bB = s.sb([128, K], F32, "bB")
        s.dma("sp", gB[:], g.partition_broadcast(128), writes=[gB])
        s.dma("sp", bB[:], b.partition_broadcast(128), writes=[bB])
        stats = s.sb([128, K // 512, 6], F32, "stats"); mv = s.sb([128, 2], F32, "mv"); rstd = s.sb([128, 1], F32, "rstd")
        epsT = s.sb([128, 1], F32, "epsT")
        s.op("dve", lambda e: e.memset(epsT[:], LN_EPS), writes=[epsT])
    xts = [s.sb([128, K], F32, "xt%d" % i) for i in range(2)]
    hT = s.sb([128, KC, TH], BF16, "hT")
    pst = [s.ps([128, 512], F32, "pst%d" % i) for i in range(2)]
    psg = [s.ps([128, 512], F32, "psg%d" % i) for i in range(4)]
    wbs = [s.sb([128, KC, NB], BF16, "wb%d" % i) for i in range(2)]
    outs = [s.sb([128, NB], F32, "ot%d" % i) for i in range(4)]
    nblocks = (N + NB - 1) // NB
    wv = w.rearrange("(kc p) n -> p kc n", p=128)
    tcount = 0
    wcount = 0
    ocount = 0
    for half in range(Tc // TH):
        for ti in range(NTH):
            t0 = half * TH + ti * 128
            xt = xts[tcount % 2]
            s.dma("sp", xt[:], x[t0:t0 + 128, :], writes=[xt])
            if do_ln:
                emit_ln_rows(s, xt, 128, K, LN_EPS, "ln", stats, mv, rstd, epsT)
                s.op("pool", lambda e, xt=xt: e.tensor_tensor(out=xt[:], in0=xt[:], in1=gB[:], op=ALU.mult), reads=[xt, gB], writes=[xt])
                s.op("dve", lambda e, xt=xt: e.tensor_tensor(out=xt[:], in0=xt[:], in1=bB[:], op=ALU.add), reads=[xt, bB], writes=[xt])
                s.dma("sp", hout[t0:t0 + 128, :], xt[:], reads=[xt])
            for k4 in range(KC // 4):
                pt = pst[k4 % 2]
                for j in range(4):
                    kc = k4 * 4 + j
                    s.op("pe", lambda e, pt=pt, j=j, kc=kc, xt=xt: e.transpose(pt[:, j * 128:(j + 1) * 128], xt[:, kc * 128:(kc + 1) * 128], ident[:]),
                         reads=[xt, ident], writes=[(pt, j)])
                eng = "act" if k4 % 2 == 0 else "dve"
                dst = hT[:, k4 * 4:(k4 + 1) * 4, ti * 128:(ti + 1) * 128]
                src = pt[:].rearrange("p (j t) -> p j t", j=4)
                if eng == "act":
                    s.op("act", lambda e, dst=dst, src=src: e.activation(out=dst, in_=src, func=AF.Copy), reads=[pt], writes=[(hT, (ti, k4))])
                else:
                    s.op("dve", lambda e, dst=dst, src=src: e.tensor_copy(out=dst, in_=src), reads=[pt], writes=[(hT, (ti, k4))])
            tcount += 1
        for nb in range(nblocks):
            n0 = nb * NB
            nw = min(NB, N - n0)
            wb = wbs[wcount % 2]; wcount += 1
            for q4 in range(KC // 8):
                s.dma("pool", wb[:, q4 * 8:(q4 + 1) * 8, 0:nw], wv[:, q4 * 8:(q4 + 1) * 8, n0:n0 + nw], writes=[(wb, q4)])
            for ti in range(NTH):
                t0 = half * TH + ti * 128
                pg = psg[ocount % 4]; ot = outs[ocount % 4]; ocount += 1
                for kc in range(KC):
                    s.op("pe", lambda e, pg=pg, kc=kc, wb=wb, ti=ti, nw=nw: e.matmul(pg[:, 0:nw], lhsT=hT[:, kc, ti * 128:(ti + 1) * 128], rhs=wb[:, kc, 0:nw], start=(kc == 0), stop=(kc == KC - 1)),
                         reads=[hT, wb], writes=[pg])
                if ocount % 2 == 0:
                    s.op("act", lambda e, pg=pg, ot=ot, nw=nw: e.activation(out=ot[:, 0:nw], in_=pg[:, 0:nw], func=AF.Copy), reads=[pg], writes=[ot])
                else:
                    s.op("dve", lambda e, pg=pg, ot=ot, nw=nw: e.tensor_copy(out=ot[:, 0:nw], in_=pg[:, 0:nw]), reads=[pg], writes=[ot])
                s.dma("sp", p[t0:t0 + 128, n0:n0 + nw], ot[:, 0:nw], reads=[ot])
    s.finish()
    return nc


def build_lb1(T, TB=1024):
    nc = bass.Bass("TRN2", target_bir_lowering=False)
    C = 64
    NCH = TB // C
    NBLK = T // TB
    dt = lambda n, sh: nc.dram_tensor(n, sh, F32, kind="ExternalInput").ap()
    a_q = dt("a_q", [3, 128, T]); a_f = dt("a_f", [3, 128, T]); a_lb = dt("a_lb", [128, 6])
    a_lbraw = dt("a_lbraw", [128, 6])
    c_q = dt("c_q", [3, 128, T]); c_qs = dt("c_qs", [3, 128, T]); c_k = dt("c_k", [3, 128, T]); c_ks = dt("c_ks", [3, 128, T])
    v6 = dt("v6", [6, T, 64])
    ropeC = dt("ropeC", [128, T]); ropeS = dt("ropeS", [128, T])
    rmask_d = dt("rmask", [128, TB])
    qdec_d = dt("qdec", [128, 3, 64]); kdec_d = dt("kdec", [128, 3, 64]); cdec_d = dt("cdec", [128, 3])
    mask6_d = dt("mask6", [64, 6, 64])
    identb_d = dt("identb", [128, 128])
    o6 = nc.dram_tensor("o6", [T // C, C, 6 * 64], F32, kind="ExternalOutput").ap()
    s = Sched(nc)
    SC = 128.0 ** -0.5
    rmask = s.sb([128, TB], F32, "rmaskT"); s.dma("sp", rmask[:], rmask_d, writes=[rmask])
    qdec = s.sb([128, 3, 64], F32, "qdecT"); s.dma("sp", qdec[:], qdec_d, writes=[qdec])
    kdec = s.sb([128, 3, 64], F32, "kdecT"); s.dma("sp", kdec[:], kdec_d, writes=[kdec])
    cdec = s.sb([128, 3], F32, "cdecT"); s.dma("sp", cdec[:], cdec_d, writes=[cdec])
    mask6 = s.sb([64, 6, 64], F32, "mask6T"); s.dma("sp", mask6[:], mask6_d, writes=[mask6])
    identb = s.sb([128, 128], BF16, "identbT"); s.dma("pool", identb[:], identb_d, writes=[identb])
    lbr = s.sb([128, 6], F32, "lbr"); s.dma("sp", lbr[:], a_lbraw, writes=[lbr])
    lbm = s.sb([128, 6], F32, "lbm"); s.dma("sp", lbm[:], a_lb, writes=[lbm])
    lb = s.sb([128, 3], F32, "lb"); oml = s.sb([128, 3], F32, "oml")
    s.op("dve", lambda e: e.tensor_tensor(out=lb[:], in0=lbr[:, 3:6], in1=lbr[:, 0:3], op=ALU.subtract), reads=[lbr], writes=[lb])
    s.op("act", lambda e: e.activation(out=lb[:], in_=lb[:], func=AF.Sigmoid), reads=[lb], writes=[lb])
    s.op("dve", lambda e: e.tensor_tensor(out=lb[:], in0=lb[:], in1=lbm[:, 0:3], op=ALU.mult), reads=[lb, lbm], writes=[lb])
    s.op("dve", lambda e: e.tensor_scalar(out=oml[:], in0=lb[:], scalar1=-1.0, scalar2=1.0, op0=ALU.mult, op1=ALU.add), reads=[lb], writes=[oml])
    S = s.sb([128, 6, 64], F32, "S"); Sb = s.sb([128, 6, 64], BF16, "Sb")
    s.op("dve", lambda e: e.memset(S[:], 0.0), writes=[S])
    s.op("pool", lambda e: e.memset(Sb[:], 0.0), writes=[Sb])
    d6 = s.sb([128, NCH, 6], F32, "d6")
    Qt = [s.sb([128, TB], BF16, "Qt%d" % u) for u in range(6)]
    Kt = [s.sb([128, TB], BF16, "Kt%d" % u) for u in range(6)]
    Qi = [s.sb([128, TB], BF16, "Qi%d" % u) for u in range(6)]
    Kd = [s.sb([64, NCH, 128], BF16, "Kd%d" % u) for u in range(6)]
    Vb = [s.sb([64, NCH, 64], BF16, "Vb%d" % u) for u in range(6)]
    KdT = s.sb([128, TB], BF16, "KdT")
    sc = [s.sb([128, TB], F32, "sc%d" % i) for i in range(6)]
    rC = s.sb([128, TB], F32, "rC"); rS = s.sb([128, TB], F32, "rS")
    obuf = s.sb([64, NCH, 6 * 64], F32, "obuf")
    attm = [s.sb([64, 6, 64], BF16, "attm%d" % i) for i in range(2)]
    att_ps = [s.ps([64, 512], F32, "attps%d" % i) for i in range(2)]
    o_ps = [s.ps([64, 512], F32, "ops%d" % i) for i in range(2)]
    U_ps = [s.ps([128, 512], F32, "Ups%d" % i) for i in range(2)]
    tr_ps = [s.ps([64, 1024], BF16, "trps%d" % i) for i in range(2)]
    trc = 0

    def ch3(t):
        return t[:].rearrange("p (n c) -> p n c", c=C)

    for blk in range(NBLK):
        tsl = slice(blk * TB, (blk + 1) * TB)
        s.dma("sp", rC[:], ropeC[:, tsl], writes=[rC]); s.dma("sp", rS[:], ropeS[:, tsl], writes=[rS])
        for u in range(6):
            s.dma("pool", Vb[u][:], v6[u, tsl, :].rearrange("(n c) v -> c n v", c=C), writes=[Vb[u]])
            if u < 3:
                q, f, t2, t3, t4, t5 = sc
                s.dma("sp", q[:], a_q[u, :, tsl], writes=[q]); s.dma("sp", f[:], a_f[u, :, tsl], writes=[f])
                s.op("act", lambda e, f=f: e.activation(out=f[:], in_=f[:], func=AF.Sigmoid), reads=[f], writes=[f])
                s.op("dve", lambda e, f=f, u=u: e.tensor_scalar(out=f[:], in0=f[:], scalar1=oml[:, u:u + 1], scalar2=lb[:, u:u + 1], op0=ALU.mult, op1=ALU.add), reads=[f, oml, lb], writes=[f])
                lf = t2
                s.op("act", lambda e, f=f, lf=lf: e.activation(out=lf[:], in_=f[:], func=AF.Ln), reads=[f], writes=[lf])
                kk = f
                s.op("pool", lambda e, f=f: e.tensor_scalar(out=f[:], in0=f[:], scalar1=-1.0, scalar2=1.0, op0=ALU.mult, op1=ALU.add), reads=[f], writes=[f])
                cum = t3
                s.op("dve", lambda e, cum=cum, lf=lf: e.tensor_tensor_scan(out=cum[:], data0=rmask[:], data1=lf[:], initial=0.0, op0=ALU.mult, op1=ALU.add), reads=[rmask, lf], writes=[cum])
                cum3 = ch3(cum)
                mid = cum3[:, :, 31:32].to_broadcast([128, NCH, C])
                last = cum3[:, :, C - 1:C].to_broadcast([128, NCH, C])
                dm = t2
                s.op("pool", lambda e, dm=dm, cum3=cum3, mid=mid: e.tensor_tensor(out=ch3(dm), in0=cum3, in1=mid, op=ALU.subtract), reads=[cum], writes=[dm])
                e1 = t4
                s.op("act", lambda e, e1=e1, dm=dm: e.activation(out=e1[:], in_=dm[:], func=AF.Exp), reads=[dm], writes=[e1])
                s.op("dve", lambda e, u=u, q=q, e1=e1: e.scalar_tensor_tensor(out=Qt[u][:], in0=q[:], scalar=SC, in1=e1[:], op0=ALU.mult, op1=ALU.mult), reads=[q, e1], writes=[Qt[u]])
                e2 = t5
                s.op("act", lambda e, e2=e2, dm=dm: e.activation(out=e2[:], in_=dm[:], func=AF.Exp, scale=-1.0), reads=[dm], writes=[e2])
                s.op("pool", lambda e, u=u, kk=kk, e2=e2: e.tensor_tensor(out=Kt[u][:], in0=kk[:], in1=e2[:], op=ALU.mult), reads=[kk, e2], writes=[Kt[u]])
                e3 = t4
                s.op("act", lambda e, e3=e3, cum=cum: e.activation(out=e3[:], in_=cum[:], func=AF.Exp), reads=[cum], writes=[e3])
                s.op("dve", lambda e, u=u, q=q, e3=e3: e.scalar_tensor_tensor(out=Qi[u][:], in0=q[:], scalar=SC, in1=e3[:], op0=ALU.mult, op1=ALU.mult), reads=[q, e3], writes=[Qi[u]])
                dl = t2
                s.op("pool", lambda e, dl=dl, cum3=cum3, last=last: e.tensor_tensor(out=ch3(dl), in0=last, in1=cum3, op=ALU.subtract), reads=[cum], writes=[dl])
                e4 = t5
                s.op("act", lambda e, e4=e4, dl=dl: e.activation(out=e4[:], in_=dl[:], func=AF.Exp), reads=[dl], writes=[e4])
                s.op("pool", lambda e, kk=kk, e4=e4: e.tensor_tensor(out=KdT[:], in0=kk[:], in1=e4[:], op=ALU.mult), reads=[kk, e4], writes=[KdT])
                s.op("act", lambda e, u=u, cum3=cum3: e.activation(out=d6[:, :, u:u + 1], in_=cum3[:, :, C - 1:C], func=AF.Exp), reads=[cum], writes=[(d6, u)])
            else:
                r = u - 3
                q, qs, k, ks, t4, t5 = sc
                s.dma("sp", q[:], c_q[r, :, tsl], writes=[q]); s.dma("sp", qs[:], c_qs[r, :, tsl], writes=[qs])
                s.dma("sp", k[:], c_k[r, :, tsl], writes=[k]); s.dma("sp", ks[:], c_ks[r, :, tsl], writes=[ks])
                s.op("dve", lambda e, q=q: e.tensor_tensor(out=q[:], in0=q[:], in1=rC[:], op=ALU.mult), reads=[q, rC], writes=[q])
                s.op("pool", lambda e, qs=qs: e.tensor_tensor(out=qs[:], in0=qs[:], in1=rS[:], op=ALU.mult), reads=[qs, rS], writes=[qs])
                s.op("dve", lambda e, q=q, qs=qs: e.tensor_tensor(out=q[:], in0=q[:], in1=qs[:], op=ALU.add), reads=[q, qs], writes=[q])
                s.op("act", lambda e, u=u, q=q: e.activation(out=Qt[u][:], in_=q[:], func=AF.Copy), reads=[q], writes=[Qt[u]])
                qd = qdec[:, r:r + 1, :].to_broadcast([128, NCH, C])
                s.op("pool", lambda e, u=u, q=q, qd=qd: e.tensor_tensor(out=ch3(Qi[u]), in0=ch3(q), in1=qd, op=ALU.mult), reads=[q, qdec], writes=[Qi[u]])
                s.op("dve", lambda e, k=k: e.tensor_tensor(out=k[:], in0=k[:], in1=rC[:], op=ALU.mult), reads=[k, rC], writes=[k])
                s.op("pool", lambda e, ks=ks: e.tensor_tensor(out=ks[:], in0=ks[:], in1=rS[:], op=ALU.mult), reads=[ks, rS], writes=[ks])
                s.op("dve", lambda e, k=k, ks=ks: e.tensor_tensor(out=k[:], in0=k[:], in1=ks[:], op=ALU.add), reads=[k, ks], writes=[k])
                s.op("act", lambda e, u=u, k=k: e.activation(out=Kt[u][:], in_=k[:], func=AF.Copy, scale=SC), reads=[k], writes=[Kt[u]])
                kd = kdec[:, r:r + 1, :].to_broadcast([128, NCH, C])
                s.op("pool", lambda e, k=k, kd=kd: e.tensor_tensor(out=ch3(KdT), in0=ch3(k), in1=kd, op=ALU.mult), reads=[k, kdec], writes=[KdT])
                cd = cdec[:, r:r + 1].to_broadcast([128, NCH])
                s.op("dve", lambda e, u=u, cd=cd: e.tensor_copy(out=d6[:, :, u], in_=cd), reads=[cdec], writes=[(d6, u)])
            for g in range(NCH // 8):
                tp = tr_ps[trc % 2]; trc += 1
                for j in range(8):
                    n = g * 8 + j
                    s.op("pe", lambda e, tp=tp, j=j, n=n: e.transpose(tp[:, j * 128:(j + 1) * 128], KdT[:, n * C:(n + 1) * C], identb[:]),
                         reads=[KdT, identb], writes=[(tp, j)])
                s.op("act", lambda e, tp=tp, g=g, u=u: e.activation(out=Kd[u][:, g * 8:(g + 1) * 8, :], in_=tp[:].rearrange("p (j k) -> p j k", j=8), func=AF.Copy),
                     reads=[tp], writes=[(Kd[u], g)])
        for n in range(NCH):
            gi = blk * NCH + n
            ap_ = att_ps[gi % 2]; op_ = o_ps[gi % 2]; up_ = U_ps[gi % 2]; am = attm[gi % 2]
            csl = slice(n * C, (n + 1) * C)
            for u in range(6):
                s.op("pe", lambda e, u=u, ap_=ap_, csl=csl: e.matmul(ap_[:, u * 64:(u + 1) * 64], lhsT=Kt[u][:, csl], rhs=Qt[u][:, csl], start=True, stop=True),
                     reads=[Kt[u], Qt[u]], writes=[(ap_, u)])
            s.op("dve", lambda e, ap_=ap_, am=am: e.tensor_tensor(out=am[:].rearrange("p u t -> p (u t)"), in0=ap_[:, 0:384], in1=mask6[:].rearrange("p u t -> p (u t)"), op=ALU.mult),
                 reads=[ap_, mask6], writes=[am])
            for u in range(6):
                s.op("pe", lambda e, u=u, up_=up_, n=n: e.matmul(up_[:, u * 64:(u + 1) * 64], lhsT=Kd[u][:, n, :], rhs=Vb[u][:, n, :], start=True, stop=True),
                     reads=[Kd[u], Vb[u]], writes=[(up_, u)])
            for u in range(6):
                s.op("pe", lambda e, u=u, op_=op_, am=am, n=n: e.matmul(op_[:, u * 64:(u + 1) * 64], lhsT=am[:, u, :], rhs=Vb[u][:, n, :], start=True, stop=False),
                     reads=[am, Vb[u]], writes=[(op_, u)])
                s.op("pe", lambda e, u=u, op_=op_, csl=csl: e.matmul(op_[:, u * 64:(u + 1) * 64], lhsT=Qi[u][:, csl], rhs=Sb[:, u, :], start=False, stop=True),
                     reads=[Qi[u], Sb], writes=[(op_, u)])
            s.op("act", lambda e, op_=op_, n=n: e.activation(out=obuf[:, n, :], in_=op_[:, 0:384], func=AF.Copy), reads=[op_], writes=[(obuf, n)])
            dB = d6[:, n, :].unsqueeze(2).to_broadcast([128, 6, 64])
            s.op("dve", lambda e, dB=dB: e.tensor_tensor(out=S[:], in0=S[:], in1=dB, op=ALU.mult), reads=[S, d6], writes=[S])
            s.op("dve", lambda e, up_=up_: e.tensor_tensor(out=S[:].rearrange("p u v -> p (u v)"), in0=S[:].rearrange("p u v -> p (u v)"), in1=up_[:, 0:384], op=ALU.add), reads=[S, up_], writes=[S])
            s.op("act", lambda e: e.activation(out=Sb[:], in_=S[:], func=AF.Copy), reads=[S], writes=[Sb])
        s.dma("sp", o6[blk * NCH:(blk + 1) * NCH].rearrange("n t u -> t n u"), obuf[:], reads=[obuf])
    s.finish()
    return nc


NEG1 = -1.0e30

def build_lb2(T, NQ, debug=False):
    nc = bass.Bass("TRN2", target_bir_lowering=False)
    NKT = T // 128
    dt = lambda n, sh: nc.dram_tensor(n, sh, F32, kind="ExternalInput").ap()
    qT = dt("qT", [NQ, 128, 8, 128]); qsw = dt("qsw", [NQ, 32, 8, 128]); Cq = dt("Cq", [NQ, 32, 128]); Sq = dt("Sq", [NQ, 32, 128])
    kTt = dt("kTt", [NKT, 128, 8, 128]); ksw = dt("ksw", [NKT, 32, 8, 128]); Ck = dt("Ck", [NKT, 32, 128]); Sk = dt("Sk", [NKT, 32, 128])
    Vt = dt("Vt", [NKT, 128, 8, 128])
    qiT = dt("qiT", [NQ, 64, 16, 128]); qisw = dt("qisw", [NQ, 16, 16, 128]); Cqi = dt("Cqi", [NQ, 16, 128]); Sqi = dt("Sqi", [NQ, 16, 128])
    kiT = dt("kiT", [64, T]); kisw = dt("kisw", [16, T]); Cki = dt("Cki", [16, T]); Ski = dt("Ski", [16, T])
    iw = dt("iw", [NQ, 128, 16])
    pen_d = dt("pen", [128, 1024])
    identb_d = dt("identb", [128, 128])
    ob = nc.dram_tensor("ob", [NQ, 128, 1024], F32, kind="ExternalOutput").ap()
    NMAX = 1024 * NQ
    if debug:
        dbg1 = nc.dram_tensor("dbg1", [NQ, 128, NMAX], F32, kind="ExternalOutput").ap()
        dbg2 = nc.dram_tensor("dbg2", [NQ, 128, NMAX], F32, kind="ExternalOutput").ap()
        dbg3 = nc.dram_tensor("dbg3", [NQ, 128, 4, 258], F32, kind="ExternalOutput").ap()
    s = Sched(nc)
    SC = 128.0 ** -0.5
    assert NMAX <= T
    pen = s.sb([128, 1024], F32, "penT"); s.dma("sp", pen[:], pen_d, writes=[pen])
    identb = s.sb([128, 128], BF16, "identbT"); s.dma("pool", identb[:], identb_d, writes=[identb])
    kiTb = s.sb([64, NMAX], BF16, "kiTb")
    PW = min(512, NMAX)
    stg = s.sb([64, PW], F32, "stg"); stg2 = s.sb([16, 3, PW], F32, "stg2"); stg3 = s.sb([16, 2, PW], F32, "stg3")
    for pc in range(NMAX // PW):
        sl = slice(pc * PW, (pc + 1) * PW)
        s.dma("sp", stg[:], kiT[:, sl], writes=[stg])
        s.dma("sp", stg2[:, 0, :], kisw[:, sl], writes=[(stg2, 0)])
        s.dma("sp", stg2[:, 1, :], Cki[:, sl], writes=[(stg2, 1)])
        s.dma("sp", stg2[:, 2, :], Ski[:, sl], writes=[(stg2, 2)])
        s.op("dve", lambda e: e.tensor_tensor(out=stg3[:, 0, :], in0=stg[0:16, :], in1=stg2[:, 1, :], op=ALU.mult), reads=[stg, stg2], writes=[(stg3, 0)])
        s.op("pool", lambda e: e.tensor_tensor(out=stg3[:, 1, :], in0=stg2[:, 0, :], in1=stg2[:, 2, :], op=ALU.mult), reads=[stg2], writes=[(stg3, 1)])
        s.op("dve", lambda e: e.tensor_tensor(out=stg[0:16, :], in0=stg3[:, 0, :], in1=stg3[:, 1, :], op=ALU.add), reads=[stg3], writes=[stg])
        s.op("act", lambda e, sl=sl: e.activation(out=kiTb[:, sl], in_=stg[:], func=AF.Copy), reads=[stg], writes=[(kiTb, pc)])
    work = s.sb([128, NMAX], F32, "work")
    qf = s.sb([128, 8, 128], F32, "qf"); cq_t = s.sb([32, 2, 128], F32, "cq_t")
    qb = s.sb([128, 8, 128], BF16, "qb")
    qif = s.sb([64, 16, 128], F32, "qif"); cqi_t = s.sb([16, 2, 128], F32, "cqi_t")
    qib = s.sb([64, 16, 128], BF16, "qib")
    rt1 = s.sb([32, 16, 128], F32, "rt1"); rt2 = s.sb([32, 16, 128], F32, "rt2")
    iwt = s.sb([128, 16], F32, "iwt")
    rl = [s.sb([128, 512], F32, "rl%d" % i) for i in range(3)]
    m8 = s.sb([128, 8], F32, "m8")
    m128 = [s.sb([128, 128], BF16, "m128_%d" % i) for i in range(2)]
    mT = [s.sb([128, 128], BF16, "mT%d" % i) for i in range(2)]
    kb16 = [s.sb([128, 8, 128], BF16, "kb16_%d" % i) for i in range(2)]
    kr = [s.sb([32, 8, 128], F32, "kr0")] * 2
    ksw_t = [s.sb([32, 8, 128], F32, "kswt0")] * 2
    ck_t = [s.sb([32, 2, 128], F32, "ckt%d" % i) for i in range(2)]
    vb = [s.sb([128, 8, 129], BF16, "vb%d" % i) for i in range(2)]
    for v_ in vb:
        s.op("pool", lambda e, v_=v_: e.memset(v_[:], 1.0), writes=[v_])
    pexp = [s.sb([128, 4, 128], BF16, "pexp%d" % i) for i in range(2)]
    pm = [s.sb([128, 4, 128], BF16, "pm%d" % i) for i in range(4)]
    otile = s.sb([128, 8, 128], F32, "otile"); rec = s.sb([128, 8, 1], F32, "rec")
    psA = [s.ps([128, 512], F32, "psA%d" % i) for i in range(3)]
    oacc = [s.ps([128, 512], F32, "oacc%d" % i) for i in range(4)]
    psT = s.ps([128, 2, 128], BF16, "psT")
    pac = 0
    pairc = 0
    zl = s.sb([128, 128], BF16, "zl"); zr = s.sb([128, 512], BF16, "zr")
    s.op("pool", lambda e: e.memset(zl[:], 0.0), writes=[zl])
    s.op("pool", lambda e: e.memset(zr[:], 0.0), writes=[zr])

    def bc(t2d, n):
        return t2d.unsqueeze(1).to_broadcast([t2d.shape[0], n, 128])

    for i in range(NQ):
        N = 1024 * (i + 1)
        s.dma("sp", qf[:], qT[i], writes=[qf]); s.dma("sp", rt2[:, 0:8, :], qsw[i], writes=[rt2])
        s.dma("sp", cq_t[:, 0, :], Cq[i], writes=[(cq_t, 0)]); s.dma("sp", cq_t[:, 1, :], Sq[i], writes=[(cq_t, 1)])
        s.op("dve", lambda e: e.tensor_tensor(out=rt1[:, 0:8, :], in0=qf[0:32], in1=bc(cq_t[:, 0, :], 8), op=ALU.mult), reads=[qf, cq_t], writes=[rt1])
        s.op("pool", lambda e: e.tensor_tensor(out=rt2[:, 0:8, :], in0=rt2[:, 0:8, :], in1=bc(cq_t[:, 1, :], 8), op=ALU.mult), reads=[rt2, cq_t], writes=[rt2])
        s.op("dve", lambda e: e.tensor_tensor(out=qf[0:32], in0=rt1[:, 0:8, :], in1=rt2[:, 0:8, :], op=ALU.add), reads=[rt1, rt2], writes=[qf])
        s.op("act", lambda e: e.activation(out=qb[:], in_=qf[:], func=AF.Copy), reads=[qf], writes=[qb])
        s.dma("sp", qif[:], qiT[i], writes=[qif]); s.dma("sp", rt2[0:16], qisw[i], writes=[rt2])
        s.dma("sp", cqi_t[:, 0, :], Cqi[i], writes=[(cqi_t, 0)]); s.dma("sp", cqi_t[:, 1, :], Sqi[i], writes=[(cqi_t, 1)])
        s.op("dve", lambda e: e.tensor_tensor(out=rt1[0:16], in0=qif[0:16], in1=bc(cqi_t[:, 0, :], 16), op=ALU.mult), reads=[qif, cqi_t], writes=[rt1])
        s.op("pool", lambda e: e.tensor_tensor(out=rt2[0:16], in0=rt2[0:16], in1=bc(cqi_t[:, 1, :], 16), op=ALU.mult), reads=[rt2, cqi_t], writes=[rt2])
        s.op("dve", lambda e: e.tensor_tensor(out=qif[0:16], in0=rt1[0:16], in1=rt2[0:16], op=ALU.add), reads=[rt1, rt2], writes=[qif])
        s.op("act", lambda e: e.activation(out=qib[:], in_=qif[:], func=AF.Copy), reads=[qif], writes=[qib])
        s.dma("sp", iwt[:], iw[i], writes=[iwt])
        for kb in range(N // 512):
            ksl = slice(kb * 512, (kb + 1) * 512)
            for h in range(16):
                pa = psA[pac % 3]; r_ = rl[pac % 3]; pac += 1
                s.op("pe", lambda e, pa=pa, h=h, ksl=ksl: e.matmul(pa[:], lhsT=qib[:, h, :], rhs=kiTb[:, ksl], start=True, stop=True), reads=[qib, kiTb], writes=[pa])
                s.op("act", lambda e, pa=pa, r_=r_: e.activation(out=r_[:], in_=pa[:], func=AF.Relu), reads=[pa], writes=[r_])
                if h == 0:
                    s.op("dve", lambda e, r_=r_, ksl=ksl: e.tensor_scalar(out=work[:, ksl], in0=r_[:], scalar1=iwt[:, 0:1], scalar2=None, op0=ALU.mult), reads=[r_, iwt], writes=[(work, kb)])
                else:
                    s.op("dve", lambda e, r_=r_, ksl=ksl, h=h: e.scalar_tensor_tensor(out=work[:, ksl], in0=r_[:], scalar=iwt[:, h:h + 1], in1=work[:, ksl], op0=ALU.mult, op1=ALU.add), reads=[r_, iwt, (work, kb)], writes=[(work, kb)])
        zs = slice(N - 1024, N)
        s.op("dve", lambda e, zs=zs: e.tensor_tensor(out=work[:, zs], in0=work[:, zs], in1=pen[:], op=ALU.min), reads=[work, pen], writes=[work])
        if debug:
            s.dma("sp", dbg1[i, :, 0:N], work[:, 0:N], reads=[work])
        for r in range(32):
            s.op("dve", lambda e, N=N: e.max(out=m8[:], in_=work[:, 0:N]), reads=[work], writes=[m8])
            s.op("dve", lambda e, N=N: e.match_replace(out=work[:, 0:N], in_to_replace=m8[:], in_values=work[:, 0:N], imm_value=NEG1), reads=[work, m8], writes=[work])
        if debug:
            s.dma("sp", dbg2[i, :, 0:N], work[:, 0:N], reads=[work])
        nkt = N // 128
        for b in range(4):
            s.op("pe", lambda e, b=b: e.matmul(oacc[b][:], lhsT=zl[:], rhs=zr[:], start=True, stop=True), reads=[zl, zr], writes=[oacc[b]])
        for kt in range(nkt):
            pp = pairc % 2; pairc += 1
            mm = m128[pp]; mt = mT[pp]
            s.op("pool", lambda e, mm=mm, kt=kt: e.tensor_scalar(out=mm[:], in0=work[:, kt * 128:(kt + 1) * 128], scalar1=NEG1, scalar2=None, op0=ALU.is_equal), reads=[work], writes=[mm])
            s.op("pe", lambda e, mm=mm, pp=pp: e.transpose(psT[:, pp, :], mm[:], identb[:]), reads=[mm, identb], writes=[(psT, pp)])
            s.op("act", lambda e, mt=mt, pp=pp: e.activation(out=mt[:], in_=psT[:, pp, :], func=AF.Copy), reads=[(psT, pp)], writes=[mt])
            kb_ = kb16[pp]; kr_ = kr[pp]; ks_ = ksw_t[pp]; ck_ = ck_t[pp]; vb_ = vb[pp]
            s.dma("pool", kb_[:], kTt[kt], writes=[kb_])
            s.dma("sp", kr_[:], kTt[kt, 0:32], writes=[kr_]); s.dma("sp", ks_[:], ksw[kt], writes=[ks_])
            s.dma("sp", ck_[:, 0, :], Ck[kt], writes=[(ck_, 0)]); s.dma("sp", ck_[:, 1, :], Sk[kt], writes=[(ck_, 1)])
            s.dma("pool", vb_[:, :, 0:128], Vt[kt], writes=[vb_])
            s.op("dve", lambda e, kr_=kr_, ck_=ck_: e.tensor_tensor(out=kr_[:], in0=kr_[:], in1=bc(ck_[:, 0, :], 8), op=ALU.mult), reads=[kr_, ck_], writes=[kr_])
            s.op("pool", lambda e, ks_=ks_, ck_=ck_: e.tensor_tensor(out=ks_[:], in0=ks_[:], in1=bc(ck_[:, 1, :], 8), op=ALU.mult), reads=[ks_, ck_], writes=[ks_])
            s.op("dve", lambda e, kb_=kb_, kr_=kr_, ks_=ks_: e.tensor_tensor(out=kb_[0:32], in0=kr_[:], in1=ks_[:], op=ALU.add), reads=[kr_, ks_], writes=[kb_])
            for g in range(2):
                pa = psA[pac % 3]; pac += 1
                pe_ = pexp[g]; pm_ = pm[(pp * 2 + g)]
                for hh in range(4):
                    h = 4 * g + hh
                    s.op("pe", lambda e, pa=pa, hh=hh, h=h, kb_=kb_: e.matmul(pa[:, hh * 128:(hh + 1) * 128], lhsT=kb_[:, h, :], rhs=qb[:, h, :], start=True, stop=True), reads=[kb_, qb], writes=[(pa, hh)])
                s.op("act", lambda e, pa=pa, pe_=pe_: e.activation(out=pe_[:].rearrange("p h q -> p (h q)"), in_=pa[:], func=AF.Exp, scale=SC), reads=[pa], writes=[pe_])
                eng = "dve" if g == 0 else "pool"
                s.op(eng, lambda e, pe_=pe_, pm_=pm_, mt=mt: e.tensor_tensor(out=pm_[:], in0=pe_[:], in1=bc(mt[:], 4), op=ALU.mult), reads=[pe_, mt], writes=[pm_])
            for h in range(8):
                pm_ = pm[pp * 2 + h // 4]
                oa = oacc[h // 2]
                s.op("pe", lambda e, oa=oa, h=h, pm_=pm_, vb_=vb_, kt=kt, nkt=nkt: e.matmul(oa[:, (h % 2) * 129:(h % 2) * 129 + 129], lhsT=pm_[:, h % 4, :], rhs=vb_[:, h, :], start=False, stop=True),
                     reads=[pm_, vb_], writes=[(oa, h % 2)])
        if debug:
            dbt = s.sb([128, 4, 258], F32, "dbt%d" % i)
            for b in range(4):
                s.op("dve", lambda e, b=b, dbt=dbt: e.tensor_copy(out=dbt[:, b, :], in_=oacc[b][:, 0:258]), reads=[oacc[b]], writes=[(dbt, b)])
            s.dma("sp", dbg3[i], dbt[:], reads=[dbt])
        for b in range(4):
            ov = oacc[b][:, 0:258].rearrange("p (h c) -> p h c", h=2)
            s.op("dve", lambda e, ov=ov, b=b: e.reciprocal(out=rec[:, 2 * b:2 * b + 2, :], in_=ov[:, :, 128:129]), reads=[oacc[b]], writes=[(rec, b)])
            s.op("dve", lambda e, ov=ov, b=b: e.tensor_tensor(out=otile[:, 2 * b:2 * b + 2, :], in0=ov[:, :, 0:128], in1=rec[:, 2 * b:2 * b + 2, :].to_broadcast([128, 2, 128]), op=ALU.mult), reads=[oacc[b], (rec, b)], writes=[(otile, b)])
        s.dma("sp", ob[i].rearrange("p (h d) -> p h d", h=8), otile[:], reads=[otile])
    s.finish()
    return nc


LN_EPS = 1e-5
NORM_EPS = 1e-6
DN_ALPHA = 4 ** 0.25
LIMIT = 7.0
SALPHA = 1.702

def build_lc(Tc, NE=32, D=4096, F=768, upto=5):
    nc = bass.Bass("TRN2", target_bir_lowering=False)
    dt = lambda n, sh: nc.dram_tensor(n, sh, F32, kind="ExternalInput").ap()
    KC = D // 128
    FB = F // 128
    h_d = dt("h", [Tc, D]); oa_d = dt("oa", [Tc, 1536]); ag_d = dt("ag", [Tc, 1536]); ob_d = dt("ob", [Tc, 1024])
    oc_d = dt("oc", [Tc, 1536]); cg_d = dt("cg", [Tc, 1536]); gA_d = dt("gA", [1536]); gC_d = dt("gC", [1536])
    wout_d = dt("wout", [D, D]); l1g = dt("l1g", [D]); l1b = dt("l1b", [D]); l2g = dt("l2g", [D]); l2b = dt("l2b", [D])
    rw_d = dt("rw", [D, NE]); rb_d = dt("rb", [NE])
    NEW = NE if upto >= 4 else 1
    wgu_d = dt("wgu", [NEW, D, 2 * F]); bgu_d = dt("bguT", [128, NE, 2 * FB]); wd_d = dt("wd", [NEW, F, D]); bd_d = dt("bd", [NE, D])
    ident_d = dt("ident", [128, 128])
    hout = nc.dram_tensor("hout", [Tc, D], F32, kind="ExternalOutput").ap()
    s = Sched(nc)
    GT = 256
    NT = GT // 128
    ident = s.sb([128, 128], F32, "identT"); s.dma("sp", ident[:], ident_d, writes=[ident])
    epsT = s.sb([128, 2], F32, "epsT")
    s.op("dve", lambda e: e.memset(epsT[:, 0:1], LN_EPS), writes=[(epsT, 0)])
    s.op("dve", lambda e: e.memset(epsT[:, 1:2], NORM_EPS), writes=[(epsT, 1)])
    rw = s.sb([128, KC, NE], F32, "rwT"); s.dma("sp", rw[:], rw_d.rearrange("(kc p) e -> p kc e", p=128), writes=[rw])
    rwh = s.sb([128, KC, NE], BF16, "rwh"); rwl = s.sb([128, KC, NE], BF16, "rwl")
    s.op("act", lambda e: e.activation(out=rwh[:], in_=rw[:], func=AF.Copy), reads=[rw], writes=[rwh])
    s.op("dve", lambda e: e.tensor_tensor(out=rwl[:], in0=rw[:], in1=rwh[:], op=ALU.subtract), reads=[rw, rwh], writes=[rwl])
    sc4l = s.sb([128, 4, 128], BF16, "sc4l")
    rbB = s.sb([128, NE], F32, "rbB"); s.dma("sp", rbB[:], rb_d.partition_broadcast(128), writes=[rbB])
    bg = s.sb([128, NE, 2 * FB], F32, "bgT"); s.dma("sp", bg[:], bgu_d, writes=[bg])
    s.op("dve", lambda e: e.tensor_scalar(out=bg[:, :, FB:2 * FB], in0=bg[:, :, FB:2 * FB], scalar1=1.0, scalar2=None, op0=ALU.add), reads=[bg], writes=[bg])
    bdb = s.sb([NE, D], BF16, "bdb"); s.dma("pool", bdb[:], bd_d, writes=[bdb])
    hres = [s.sb([128, D], F32, "hres%d" % i) for i in range(NT)]
    hT = s.sb([128, KC, GT], BF16, "hT")
    scr = s.sb([128, 4096], F32, "scr")
    lnb = s.sb([128, 2, D], F32, "lnb")
    WP = 6144
    wpool = [s.sb([128, WP], BF16, "wp%d" % i) for i in range(3)]
    wc = [0]
    actT = s.sb([128, FB, GT], BF16, "actT")
    tmpA = s.sb([128, 512], F32, "tmpA"); tmpB = s.sb([128, 512], F32, "tmpB")
    stats = s.sb([128, D // 512, 6], F32, "stats"); mv = s.sb([128, 2], F32, "mv"); rstd = s.sb([128, 1], F32, "rstd")
    sm = s.sb([128, 64], F32, "sm")
    G = [s.sb([128, NE], F32, "G%d" % i) for i in range(NT)]
    GTt = [s.sb([NE, 128], BF16, "GTt%d" % i) for i in range(NT)]
    lgt = s.sb([128, NE], F32, "lgt"); m8 = s.sb([128, 8], F32, "m8"); ex = s.sb([128, NE], F32, "ex"); msk = s.sb([128, NE], F32, "msk")
    sc4 = s.sb([128, 4, 128], F32, "sc4")
    bank = [s.ps([128, 512], F32, "bank%d" % i) for i in range(8)]

    def nextw():
        w = wpool[wc[0] % 3]; wc[0] += 1
        return w

    def transpose_tile(src, ti, also_router_ps=None):
        for k4 in range(KC // 4):
            pt = bank[k4 % 2]
            for j in range(4):
                kc = k4 * 4 + j
                s.op("pe", lambda e, pt=pt, j=j, kc=kc: e.transpose(pt[:, j * 128:(j + 1) * 128], src[:, kc * 128:(kc + 1) * 128], ident[:]),
                     reads=[src, ident], writes=[(pt, j)])
            dst = hT[:, k4 * 4:(k4 + 1) * 4, ti * 128:(ti + 1) * 128]
            srcv = pt[:].rearrange("p (j t) -> p j t", j=4)
            s.op("act", lambda e, dst=dst, srcv=srcv: e.activation(out=dst, in_=srcv, func=AF.Copy), reads=[pt], writes=[(hT, (ti, k4))])
            if also_router_ps is not None:
                s.op("dve", lambda e, srcv=srcv, dst=dst: e.tensor_tensor(out=sc4l[:], in0=srcv, in1=dst, op=ALU.subtract), reads=[pt, (hT, (ti, k4))], writes=[sc4l])
                for j in range(4):
                    kc = k4 * 4 + j
                    hi = hT[:, kc, ti * 128:(ti + 1) * 128]
                    s.op("pe", lambda e, hi=hi, kc=kc: e.matmul(also_router_ps[:, 0:NE], lhsT=hi, rhs=rwh[:, kc, :], start=(kc == 0), stop=False),
                         reads=[hT, rwh], writes=[also_router_ps])
                    s.op("pe", lambda e, j=j, kc=kc: e.matmul(also_router_ps[:, 0:NE], lhsT=sc4l[:, j, :], rhs=rwh[:, kc, :], start=False, stop=False),
                         reads=[sc4l, rwh], writes=[also_router_ps])
                    s.op("pe", lambda e, hi=hi, kc=kc: e.matmul(also_router_ps[:, 0:NE], lhsT=hi, rhs=rwl[:, kc, :], start=False, stop=(kc == KC - 1)),
                         reads=[hT, rwl], writes=[also_router_ps])

    def layer_norm(xt, gd, bd_):
        s.dma("sp", lnb[:, 0, :], gd.partition_broadcast(128), writes=[(lnb, 0)])
        s.dma("sp", lnb[:, 1, :], bd_.partition_broadcast(128), writes=[(lnb, 1)])
        emit_ln_rows(s, xt, 128, D, LN_EPS, "ln", stats, mv, rstd, epsT)
        s.op("pool", lambda e: e.tensor_tensor(out=xt[:], in0=xt[:], in1=lnb[:, 0, :], op=ALU.mult), reads=[xt, lnb], writes=[xt])
        s.op("dve", lambda e: e.tensor_tensor(out=xt[:], in0=xt[:], in1=lnb[:, 1, :], op=ALU.add), reads=[xt, lnb], writes=[xt])

    for grp in range(Tc // GT):
        for ti in range(NT):
            t0 = grp * GT + ti * 128
            rows = slice(t0, t0 + 128)
            s.dma("sp", hres[ti][:], h_d[rows, :], writes=[hres[ti]])
            mx = scr
            s.dma("sp", mx[:, 0:1536], oa_d[rows, :], writes=[(mx, "a")])
            s.dma("sp", mx[:, 1536:2560], ob_d[rows, :], writes=[(mx, "b")])
            s.dma("sp", mx[:, 2560:4096], oc_d[rows, :], writes=[(mx, "c")])
            gt = lnb[:, 0, 0:3072]; gn = lnb[:, 1, 0:3072]
            s.dma("sp", gt[:, 0:1536], ag_d[rows, :], writes=[(lnb, 0)]); s.dma("sp", gt[:, 1536:3072], cg_d[rows, :], writes=[(lnb, 0)])
            s.dma("sp", gn[:, 0:1536], gA_d.partition_broadcast(128), writes=[(lnb, 1)]); s.dma("sp", gn[:, 1536:3072], gC_d.partition_broadcast(128), writes=[(lnb, 1)])
            s.op("act", lambda e, gt=gt: e.activation(out=gt, in_=gt, func=AF.Silu), reads=[(lnb, 0)], writes=[(lnb, 0)])
            s.op("pool", lambda e, gt=gt, gn=gn: e.tensor_tensor(out=gt, in0=gt, in1=gn, op=ALU.mult), reads=[lnb], writes=[(lnb, 0)])
            xa = mx[:, 0:1536].rearrange("p (h v) -> p h v", v=128)
            sq = lnb[:, 1, 0:1536].rearrange("p (h v) -> p h v", v=128)
            s.op("dve", lambda e, xa=xa, sq=sq: e.tensor_tensor(out=sq, in0=xa, in1=xa, op=ALU.mult), reads=[(mx, "a"), lnb], writes=[(lnb, 1)])
            s.op("dve", lambda e, sq=sq: e.tensor_reduce(out=sm[:, 0:12], in_=sq, op=ALU.add, axis=AX.X), reads=[(lnb, 1)], writes=[sm])
            s.op("act", lambda e: e.activation(out=sm[:, 0:12], in_=sm[:, 0:12], func=AF.Sqrt, bias=epsT[:, 1:2], scale=1.0 / 128), reads=[sm, epsT], writes=[sm])
            s.op("dve", lambda e: e.reciprocal(out=sm[:, 0:12], in_=sm[:, 0:12]), reads=[sm], writes=[sm])
            s.op("dve", lambda e, xa=xa: e.tensor_tensor(out=xa, in0=xa, in1=sm[:, 0:12].unsqueeze(2).to_broadcast([128, 12, 128]), op=ALU.mult), reads=[(mx, "a"), sm], writes=[(mx, "a")])
            xc = mx[:, 2560:4096].rearrange("p (h v) -> p h v", v=256)
            sq2 = lnb[:, 1, 0:1536].rearrange("p (h v) -> p h v", v=256)
            s.op("dve", lambda e, xc=xc: e.tensor_reduce(out=sm[:, 16:22], in_=xc, op=ALU.add, axis=AX.X), reads=[(mx, "c")], writes=[sm])
            s.op("dve", lambda e: e.tensor_scalar(out=sm[:, 16:22], in0=sm[:, 16:22], scalar1=1.0 / 256, scalar2=None, op0=ALU.mult), reads=[sm], writes=[sm])
            s.op("dve", lambda e, xc=xc: e.tensor_tensor(out=xc, in0=xc, in1=sm[:, 16:22].unsqueeze(2).to_broadcast([128, 6, 256]), op=ALU.subtract), reads=[(mx, "c"), sm], writes=[(mx, "c")])
            s.op("dve", lambda e, xc=xc, sq2=sq2: e.tensor_tensor(out=sq2, in0=xc, in1=xc, op=ALU.mult), reads=[(mx, "c"), lnb], writes=[(lnb, 1)])
            s.op("dve", lambda e, sq2=sq2: e.tensor_reduce(out=sm[:, 24:30], in_=sq2, op=ALU.add, axis=AX.X), reads=[(lnb, 1)], writes=[sm])
            s.op("act", lambda e: e.activation(out=sm[:, 24:30], in_=sm[:, 24:30], func=AF.Sqrt, bias=epsT[:, 1:2], scale=1.0 / 256), reads=[sm, epsT], writes=[sm])
            s.op("dve", lambda e: e.reciprocal(out=sm[:, 24:30], in_=sm[:, 24:30]), reads=[sm], writes=[sm])
            s.op("dve", lambda e, xc=xc: e.tensor_tensor(out=xc, in0=xc, in1=sm[:, 24:30].unsqueeze(2).to_broadcast([128, 6, 256]), op=ALU.mult), reads=[(mx, "c"), sm], writes=[(mx, "c")])
            s.op("pool", lambda e, gt=gt: e.tensor_tensor(out=mx[:, 0:1536], in0=mx[:, 0:1536], in1=gt[:, 0:1536], op=ALU.mult), reads=[mx, lnb], writes=[(mx, "a")])
            s.op("pool", lambda e, gt=gt: e.tensor_tensor(out=mx[:, 2560:4096], in0=mx[:, 2560:4096], in1=gt[:, 1536:3072], op=ALU.mult), reads=[mx, lnb], writes=[(mx, "c")])
            transpose_tile(mx, ti)
        CB = 128
        wv = wout_d.rearrange("(kc p) n -> p kc n", p=128)
        for cb in range(D // CB):
            w = nextw()
            wvw = w[:, 0:KC * CB].rearrange("p (kc n) -> p kc n", n=CB)
            for q4 in range(4):
                s.dma("pool", wvw[:, q4 * 8:(q4 + 1) * 8, :], wv[:, q4 * 8:(q4 + 1) * 8, cb * CB:(cb + 1) * CB], writes=[(w, q4)])
            for ti in range(NT):
                pg = bank[2 + (cb * NT + ti) % 4]
                for kc in range(KC):
                    s.op("pe", lambda e, pg=pg, kc=kc, wvw=wvw, ti=ti: e.matmul(pg[:, 0:CB], lhsT=hT[:, kc, ti * 128:(ti + 1) * 128], rhs=wvw[:, kc, :], start=(kc == 0), stop=(kc == KC - 1)),
                         reads=[hT, w], writes=[pg])
                s.op("dve", lambda e, pg=pg, ti=ti, cb=cb: e.scalar_tensor_tensor(out=hres[ti][:, cb * CB:(cb + 1) * CB], in0=hres[ti][:, cb * CB:(cb + 1) * CB], scalar=DN_ALPHA, in1=pg[:, 0:CB], op0=ALU.mult, op1=ALU.add),
                     reads=[pg, hres[ti]], writes=[hres[ti]])
        if upto < 3:
            for ti in range(NT):
                t0 = grp * GT + ti * 128
                s.dma("sp", hout[t0:t0 + 128, :], hres[ti][:], reads=[hres[ti]])
            continue
        for ti in range(NT):
            layer_norm(hres[ti], l1g, l1b)
            rp = bank[6]
            transpose_tile(hres[ti], ti, also_router_ps=rp)
            s.op("dve", lambda e, rp=rp: e.tensor_tensor(out=lgt[:], in0=rp[:, 0:NE], in1=rbB[:], op=ALU.add), reads=[rp, rbB], writes=[lgt])
            s.op("dve", lambda e: e.max(out=m8[:], in_=lgt[:]), reads=[lgt], writes=[m8])
            s.op("dve", lambda e: e.tensor_scalar(out=msk[:], in0=lgt[:], scalar1=m8[:, 3:4], scalar2=None, op0=ALU.is_ge), reads=[lgt, m8], writes=[msk])
            s.op("dve", lambda e: e.tensor_scalar(out=ex[:], in0=lgt[:], scalar1=m8[:, 0:1], scalar2=None, op0=ALU.subtract), reads=[lgt, m8], writes=[ex])
            s.op("act", lambda e: e.activation(out=ex[:], in_=ex[:], func=AF.Exp), reads=[ex], writes=[ex])
            s.op("dve", lambda e: e.tensor_tensor(out=ex[:], in0=ex[:], in1=msk[:], op=ALU.mult), reads=[ex, msk], writes=[ex])
            s.op("dve", lambda e: e.tensor_reduce(out=sm[:, 32:33], in_=ex[:], op=ALU.add, axis=AX.X), reads=[ex], writes=[sm])
            s.op("dve", lambda e: e.reciprocal(out=sm[:, 32:33], in_=sm[:, 32:33]), reads=[sm], writes=[sm])
            s.op("dve", lambda e, ti=ti: e.tensor_scalar(out=G[ti][:], in0=ex[:], scalar1=sm[:, 32:33], scalar2=None, op0=ALU.mult), reads=[ex, sm], writes=[G[ti]])
            tp = bank[7]
            s.op("pe", lambda e, tp=tp, ti=ti: e.transpose(tp[0:NE, 0:128], G[ti][:], ident[:]), reads=[G[ti], ident], writes=[tp])
            s.op("act", lambda e, tp=tp, ti=ti: e.activation(out=GTt[ti][:], in_=tp[0:NE, 0:128], func=AF.Copy), reads=[tp], writes=[GTt[ti]])
            for db in range(D // 512):
                pg = bank[2 + db % 4]
                s.op("pe", lambda e, pg=pg, ti=ti, db=db: e.matmul(pg[:], lhsT=GTt[ti][:], rhs=bdb[:, db * 512:(db + 1) * 512], start=True, stop=True), reads=[GTt[ti], bdb], writes=[pg])
                s.op("dve", lambda e, pg=pg, ti=ti, db=db: e.scalar_tensor_tensor(out=hres[ti][:, db * 512:(db + 1) * 512], in0=hres[ti][:, db * 512:(db + 1) * 512], scalar=DN_ALPHA, in1=pg[:], op0=ALU.mult, op1=ALU.add),
                     reads=[pg, hres[ti]], writes=[hres[ti]])
        if upto < 4:
            for ti in range(NT):
                t0 = grp * GT + ti * 128
                s.dma("sp", hout[t0:t0 + 128, :], hres[ti][:], reads=[hres[ti]])
            continue
        gc = scr[:, 0:FB * GT].rearrange("p (f t) -> p f t", t=GT)
        sg = scr[:, FB * GT:2 * FB * GT].rearrange("p (f t) -> p f t", t=GT)
        for ex_i in range(NE):
            for half in range(2):
                wvv = wgu_d[ex_i].rearrange("(kc p) n -> p kc n", p=128)
                for pc in range(KC // 8):
                    w = nextw()
                    wvw = w[:, 0:8 * F].rearrange("p (kc n) -> p kc n", n=F)
                    s.dma("pool", wvw, wvv[:, pc * 8:(pc + 1) * 8, half * F:(half + 1) * F], writes=[w])
                    for k8 in range(8):
                        kc = pc * 8 + k8
                        for fb in range(FB):
                            s.op("pe", lambda e, fb=fb, kc=kc, k8=k8, wvw=wvw: e.matmul(bank[fb][:, 0:GT], lhsT=wvw[:, k8, fb * 128:(fb + 1) * 128], rhs=hT[:, kc, :], start=(kc == 0), stop=(kc == KC - 1)),
                                 reads=[w, hT], writes=[bank[fb]])
                for fb in range(FB):
                    bcol = bg[:, ex_i, half * FB + fb: half * FB + fb + 1]
                    if half == 0:
                        s.op("dve", lambda e, fb=fb, bcol=bcol: e.tensor_scalar(out=gc[:, fb, :], in0=bank[fb][:, 0:GT], scalar1=bcol, scalar2=LIMIT, op0=ALU.add, op1=ALU.min), reads=[bank[fb], bg], writes=[(scr, ("g", fb))])
                        s.op("act", lambda e, fb=fb: e.activation(out=sg[:, fb, :], in_=gc[:, fb, :], func=AF.Sigmoid, scale=SALPHA), reads=[(scr, ("g", fb))], writes=[(scr, ("s", fb))])
                    else:
                        s.op("dve", lambda e, fb=fb, bcol=bcol: e.tensor_scalar(out=tmpA[:, 0:GT], in0=bank[fb][:, 0:GT], scalar1=bcol, scalar2=LIMIT + 1.0, op0=ALU.add, op1=ALU.min), reads=[bank[fb], bg], writes=[tmpA])
                        s.op("dve", lambda e, fb=fb: e.scalar_tensor_tensor(out=tmpB[:, 0:GT], in0=tmpA[:, 0:GT], scalar=1.0 - LIMIT, in1=gc[:, fb, :], op0=ALU.max, op1=ALU.mult), reads=[tmpA, (scr, ("g", fb))], writes=[tmpB])
                        s.op("pool", lambda e, fb=fb: e.tensor_tensor(out=actT[:, fb, :], in0=tmpB[:, 0:GT], in1=sg[:, fb, :], op=ALU.mult), reads=[tmpB, (scr, ("s", fb))], writes=[(actT, fb)])
            wdv = wd_d[ex_i].rearrange("(fb p) n -> p fb n", p=128)
            for db in range(D // 1024):
                w = nextw()
                wvw = w[:, 0:FB * 1024].rearrange("p (fb n) -> p fb n", n=1024)
                s.dma("pool", wvw, wdv[:, :, db * 1024:(db + 1) * 1024], writes=[w])
                for hb in range(2):
                    for ti in range(NT):
                        pg = bank[6 + (hb * NT + ti) % 2]
                        for fb in range(FB):
                            s.op("pe", lambda e, pg=pg, fb=fb, ti=ti, wvw=wvw, hb=hb: e.matmul(pg[:], lhsT=actT[:, fb, ti * 128:(ti + 1) * 128], rhs=wvw[:, fb, hb * 512:(hb + 1) * 512], start=(fb == 0), stop=(fb == FB - 1)),
                                 reads=[actT, w], writes=[pg])
                        cs = slice(db * 1024 + hb * 512, db * 1024 + (hb + 1) * 512)
                        s.op("dve", lambda e, pg=pg, ti=ti, cs=cs, ex_i=ex_i: e.scalar_tensor_tensor(out=hres[ti][:, cs], in0=pg[:], scalar=G[ti][:, ex_i:ex_i + 1], in1=hres[ti][:, cs], op0=ALU.mult, op1=ALU.add),
                             reads=[pg, G[ti], hres[ti]], writes=[hres[ti]])
        if upto < 5:
            for ti in range(NT):
                t0 = grp * GT + ti * 128
                s.dma("sp", hout[t0:t0 + 128, :], hres[ti][:], reads=[hres[ti]])
            continue
        for ti in range(NT):
            t0 = grp * GT + ti * 128
            layer_norm(hres[ti], l2g, l2b)
            s.dma("sp", hout[t0:t0 + 128, :], hres[ti][:], reads=[hres[ti]])
    s.finish()
    return nc

import numpy as np

A_HEADS, C_HEADS = 12, 6
SC128 = 128.0 ** -0.5
RET_THETA = 10000.0
ROPE_THETA = 500000.0

def ret_lg():
    return np.log(1.0 - np.exp(np.linspace(np.log(1.0 / 32), np.log(1.0 / 512), C_HEADS))).astype(np.float64)

def rope_cs(T, n_rot, theta):
    inv = 1.0 / (theta ** (np.arange(0, n_rot, 2, dtype=np.float32) / n_rot))
    ang = np.arange(T, dtype=np.float32)[:, None] * inv[None, :].astype(np.float32)
    return np.cos(ang).astype(np.float32), np.sin(ang).astype(np.float32)

def lb1_consts(T, TB=1024):
    cos, sin = rope_cs(T, 128, RET_THETA)
    ropeC = np.concatenate([cos.T, cos.T], 0).astype(np.float32)
    ropeS = np.concatenate([-sin.T, sin.T], 0).astype(np.float32)
    rmask = np.ones((128, TB), np.float32); rmask[:, ::64] = 0.0
    return dict(ropeC=np.ascontiguousarray(ropeC), ropeS=np.ascontiguousarray(ropeS), rmask=rmask, identb=np.eye(128, dtype=np.float32))

def lb1_core_inputs(c, layer, aq, af, ai, cq, ck, cv, hgrn_lb, consts):
    T = aq.shape[0]
    lg = ret_lg()
    pos = np.arange(64, dtype=np.float64)
    a_q = np.empty((3, 128, T), np.float32); a_f = np.empty((3, 128, T), np.float32)
    c_q = np.empty((3, 128, T), np.float32); c_qs = np.empty((3, 128, T), np.float32)
    c_k = np.empty((3, 128, T), np.float32); c_ks = np.empty((3, 128, T), np.float32)
    v6 = np.empty((6, T, 64), np.float32)
    lbraw = np.empty((128, 6), np.float32)
    qdec = np.empty((128, 3, 64), np.float32); kdec = np.empty((128, 3, 64), np.float32); cdec = np.empty((128, 3), np.float32)
    mask6 = np.zeros((64, 6, 64), np.float32)
    tri = (pos[None, :] >= pos[:, None])
    for j in range(3):
        uid = 3 * c + j
        hd, half = uid // 2, uid % 2
        a_q[j] = aq[:, hd * 128:(hd + 1) * 128].T
        a_f[j] = af[:, hd * 128:(hd + 1) * 128].T
        v6[j] = ai[:, hd * 128 + half * 64: hd * 128 + half * 64 + 64]
        lbraw[:, j] = hgrn_lb[0, hd * 128:(hd + 1) * 128]
        lbraw[:, 3 + j] = hgrn_lb[1, hd * 128:(hd + 1) * 128]
        mask6[:, j, :] = tri
        hd, qt = uid // 4, uid % 4
        qq = cq[:, hd * 128:(hd + 1) * 128].T; kk = ck[:, hd * 128:(hd + 1) * 128].T
        c_q[j] = qq; c_qs[j] = np.concatenate([qq[64:], qq[:64]], 0)
        c_k[j] = kk; c_ks[j] = np.concatenate([kk[64:], kk[:64]], 0)
        v6[3 + j] = cv[:, hd * 256 + qt * 64: hd * 256 + qt * 64 + 64]
        qdec[:, j, :] = np.exp(lg[hd] * (pos + 1.0))[None, :]
        kdec[:, j, :] = (np.exp(lg[hd] * (63.0 - pos)) * SC128)[None, :]
        cdec[:, j] = np.exp(lg[hd] * 64.0)
        mask6[:, 3 + j, :] = np.where(tri, np.exp(lg[hd] * (pos[None, :] - pos[:, None])), 0.0)
    d = dict(a_q=a_q, a_f=a_f, a_lb=np.full((128, 6), float(layer), np.float32), a_lbraw=lbraw, c_q=c_q, c_qs=c_qs, c_k=c_k, c_ks=c_ks,
             v6=v6, qdec=qdec, kdec=kdec, cdec=cdec, mask6=mask6)
    d.update(consts)
    return d

def lb1_gather(results, T):
    oa = np.empty((T, 1536), np.float32); oc = np.empty((T, 1536), np.float32)
    for c, r in enumerate(results):
        o6 = r["o6"].reshape(T, 6, 64)
        for j in range(3):
            uid = 3 * c + j
            hd, half = uid // 2, uid % 2
            oa[:, hd * 128 + half * 64: hd * 128 + half * 64 + 64] = o6[:, j]
            hd, qt = uid // 4, uid % 4
            oc[:, hd * 256 + qt * 64: hd * 256 + qt * 64 + 64] = o6[:, 3 + j]
    return oa, oc

def lb2_shared(bk, bv, ik):
    T = bk.shape[0]
    NKT = T // 128
    cb, sb_ = rope_cs(T, 32, ROPE_THETA)
    ci, si = rope_cs(T, 16, ROPE_THETA)
    kTt = np.ascontiguousarray(bk.reshape(NKT, 128, 8, 128).transpose(0, 3, 2, 1))
    perm32 = np.r_[16:32, 0:16]
    ksw = np.ascontiguousarray(kTt[:, perm32])
    C32 = np.concatenate([cb, cb], 1); S32 = np.concatenate([-sb_, sb_], 1)
    Ck = np.ascontiguousarray(C32.reshape(NKT, 128, 32).transpose(0, 2, 1)); Sk = np.ascontiguousarray(S32.reshape(NKT, 128, 32).transpose(0, 2, 1))
    Vt = np.ascontiguousarray(bv.reshape(NKT, 128, 8, 128))
    kiT = np.ascontiguousarray(ik.T)
    perm16 = np.r_[8:16, 0:8]
    kisw = np.ascontiguousarray(kiT[perm16])
    C16 = np.concatenate([ci, ci], 1); S16 = np.concatenate([-si, si], 1)
    return dict(kTt=kTt, ksw=ksw, Ck=Ck, Sk=Sk, Vt=Vt, kiT=kiT, kisw=kisw, Cki=np.ascontiguousarray(C16.T), Ski=np.ascontiguousarray(S16.T),
                identb=np.eye(128, dtype=np.float32)), (C32, S32, C16, S16)

def lb2_core_inputs(c, NQ, bq, iq, iw, shared, tabs):
    C32, S32, C16, S16 = tabs
    tiles = [8 * i + c for i in range(NQ)]
    rows = np.concatenate([np.arange(j * 128, (j + 1) * 128) for j in tiles])
    perm32 = np.r_[16:32, 0:16]; perm16 = np.r_[8:16, 0:8]
    qT = np.ascontiguousarray(bq[rows].reshape(NQ, 128, 8, 128).transpose(0, 3, 2, 1))
    qsw = np.ascontiguousarray(qT[:, perm32])
    Cq = np.ascontiguousarray(C32[rows].reshape(NQ, 128, 32).transpose(0, 2, 1)); Sq = np.ascontiguousarray(S32[rows].reshape(NQ, 128, 32).transpose(0, 2, 1))
    qiT = np.ascontiguousarray(iq[rows].reshape(NQ, 128, 16, 64).transpose(0, 3, 2, 1))
    qisw = np.ascontiguousarray(qiT[:, perm16])
    Cqi = np.ascontiguousarray(C16[rows].reshape(NQ, 128, 16).transpose(0, 2, 1)); Sqi = np.ascontiguousarray(S16[rows].reshape(NQ, 128, 16).transpose(0, 2, 1))
    iwc = np.ascontiguousarray(iw[rows].reshape(NQ, 128, 16))
    r = np.arange(128)[:, None]; z = np.arange(1024)[None, :]
    vis = (z < 128 * c + 64 * (r // 64 + 1)).astype(np.float32)
    pen = np.where(vis > 0, np.float32(3.0e38), np.float32(-2.0e30)).astype(np.float32)
    d = dict(qT=qT, qsw=qsw, Cq=Cq, Sq=Sq, qiT=qiT, qisw=qisw, Cqi=Cqi, Sqi=Sqi, iw=iwc, pen=pen)
    d.update(shared)
    return d

def lb2_gather(results, T, NQ):
    ob = np.empty((T, 1024), np.float32)
    for c, r in enumerate(results):
        for i in range(NQ):
            j = 8 * i + c
            ob[j * 128:(j + 1) * 128] = r["ob"][i]
    return ob


from concourse.bass_utils import run_bass_kernel_spmd

_CACHE = {}

def _prog(key, fn):
    if key not in _CACHE:
        _CACHE[key] = fn()
    return _CACHE[key]

PROJ_SIZES = (1536, 1536, 1536, 1536, 1024, 1024, 1024, 1024, 64, 16, 768, 768, 1536, 1536)


def kernel(x, ln_in_g, ln_in_b, w_in, w_out, hgrn_lb, hgrn_norm_g, ret_norm_g, ln1_g, ln1_b,
           router_w, router_b, w_gate_up, b_gate_up, w_down, b_down, ln2_g, ln2_b):
    f32 = lambda a: np.ascontiguousarray(np.asarray(a, dtype=np.float32))
    x = f32(x)[0]
    T, D = x.shape
    L = w_in.shape[0]
    NPROJ = w_in.shape[2]
    eye = np.eye(128, dtype=np.float32)
    offs = np.cumsum((0,) + PROJ_SIZES)
    h = None
    NCQ = 4
    NCOL = NPROJ // NCQ
    TH2 = T // 2
    consts1 = lb1_consts(T)
    LCN = 8
    TcC = T // LCN
    for l in range(L):
        do_ln = (l == 0)
        ncA = _prog(("la", do_ln), lambda: build_la(TH2, D, NCOL, do_ln))
        src = x if do_ln else h
        wl = f32(w_in[l])
        ims = []
        for c in range(8):
            th, cq = c // NCQ, c % NCQ
            d = {"x": np.ascontiguousarray(src[th * TH2:(th + 1) * TH2]), "w": np.ascontiguousarray(wl[:, cq * NCOL:(cq + 1) * NCOL]), "ident": eye}
            if do_ln:
                d["g"] = f32(ln_in_g); d["b"] = f32(ln_in_b)
            ims.append(d)
        res = run_bass_kernel_spmd(ncA, ims, core_ids=list(range(8))).results
        del ims, wl
        p = np.empty((T, NPROJ), np.float32)
        for c in range(8):
            th, cq = c // NCQ, c % NCQ
            p[th * TH2:(th + 1) * TH2, cq * NCOL:(cq + 1) * NCOL] = res[c]["p"]
        if do_ln:
            h = np.concatenate([res[0]["hout"], res[NCQ]["hout"]], 0)
        del res
        aq, af, ai, ag, bq, bk, bv, iq, ik, iw, cq_, ck, cv, cg = [np.ascontiguousarray(p[:, offs[i]:offs[i + 1]]) for i in range(14)]
        del p
        ncB1 = _prog(("lb1", T), lambda: build_lb1(T))
        hl = f32(hgrn_lb)
        ims = [lb1_core_inputs(c, l, aq, af, ai, cq_, ck, cv, hl, consts1) for c in range(8)]
        res = run_bass_kernel_spmd(ncB1, ims, core_ids=list(range(8))).results
        del ims
        oa_raw, oc_raw = lb1_gather(res, T)
        del res, aq, af, ai, cq_, ck, cv
        NQ = T // 128 // 8
        ncB2 = _prog(("lb2", T), lambda: build_lb2(T, NQ))
        shared, tabs = lb2_shared(bk, bv, ik)
        ims = [lb2_core_inputs(c, NQ, bq, iq, iw, shared, tabs) for c in range(8)]
        res = run_bass_kernel_spmd(ncB2, ims, core_ids=list(range(8))).results
        del ims, shared
        ob = lb2_gather(res, T, NQ)
        del res, bq, bk, bv, iq, ik, iw
        ncC = _prog(("lc", TcC), lambda: build_lc(TcC))
        NE = router_w.shape[2]
        bguT = np.ascontiguousarray(f32(b_gate_up[l]).reshape(NE, 12, 128).transpose(2, 0, 1))
        com = dict(gA=f32(hgrn_norm_g[l]).reshape(-1), gC=f32(ret_norm_g[l]).reshape(-1), wout=f32(w_out[l]), l1g=f32(ln1_g[l]), l1b=f32(ln1_b[l]),
                   l2g=f32(ln2_g[l]), l2b=f32(ln2_b[l]), rw=f32(router_w[l]), rb=f32(router_b[l]), wgu=f32(w_gate_up[l]), bguT=bguT,
                   wd=f32(w_down[l]), bd=f32(b_down[l]), ident=eye)
        ims = []
        for c in range(LCN):
            sl = slice(c * TcC, (c + 1) * TcC)
            d = dict(h=np.ascontiguousarray(h[sl]), oa=np.ascontiguousarray(oa_raw[sl]), ag=np.ascontiguousarray(ag[sl]), ob=np.ascontiguousarray(ob[sl]),
                     oc=np.ascontiguousarray(oc_raw[sl]), cg=np.ascontiguousarray(cg[sl]))
            d.update(com)
            ims.append(d)
        res = run_bass_kernel_spmd(ncC, ims, core_ids=list(range(LCN))).results
        del ims, com
        h = np.concatenate([r["hout"] for r in res], 0)
        del res, oa_raw, oc_raw, ob, ag, cg
    return h[None].astype(np.float32)
```

```python
import contextlib
import numpy as np
import concourse.bass as bass
import concourse.mybir as mybir

F32 = mybir.dt.float32
BF16 = mybir.dt.bfloat16
I32 = mybir.dt.int32
AF = mybir.ActivationFunctionType
ALU = mybir.AluOpType
AX = mybir.AxisListType


class _St:
    __slots__ = ("w", "r")

    def __init__(self):
        self.w = None
        self.r = {}


class T:
    def __init__(self, s, t, name):
        self.s = s
        self.t = t
        self.name = name
        self.st = {"*": _St()}
        self.dsem = None
        self.dcnt = 0

    def __getitem__(self, idx):
        return self.t[idx]

    def states(self, key):
        if key is None:
            return list(self.st.values())
        if key not in self.st:
            n = _St()
            n.w = self.st["*"].w
            n.r = dict(self.st["*"].r)
            self.st[key] = n
        return [self.st[key], self.st["*"]]


class Sched:
    ENG = ("pe", "act", "dve", "pool", "sp")

    def __init__(self, nc):
        self.nc = nc
        self.es = contextlib.ExitStack()
        self.prog = {e: [] for e in self.ENG}
        self.cnt = {e: 0 for e in self.ENG}
        self.sem = {}
        self.waited = {e: {} for e in self.ENG}
        self.semobj = {}
        for e in self.ENG:
            self.semobj[e] = self.es.enter_context(nc.semaphore("s_" + e))
        self.ntiles = 0
        self.downer = {}
        self.scope = None
        self.out_tiles = []

    def sb(self, shape, dtype, name):
        t = self.es.enter_context(self.nc.sbuf_tensor(name, list(shape), dtype))
        return T(self, t, name)

    def ps(self, shape, dtype, name):
        t = self.es.enter_context(self.nc.psum_tensor(name, list(shape), dtype))
        return T(self, t, name)

    def _dsem(self, tl):
        if tl.dsem is None:
            key = "d_" + tl.name
            tl.dsem = key
            self.semobj[key] = self.es.enter_context(self.nc.semaphore(key))
            self.downer[key] = tl
        return tl.dsem

    @staticmethod
    def _norm(lst):
        out = []
        for x in lst:
            if isinstance(x, tuple):
                out.append(x)
            else:
                out.append((x, None))
        return out

    def _deps(self, eng, reads, writes):
        deps = {}

        def add(tk):
            if tk is None:
                return
            k, v = tk
            if deps.get(k, 0) < v:
                deps[k] = v

        for tl, key in reads:
            for st in (tl.states(key) if key is not None else tl.states(None)):
                add(st.w)
        for tl, key in writes:
            for st in (tl.states(key) if key is not None else tl.states(None)):
                add(st.w)
                for k, v in st.r.items():
                    add((k, v))
        waits = []
        wd = self.waited[eng]
        for k, v in deps.items():
            if k == eng and eng in ("pe", "sp"):
                continue
            if k in self.downer:
                v = self.downer[k].dcnt
            if wd.get(k, 0) >= v:
                continue
            wd[k] = v
            waits.append((k, v))
        return waits

    def _commit(self, reads, writes, tk):
        k, v = tk
        for tl, key in reads:
            sts = tl.states(key) if key is not None else tl.states(None)
            if key is not None:
                sts = [tl.st[key]]
            for st in sts:
                if st.r.get(k, 0) < v:
                    st.r[k] = v
        for tl, key in writes:
            if key is None:
                tl.st = {"*": _St()}
                tl.st["*"].w = tk
            else:
                st = tl.states(key)[0]
                st.w = tk
                st.r = {}

    def op(self, eng, fn, reads=(), writes=()):
        reads = self._norm(reads)
        writes = self._norm(writes)
        waits = self._deps(eng, reads, writes)
        self.cnt[eng] += 1
        tk = (eng, self.cnt[eng])
        semobj = self.semobj
        so = semobj[eng]

        scope = self.scope
        nc = self.nc

        def run(e, waits=waits, fn=fn, so=so):
            for k, v in waits:
                e.wait_ge(semobj[k], v)
            if scope is None:
                fn(e).then_inc(so, 1)
            else:
                with nc.named_scope(scope):
                    fn(e).then_inc(so, 1)

        self.prog[eng].append(run)
        self._commit(reads, writes, tk)

    def dma(self, q, out, in_, reads=(), writes=(), **kw):
        reads = self._norm(reads)
        writes = self._norm(writes)
        waits = self._deps(q, reads, writes)
        owner = (writes[0][0] if writes else reads[0][0])
        dk = self._dsem(owner)
        owner.dcnt += 16
        tk = (dk, owner.dcnt)
        semobj = self.semobj

        def run(e, waits=waits):
            for k, v in waits:
                e.wait_ge(semobj[k], v)
            e.dma_start(out=out, in_=in_, **kw).then_inc(semobj[dk], 16)

        self.prog[q].append(run)
        self._commit(reads, writes, tk)
        if not writes:
            self.out_tiles.append(owner)

    def collective(self, kind, in_ap, out_ap, reads=(), writes=(), groups=None):
        reads = self._norm(reads)
        writes = self._norm(writes)
        waits = self._deps("pool", reads, writes)
        owner = writes[0][0]
        dk = self._dsem(owner)
        owner.dcnt += 16
        tk = (dk, owner.dcnt)
        semobj = self.semobj
        if groups is None:
            groups = [list(range(8))]

        def run(e, waits=waits):
            for k, v in waits:
                e.wait_ge(semobj[k], v)
            e.collective_compute(kind, (mybir.AluOpType.add if kind in ('AllReduce', 'ReduceScatter') else mybir.AluOpType.bypass), replica_groups=groups, ins=[in_ap], outs=[out_ap]).then_inc(semobj[dk], 16)

        self.prog["pool"].append(run)
        self._commit(reads, writes, tk)

    def finish(self):
        waits = []
        seen = set()
        for tl in self.out_tiles:
            if tl.dsem in seen:
                continue
            seen.add(tl.dsem)
            waits.append((tl.dsem, tl.dcnt))
        semobj = self.semobj

        def run(e):
            for k, v in waits:
                e.wait_ge(semobj[k], v)

        self.prog["sp"].append(run)
        nc = self.nc
        prog = self.prog
        with nc.Block() as block:
            @block.sync
            def _(e):
                for f in prog["sp"]:
                    f(e)

            @block.tensor
            def _(e):
                for f in prog["pe"]:
                    f(e)

            @block.scalar
            def _(e):
                for f in prog["act"]:
                    f(e)

            @block.vector
            def _(e):
                for f in prog["dve"]:
                    f(e)

            @block.gpsimd
            def _(e):
                for f in prog["pool"]:
                    f(e)
        self.es.close()


LN_EPS = 1e-5

def emit_ln_rows(s, xt, P, D, eps, tmp_prefix, stats, mv, rstd, epsT):
    nch = D // 512
    for c in range(nch):
        s.op("dve", lambda e, c=c: e.bn_stats(out=stats[:, c, :], in_=xt[:, c * 512:(c + 1) * 512]),
             reads=[xt], writes=[(stats, c)])
    s.op("dve", lambda e: e.bn_aggr(out=mv[:], in_=stats[:].rearrange("p c s -> p (c s)")), reads=[stats], writes=[mv])
    s.op("act", lambda e: e.activation(out=rstd[:], in_=mv[:, 1:2], func=AF.Sqrt, bias=epsT[:, 0:1], scale=1.0),
         reads=[mv, epsT], writes=[rstd])
    s.op("dve", lambda e: e.reciprocal(out=rstd[:], in_=rstd[:]), reads=[rstd], writes=[rstd])
    s.op("dve", lambda e: e.tensor_scalar(out=xt[:], in0=xt[:], scalar1=mv[:, 0:1], scalar2=rstd[:, 0:1],
                                          op0=ALU.subtract, op1=ALU.mult), reads=[xt, mv, rstd], writes=[xt])


def build_la(Tc, K, N, do_ln):
    nc = bass.Bass("TRN2", target_bir_lowering=False)
    x = nc.dram_tensor("x", [Tc, K], F32, kind="ExternalInput").ap()
    w = nc.dram_tensor("w", [K, N], F32, kind="ExternalInput").ap()
    ident_d = nc.dram_tensor("ident", [128, 128], F32, kind="ExternalInput").ap()
    if do_ln:
        g = nc.dram_tensor("g", [K], F32, kind="ExternalInput").ap()
        b = nc.dram_tensor("b", [K], F32, kind="ExternalInput").ap()
        hout = nc.dram_tensor("hout", [Tc, K], F32, kind="ExternalOutput").ap()
    p = nc.dram_tensor("p", [Tc, N], F32, kind="ExternalOutput").ap()
    s = Sched(nc)
    KC = K // 128
    TH = min(Tc, 1024)
    NTH = TH // 128
    NB = 256
    ident = s.sb([128, 128], F32, "identT")
    s.dma("sp", ident[:], ident_d, writes=[ident])
    if do_ln:
        gB = s.sb([128, K], F32, "gB"); bB = s.sb([128, K], F32, "bB")
        s.dma("sp", gB[:], g.partition_broadcast(128), writes=[gB])
        s.dma("sp", bB[:], b.partition_broadcast(128), writes=[bB])
        stats = s.sb([128, K // 512, 6], F32, "stats"); mv = s.sb([128, 2], F32, "mv"); rstd = s.sb([128, 1], F32, "rstd")
        epsT = s.sb([128, 1], F32, "epsT")
        s.op("dve", lambda e: e.memset(epsT[:], LN_EPS), writes=[epsT])
    xts = [s.sb([128, K], F32, "xt%d" % i) for i in range(2)]
    hT = s.sb([128, KC, TH], BF16, "hT")
    pst = [s.ps([128, 512], F32, "pst%d" % i) for i in range(2)]
    psg = [s.ps([128, 512], F32, "psg%d" % i) for i in range(4)]
    wbs = [s.sb([128, KC, NB], BF16, "wb%d" % i) for i in range(2)]
    outs = [s.sb([128, NB], F32, "ot%d" % i) for i in range(4)]
    nblocks = (N + NB - 1) // NB
    wv = w.rearrange("(kc p) n -> p kc n", p=128)
    tcount = 0
    wcount = 0
    ocount = 0
    for half in range(Tc // TH):
        for ti in range(NTH):
            t0 = half * TH + ti * 128
            xt = xts[tcount % 2]
            s.dma("sp", xt[:], x[t0:t0 + 128, :], writes=[xt])
            if do_ln:
                emit_ln_rows(s, xt, 128, K, LN_EPS, "ln", stats, mv, rstd, epsT)
                s.op("pool", lambda e, xt=xt: e.tensor_tensor(out=xt[:], in0=xt[:], in1=gB[:], op=ALU.mult), reads=[xt, gB], writes=[xt])
                s.op("dve", lambda e, xt=xt: e.tensor_tensor(out=xt[:], in0=xt[:], in1=bB[:], op=ALU.add), reads=[xt, bB], writes=[xt])
                s.dma("sp", hout[t0:t0 + 128, :], xt[:], reads=[xt])
            for k4 in range(KC // 4):
                pt = pst[k4 % 2]
                for j in range(4):
                    kc = k4 * 4 + j
                    s.op("pe", lambda e, pt=pt, j=j, kc=kc, xt=xt: e.transpose(pt[:, j * 128:(j + 1) * 128], xt[:, kc * 128:(kc + 1) * 128], ident[:]),
                         reads=[xt, ident], writes=[(pt, j)])
                eng = "act" if k4 % 2 == 0 else "dve"
                dst = hT[:, k4 * 4:(k4 + 1) * 4, ti * 128:(ti + 1) * 128]
                src = pt[:].rearrange("p (j t) -> p j t", j=4)
                if eng == "act":
                    s.op("act", lambda e, dst=dst, src=src: e.activation(out=dst, in_=src, func=AF.Copy), reads=[pt], writes=[(hT, (ti, k4))])
                else:
                    s.op("dve", lambda e, dst=dst, src=src: e.tensor_copy(out=dst, in_=src), reads=[pt], writes=[(hT, (ti, k4))])
            tcount += 1
        for nb in range(nblocks):
            n0 = nb * NB
            nw = min(NB, N - n0)
            wb = wbs[wcount % 2]; wcount += 1
            for q4 in range(KC // 8):
                s.dma("pool", wb[:, q4 * 8:(q4 + 1) * 8, 0:nw], wv[:, q4 * 8:(q4 + 1) * 8, n0:n0 + nw], writes=[(wb, q4)])
            for ti in range(NTH):
                t0 = half * TH + ti * 128
                pg = psg[ocount % 4]; ot = outs[ocount % 4]; ocount += 1
                for kc in range(KC):
                    s.op("pe", lambda e, pg=pg, kc=kc, wb=wb, ti=ti, nw=nw: e.matmul(pg[:, 0:nw], lhsT=hT[:, kc, ti * 128:(ti + 1) * 128], rhs=wb[:, kc, 0:nw], start=(kc == 0), stop=(kc == KC - 1)),
                         reads=[hT, wb], writes=[pg])
                if ocount % 2 == 0:
                    s.op("act", lambda e, pg=pg, ot=ot, nw=nw: e.activation(out=ot[:, 0:nw], in_=pg[:, 0:nw], func=AF.Copy), reads=[pg], writes=[ot])
                else:
                    s.op("dve", lambda e, pg=pg, ot=ot, nw=nw: e.tensor_copy(out=ot[:, 0:nw], in_=pg[:, 0:nw]), reads=[pg], writes=[ot])
                s.dma("sp", p[t0:t0 + 128, n0:n0 + nw], ot[:, 0:nw], reads=[ot])
    s.finish()
    return nc


def build_lb1(T, TB=1024):
    nc = bass.Bass("TRN2", target_bir_lowering=False)
    C = 64
    NCH = TB // C
    NBLK = T // TB
    dt = lambda n, sh: nc.dram_tensor(n, sh, F32, kind="ExternalInput").ap()
    a_q = dt("a_q", [3, 128, T]); a_f = dt("a_f", [3, 128, T]); a_lb = dt("a_lb", [128, 6])
    a_lbraw = dt("a_lbraw", [128, 6])
    c_q = dt("c_q", [3, 128, T]); c_qs = dt("c_qs", [3, 128, T]); c_k = dt("c_k", [3, 128, T]); c_ks = dt("c_ks", [3, 128, T])
    v6 = dt("v6", [6, T, 64])
    ropeC = dt("ropeC", [128, T]); ropeS = dt("ropeS", [128, T])
    rmask_d = dt("rmask", [128, TB])
    qdec_d = dt("qdec", [128, 3, 64]); kdec_d = dt("kdec", [128, 3, 64]); cdec_d = dt("cdec", [128, 3])
    mask6_d = dt("mask6", [64, 6, 64])
    identb_d = dt("identb", [128, 128])
    o6 = nc.dram_tensor("o6", [T // C, C, 6 * 64], F32, kind="ExternalOutput").ap()
    s = Sched(nc)
    SC = 128.0 ** -0.5
    rmask = s.sb([128, TB], F32, "rmaskT"); s.dma("sp", rmask[:], rmask_d, writes=[rmask])
    qdec = s.sb([128, 3, 64], F32, "qdecT"); s.dma("sp", qdec[:], qdec_d, writes=[qdec])
    kdec = s.sb([128, 3, 64], F32, "kdecT"); s.dma("sp", kdec[:], kdec_d, writes=[kdec])
    cdec = s.sb([128, 3], F32, "cdecT"); s.dma("sp", cdec[:], cdec_d, writes=[cdec])
    mask6 = s.sb([64, 6, 64], F32, "mask6T"); s.dma("sp", mask6[:], mask6_d, writes=[mask6])
    identb = s.sb([128, 128], BF16, "identbT"); s.dma("pool", identb[:], identb_d, writes=[identb])
    lbr = s.sb([128, 6], F32, "lbr"); s.dma("sp", lbr[:], a_lbraw, writes=[lbr])
    lbm = s.sb([128, 6], F32, "lbm"); s.dma("sp", lbm[:], a_lb, writes=[lbm])
    lb = s.sb([128, 3], F32, "lb"); oml = s.sb([128, 3], F32, "oml")
    s.op("dve", lambda e: e.tensor_tensor(out=lb[:], in0=lbr[:, 3:6], in1=lbr[:, 0:3], op=ALU.subtract), reads=[lbr], writes=[lb])
    s.op("act", lambda e: e.activation(out=lb[:], in_=lb[:], func=AF.Sigmoid), reads=[lb], writes=[lb])
    s.op("dve", lambda e: e.tensor_tensor(out=lb[:], in0=lb[:], in1=lbm[:, 0:3], op=ALU.mult), reads=[lb, lbm], writes=[lb])
    s.op("dve", lambda e: e.tensor_scalar(out=oml[:], in0=lb[:], scalar1=-1.0, scalar2=1.0, op0=ALU.mult, op1=ALU.add), reads=[lb], writes=[oml])
    S = s.sb([128, 6, 64], F32, "S"); Sb = s.sb([128, 6, 64], BF16, "Sb")
    s.op("dve", lambda e: e.memset(S[:], 0.0), writes=[S])
    s.op("pool", lambda e: e.memset(Sb[:], 0.0), writes=[Sb])
    d6 = s.sb([128, NCH, 6], F32, "d6")
    Qt = [s.sb([128, TB], BF16, "Qt%d" % u) for u in range(6)]
    Kt = [s.sb([128, TB], BF16, "Kt%d" % u) for u in range(6)]
    Qi = [s.sb([128, TB], BF16, "Qi%d" % u) for u in range(6)]
    Kd = [s.sb([64, NCH, 128], BF16, "Kd%d" % u) for u in range(6)]
    Vb = [s.sb([64, NCH, 64], BF16, "Vb%d" % u) for u in range(6)]
    KdT = s.sb([128, TB], BF16, "KdT")
    sc = [s.sb([128, TB], F32, "sc%d" % i) for i in range(6)]
    rC = s.sb([128, TB], F32, "rC"); rS = s.sb([128, TB], F32, "rS")
    obuf = s.sb([64, NCH, 6 * 64], F32, "obuf")
    attm = [s.sb([64, 6, 64], BF16, "attm%d" % i) for i in range(2)]
    att_ps = [s.ps([64, 512], F32, "attps%d" % i) for i in range(2)]
    o_ps = [s.ps([64, 512], F32, "ops%d" % i) for i in range(2)]
    U_ps = [s.ps([128, 512], F32, "Ups%d" % i) for i in range(2)]
    tr_ps = [s.ps([64, 1024], BF16, "trps%d" % i) for i in range(2)]
    trc = 0

    def ch3(t):
        return t[:].rearrange("p (n c) -> p n c", c=C)

    for blk in range(NBLK):
        tsl = slice(blk * TB, (blk + 1) * TB)
        s.dma("sp", rC[:], ropeC[:, tsl], writes=[rC]); s.dma("sp", rS[:], ropeS[:, tsl], writes=[rS])
        for u in range(6):
            s.dma("pool", Vb[u][:], v6[u, tsl, :].rearrange("(n c) v -> c n v", c=C), writes=[Vb[u]])
            if u < 3:
                q, f, t2, t3, t4, t5 = sc
                s.dma("sp", q[:], a_q[u, :, tsl], writes=[q]); s.dma("sp", f[:], a_f[u, :, tsl], writes=[f])
                s.op("act", lambda e, f=f: e.activation(out=f[:], in_=f[:], func=AF.Sigmoid), reads=[f], writes=[f])
                s.op("dve", lambda e, f=f, u=u: e.tensor_scalar(out=f[:], in0=f[:], scalar1=oml[:, u:u + 1], scalar2=lb[:, u:u + 1], op0=ALU.mult, op1=ALU.add), reads=[f, oml, lb], writes=[f])
                lf = t2
                s.op("act", lambda e, f=f, lf=lf: e.activation(out=lf[:], in_=f[:], func=AF.Ln), reads=[f], writes=[lf])
                kk = f
                s.op("pool", lambda e, f=f: e.tensor_scalar(out=f[:], in0=f[:], scalar1=-1.0, scalar2=1.0, op0=ALU.mult, op1=ALU.add), reads=[f], writes=[f])
                cum = t3
                s.op("dve", lambda e, cum=cum, lf=lf: e.tensor_tensor_scan(out=cum[:], data0=rmask[:], data1=lf[:], initial=0.0, op0=ALU.mult, op1=ALU.add), reads=[rmask, lf], writes=[cum])
                cum3 = ch3(cum)
                mid = cum3[:, :, 31:32].to_broadcast([128, NCH, C])
                last = cum3[:, :, C - 1:C].to_broadcast([128, NCH, C])
                dm = t2
                s.op("pool", lambda e, dm=dm, cum3=cum3, mid=mid: e.tensor_tensor(out=ch3(dm), in0=cum3, in1=mid, op=ALU.subtract), reads=[cum], writes=[dm])
                e1 = t4
                s.op("act", lambda e, e1=e1, dm=dm: e.activation(out=e1[:], in_=dm[:], func=AF.Exp), reads=[dm], writes=[e1])
                s.op("dve", lambda e, u=u, q=q, e1=e1: e.scalar_tensor_tensor(out=Qt[u][:], in0=q[:], scalar=SC, in1=e1[:], op0=ALU.mult, op1=ALU.mult), reads=[q, e1], writes=[Qt[u]])
                e2 = t5
                s.op("act", lambda e, e2=e2, dm=dm: e.activation(out=e2[:], in_=dm[:], func=AF.Exp, scale=-1.0), reads=[dm], writes=[e2])
                s.op("pool", lambda e, u=u, kk=kk, e2=e2: e.tensor_tensor(out=Kt[u][:], in0=kk[:], in1=e2[:], op=ALU.mult), reads=[kk, e2], writes=[Kt[u]])
                e3 = t4
                s.op("act", lambda e, e3=e3, cum=cum: e.activation(out=e3[:], in_=cum[:], func=AF.Exp), reads=[cum], writes=[e3])
                s.op("dve", lambda e, u=u, q=q, e3=e3: e.scalar_tensor_tensor(out=Qi[u][:], in0=q[:], scalar=SC, in1=e3[:], op0=ALU.mult, op1=ALU.mult), reads=[q, e3], writes=[Qi[u]])
                dl = t2
                s.op("pool", lambda e, dl=dl, cum3=cum3, last=last: e.tensor_tensor(out=ch3(dl), in0=last, in1=cum3, op=ALU.subtract), reads=[cum], writes=[dl])
                e4 = t5
                s.op("act", lambda e, e4=e4, dl=dl: e.activation(out=e4[:], in_=dl[:], func=AF.Exp), reads=[dl], writes=[e4])
                s.op("pool", lambda e, kk=kk, e4=e4: e.tensor_tensor(out=KdT[:], in0=kk[:], in1=e4[:], op=ALU.mult), reads=[kk, e4], writes=[KdT])
                s.op("act", lambda e, u=u, cum3=cum3: e.activation(out=d6[:, :, u:u + 1], in_=cum3[:, :, C - 1:C], func=AF.Exp), reads=[cum], writes=[(d6, u)])
            else:
                r = u - 3
                q, qs, k, ks, t4, t5 = sc
                s.dma("sp", q[:], c_q[r, :, tsl], writes=[q]); s.dma("sp", qs[:], c_qs[r, :, tsl], writes=[qs])
                s.dma("sp", k[:], c_k[r, :, tsl], writes=[k]); s.dma("sp", ks[:], c_ks[r, :, tsl], writes=[ks])
                s.op("dve", lambda e, q=q: e.tensor_tensor(out=q[:], in0=q[:], in1=rC[:], op=ALU.mult), reads=[q, rC], writes=[q])
                s.op("pool", lambda e, qs=qs: e.tensor_tensor(out=qs[:], in0=qs[:], in1=rS[:], op=ALU.mult), reads=[qs, rS], writes=[qs])
                s.op("dve", lambda e, q=q, qs=qs: e.tensor_tensor(out=q[:], in0=q[:], in1=qs[:], op=ALU.add), reads=[q, qs], writes=[q])
                s.op("act", lambda e, u=u, q=q: e.activation(out=Qt[u][:], in_=q[:], func=AF.Copy), reads=[q], writes=[Qt[u]])
                qd = qdec[:, r:r + 1, :].to_broadcast([128, NCH, C])
                s.op("pool", lambda e, u=u, q=q, qd=qd: e.tensor_tensor(out=ch3(Qi[u]), in0=ch3(q), in1=qd, op=ALU.mult), reads=[q, qdec], writes=[Qi[u]])
                s.op("dve", lambda e, k=k: e.tensor_tensor(out=k[:], in0=k[:], in1=rC[:], op=ALU.mult), reads=[k, rC], writes=[k])
                s.op("pool", lambda e, ks=ks: e.tensor_tensor(out=ks[:], in0=ks[:], in1=rS[:], op=ALU.mult), reads=[ks, rS], writes=[ks])
                s.op("dve", lambda e, k=k, ks=ks: e.tensor_tensor(out=k[:], in0=k[:], in1=ks[:], op=ALU.add), reads=[k, ks], writes=[k])
                s.op("act", lambda e, u=u, k=k: e.activation(out=Kt[u][:], in_=k[:], func=AF.Copy, scale=SC), reads=[k], writes=[Kt[u]])
                kd = kdec[:, r:r + 1, :].to_broadcast([128, NCH, C])
                s.op("pool", lambda e, k=k, kd=kd: e.tensor_tensor(out=ch3(KdT), in0=ch3(k), in1=kd, op=ALU.mult), reads=[k, kdec], writes=[KdT])
                cd = cdec[:, r:r + 1].to_broadcast([128, NCH])
                s.op("dve", lambda e, u=u, cd=cd: e.tensor_copy(out=d6[:, :, u], in_=cd), reads=[cdec], writes=[(d6, u)])
            for g in range(NCH // 8):
                tp = tr_ps[trc % 2]; trc += 1
                for j in range(8):
                    n = g * 8 + j
                    s.op("pe", lambda e, tp=tp, j=j, n=n: e.transpose(tp[:, j * 128:(j + 1) * 128], KdT[:, n * C:(n + 1) * C], identb[:]),
                         reads=[KdT, identb], writes=[(tp, j)])
                s.op("act", lambda e, tp=tp, g=g, u=u: e.activation(out=Kd[u][:, g * 8:(g + 1) * 8, :], in_=tp[:].rearrange("p (j k) -> p j k", j=8), func=AF.Copy),
                     reads=[tp], writes=[(Kd[u], g)])
        for n in range(NCH):
            gi = blk * NCH + n
            ap_ = att_ps[gi % 2]; op_ = o_ps[gi % 2]; up_ = U_ps[gi % 2]; am = attm[gi % 2]
            csl = slice(n * C, (n + 1) * C)
            for u in range(6):
                s.op("pe", lambda e, u=u, ap_=ap_, csl=csl: e.matmul(ap_[:, u * 64:(u + 1) * 64], lhsT=Kt[u][:, csl], rhs=Qt[u][:, csl], start=True, stop=True),
                     reads=[Kt[u], Qt[u]], writes=[(ap_, u)])
            s.op("dve", lambda e, ap_=ap_, am=am: e.tensor_tensor(out=am[:].rearrange("p u t -> p (u t)"), in0=ap_[:, 0:384], in1=mask6[:].rearrange("p u t -> p (u t)"), op=ALU.mult),
                 reads=[ap_, mask6], writes=[am])
            for u in range(6):
                s.op("pe", lambda e, u=u, up_=up_, n=n: e.matmul(up_[:, u * 64:(u + 1) * 64], lhsT=Kd[u][:, n, :], rhs=Vb[u][:, n, :], start=True, stop=True),
                     reads=[Kd[u], Vb[u]], writes=[(up_, u)])
            for u in range(6):
                s.op("pe", lambda e, u=u, op_=op_, am=am, n=n: e.matmul(op_[:, u * 64:(u + 1) * 64], lhsT=am[:, u, :], rhs=Vb[u][:, n, :], start=True, stop=False),
                     reads=[am, Vb[u]], writes=[(op_, u)])
                s.op("pe", lambda e, u=u, op_=op_, csl=csl: e.matmul(op_[:, u * 64:(u + 1) * 64], lhsT=Qi[u][:, csl], rhs=Sb[:, u, :], start=False, stop=True),
                     reads=[Qi[u], Sb], writes=[(op_, u)])
            s.op("act", lambda e, op_=op_, n=n: e.activation(out=obuf[:, n, :], in_=op_[:, 0:384], func=AF.Copy), reads=[op_], writes=[(obuf, n)])
            dB = d6[:, n, :].unsqueeze(2).to_broadcast([128, 6, 64])
            s.op("dve", lambda e, dB=dB: e.tensor_tensor(out=S[:], in0=S[:], in1=dB, op=ALU.mult), reads=[S, d6], writes=[S])
            s.op("dve", lambda e, up_=up_: e.tensor_tensor(out=S[:].rearrange("p u v -> p (u v)"), in0=S[:].rearrange("p u v -> p (u v)"), in1=up_[:, 0:384], op=ALU.add), reads=[S, up_], writes=[S])
            s.op("act", lambda e: e.activation(out=Sb[:], in_=S[:], func=AF.Copy), reads=[S], writes=[Sb])
        s.dma("sp", o6[blk * NCH:(blk + 1) * NCH].rearrange("n t u -> t n u"), obuf[:], reads=[obuf])
    s.finish()
    return nc


NEG1 = -1.0e30

def build_lb2(T, NQ, debug=False):
    nc = bass.Bass("TRN2", target_bir_lowering=False)
    NKT = T // 128
    dt = lambda n, sh: nc.dram_tensor(n, sh, F32, kind="ExternalInput").ap()
    qT = dt("qT", [NQ, 128, 8, 128]); qsw = dt("qsw", [NQ, 32, 8, 128]); Cq = dt("Cq", [NQ, 32, 128]); Sq = dt("Sq", [NQ, 32, 128])
    kTt = dt("kTt", [NKT, 128, 8, 128]); krsw = dt("krsw", [NKT, 32, 2, 8, 128]); CSk = dt("CSk", [NKT, 32, 2, 128])
    Vt = dt("Vt", [NKT, 128, 8, 132])
    qiT = dt("qiT", [NQ, 64, 16, 128]); qisw = dt("qisw", [NQ, 16, 16, 128]); Cqi = dt("Cqi", [NQ, 16, 128]); Sqi = dt("Sqi", [NQ, 16, 128])
    kiT = dt("kiT", [64, T]); kisw = dt("kisw", [16, T]); Cki = dt("Cki", [16, T]); Ski = dt("Ski", [16, T])
    iw = dt("iw", [NQ, 128, 16])
    pen_d = dt("pen", [128, 1024])
    identb_d = dt("identb", [128, 128])
    ob = nc.dram_tensor("ob", [NQ, 128, 1024], F32, kind="ExternalOutput").ap()
    NMAX = 1024 * NQ
    if debug:
        dbg1 = nc.dram_tensor("dbg1", [NQ, 128, NMAX], F32, kind="ExternalOutput").ap()
        dbg2 = nc.dram_tensor("dbg2", [NQ, 128, NMAX], F32, kind="ExternalOutput").ap()
        dbg3 = nc.dram_tensor("dbg3", [NQ, 128, 4, 258], F32, kind="ExternalOutput").ap()
    s = Sched(nc)
    SC = 128.0 ** -0.5
    assert NMAX <= T
    pen = s.sb([128, 1024], F32, "penT"); s.dma("sp", pen[:], pen_d, writes=[pen])
    identb = s.sb([128, 128], BF16, "identbT"); s.dma("pool", identb[:], identb_d, writes=[identb])
    kiTb = s.sb([64, NMAX], BF16, "kiTb")
    PW = min(512, NMAX)
    stg = s.sb([64, PW], F32, "stg"); stg2 = s.sb([16, 3, PW], F32, "stg2"); stg3 = s.sb([16, 2, PW], F32, "stg3")
    for pc in range(NMAX // PW):
        sl = slice(pc * PW, (pc + 1) * PW)
        s.dma("sp", stg[:], kiT[:, sl], writes=[stg])
        s.dma("sp", stg2[:, 0, :], kisw[:, sl], writes=[(stg2, 0)])
        s.dma("sp", stg2[:, 1, :], Cki[:, sl], writes=[(stg2, 1)])
        s.dma("sp", stg2[:, 2, :], Ski[:, sl], writes=[(stg2, 2)])
        s.op("dve", lambda e: e.tensor_tensor(out=stg3[:, 0, :], in0=stg[0:16, :], in1=stg2[:, 1, :], op=ALU.mult), reads=[stg, stg2], writes=[(stg3, 0)])
        s.op("pool", lambda e: e.tensor_tensor(out=stg3[:, 1, :], in0=stg2[:, 0, :], in1=stg2[:, 2, :], op=ALU.mult), reads=[stg2], writes=[(stg3, 1)])
        s.op("dve", lambda e: e.tensor_tensor(out=stg[0:16, :], in0=stg3[:, 0, :], in1=stg3[:, 1, :], op=ALU.add), reads=[stg3], writes=[stg])
        s.op("act", lambda e, sl=sl: e.activation(out=kiTb[:, sl], in_=stg[:], func=AF.Copy), reads=[stg], writes=[(kiTb, pc)])
    work = s.sb([128, NMAX], F32, "work")
    qf = s.sb([128, 8, 128], F32, "qf"); cq_t = s.sb([32, 2, 128], F32, "cq_t")
    qb = s.sb([128, 8, 128], BF16, "qb")
    qif = s.sb([64, 16, 128], F32, "qif"); cqi_t = s.sb([16, 2, 128], F32, "cqi_t")
    qib = s.sb([64, 16, 128], BF16, "qib")
    rt1 = s.sb([32, 16, 128], F32, "rt1"); rt2 = s.sb([32, 16, 128], F32, "rt2")
    iwt = s.sb([128, 16], F32, "iwt")
    rl = [s.sb([128, 512], F32, "rl%d" % i) for i in range(3)]
    m8 = s.sb([128, 8], F32, "m8")
    mT = [s.sb([128, 128], BF16, "mT%d" % i) for i in range(3)]
    kb16 = [s.sb([128, 8, 128], BF16, "kb16_%d" % i) for i in range(4)]
    krs0 = s.sb([32, 2, 8, 128], F32, "krs0")
    krs_tiles = [krs0, rt1, rt2]
    krs_views = [krs0[:], rt1[:].rearrange("p (a h) q -> p a h q", a=2), rt2[:].rearrange("p (a h) q -> p a h q", a=2)]
    ck_t = [s.sb([32, 2, 128], F32, "ckt%d" % i) for i in range(3)]
    pexp = None
    vb = [s.sb([128, 8, 132], BF16, "vb%d" % i) for i in range(4)]
    pexp = [s.sb([128, 4, 128], BF16, "pexp%d" % i) for i in range(6)]
    pm = [s.sb([128, 4, 128], BF16, "pm%d" % i) for i in range(6)]
    otile = s.sb([128, 8, 128], F32, "otile"); rec = s.sb([128, 8, 1], F32, "rec")
    psA = [s.ps([128, 512], F32, "psA%d" % i) for i in range(3)]
    oacc = [s.ps([128, 512], F32, "oacc%d" % i) for i in range(4)]
    psT = s.ps([128, 3, 128], F32, "psT")
    identf = s.sb([128, 128], F32, "identfT"); s.dma("sp", identf[:], identb_d, writes=[identf])
    pac = 0
    pairc = 0
    zl = s.sb([128, 128], BF16, "zl"); zr = s.sb([128, 512], BF16, "zr")
    s.op("pool", lambda e: e.memset(zl[:], 0.0), writes=[zl])
    s.op("pool", lambda e: e.memset(zr[:], 0.0), writes=[zr])

    def bc(t2d, n):
        return t2d.unsqueeze(1).to_broadcast([t2d.shape[0], n, 128])

    for i in range(NQ):
        N = 1024 * (i + 1)
        s.scope = 'prep%d' % i
        s.dma("sp", qf[:], qT[i], writes=[qf]); s.dma("sp", rt2[:, 0:8, :], qsw[i], writes=[rt2])
        s.dma("sp", cq_t[:, 0, :], Cq[i], writes=[(cq_t, 0)]); s.dma("sp", cq_t[:, 1, :], Sq[i], writes=[(cq_t, 1)])
        s.op("dve", lambda e: e.tensor_tensor(out=rt1[:, 0:8, :], in0=qf[0:32], in1=bc(cq_t[:, 0, :], 8), op=ALU.mult), reads=[qf, cq_t], writes=[rt1])
        s.op("pool", lambda e: e.tensor_tensor(out=rt2[:, 0:8, :], in0=rt2[:, 0:8, :], in1=bc(cq_t[:, 1, :], 8), op=ALU.mult), reads=[rt2, cq_t], writes=[rt2])
        s.op("dve", lambda e: e.tensor_tensor(out=qf[0:32], in0=rt1[:, 0:8, :], in1=rt2[:, 0:8, :], op=ALU.add), reads=[rt1, rt2], writes=[qf])
        s.op("act", lambda e: e.activation(out=qb[:], in_=qf[:], func=AF.Copy), reads=[qf], writes=[qb])
        s.dma("sp", qif[:], qiT[i], writes=[qif]); s.dma("sp", rt2[0:16], qisw[i], writes=[rt2])
        s.dma("sp", cqi_t[:, 0, :], Cqi[i], writes=[(cqi_t, 0)]); s.dma("sp", cqi_t[:, 1, :], Sqi[i], writes=[(cqi_t, 1)])
        s.op("dve", lambda e: e.tensor_tensor(out=rt1[0:16], in0=qif[0:16], in1=bc(cqi_t[:, 0, :], 16), op=ALU.mult), reads=[qif, cqi_t], writes=[rt1])
        s.op("pool", lambda e: e.tensor_tensor(out=rt2[0:16], in0=rt2[0:16], in1=bc(cqi_t[:, 1, :], 16), op=ALU.mult), reads=[rt2, cqi_t], writes=[rt2])
        s.op("dve", lambda e: e.tensor_tensor(out=qif[0:16], in0=rt1[0:16], in1=rt2[0:16], op=ALU.add), reads=[rt1, rt2], writes=[qif])
        s.op("act", lambda e: e.activation(out=qib[:], in_=qif[:], func=AF.Copy), reads=[qif], writes=[qib])
        s.dma("sp", iwt[:], iw[i], writes=[iwt])
        s.scope = 'idx%d' % i
        for kb in range(N // 512):
            ksl = slice(kb * 512, (kb + 1) * 512)
            for h in range(16):
                pa = psA[pac % 3]; r_ = rl[pac % 3]; pac += 1
                s.op("pe", lambda e, pa=pa, h=h, ksl=ksl: e.matmul(pa[:], lhsT=qib[:, h, :], rhs=kiTb[:, ksl], start=True, stop=True), reads=[qib, kiTb], writes=[pa])
                s.op("act", lambda e, pa=pa, r_=r_: e.activation(out=r_[:], in_=pa[:], func=AF.Relu), reads=[pa], writes=[r_])
                if h == 0:
                    s.op("dve", lambda e, r_=r_, ksl=ksl: e.tensor_scalar(out=work[:, ksl], in0=r_[:], scalar1=iwt[:, 0:1], scalar2=None, op0=ALU.mult), reads=[r_, iwt], writes=[(work, kb)])
                else:
                    s.op("dve", lambda e, r_=r_, ksl=ksl, h=h: e.scalar_tensor_tensor(out=work[:, ksl], in0=r_[:], scalar=iwt[:, h:h + 1], in1=work[:, ksl], op0=ALU.mult, op1=ALU.add), reads=[r_, iwt, (work, kb)], writes=[(work, kb)])
        zs = slice(N - 1024, N)
        s.op("dve", lambda e, zs=zs: e.tensor_tensor(out=work[:, zs], in0=work[:, zs], in1=pen[:], op=ALU.min), reads=[work, pen], writes=[work])
        if debug:
            s.dma("sp", dbg1[i, :, 0:N], work[:, 0:N], reads=[work])
        s.scope = 'topk%d' % i
        for r in range(32):
            s.op("dve", lambda e, N=N: e.max(out=m8[:], in_=work[:, 0:N]), reads=[work], writes=[m8])
            s.op("dve", lambda e, N=N: e.match_replace(out=work[:, 0:N], in_to_replace=m8[:], in_values=work[:, 0:N], imm_value=NEG1), reads=[work, m8], writes=[work])
        if debug:
            s.dma("sp", dbg2[i, :, 0:N], work[:, 0:N], reads=[work])
        s.op("pool", lambda e, N=N: e.tensor_scalar(out=work[:, 0:N], in0=work[:, 0:N], scalar1=NEG1, scalar2=None, op0=ALU.is_equal), reads=[work], writes=[work])
        s.scope = 'attn%d' % i
        nkt = N // 128
        for b in range(4):
            s.op("pe", lambda e, b=b: e.matmul(oacc[b][:], lhsT=zl[:], rhs=zr[:], start=True, stop=True), reads=[zl, zr], writes=[oacc[b]])
        def stageA1(kt):
            pp = kt % 3
            kb_ = kb16[kt % 4]; kt_ = krs_tiles[pp]; kv_ = krs_views[pp]; ck_ = ck_t[pp]; vb_ = vb[kt % 4]
            s.dma("pool", kb_[:].rearrange("p h k -> p (h k)"), kTt[kt].rearrange("p h k -> p (h k)"), writes=[kb_])
            s.dma("sp", kv_.rearrange("p a h k -> p (a h k)"), krsw[kt].rearrange("p a h k -> p (a h k)"), writes=[kt_])
            s.dma("sp", ck_[:], CSk[kt], writes=[ck_])
            s.dma("pool", vb_[:].rearrange("p h k -> p (h k)"), Vt[kt].rearrange("p h k -> p (h k)"), writes=[vb_])

        def stageA2(kt):
            nonlocal pac
            pp = kt % 3
            mt = mT[pp]
            s.op("pe", lambda e, kt=kt, pp=pp: e.transpose(psT[:, pp, :], work[:, kt * 128:(kt + 1) * 128], identf[:]), reads=[work, identf], writes=[(psT, pp)])
            s.op("act", lambda e, mt=mt, pp=pp: e.activation(out=mt[:], in_=psT[:, pp, :], func=AF.Copy), reads=[(psT, pp)], writes=[mt])
            kb_ = kb16[kt % 4]; kt_ = krs_tiles[pp]; kv_ = krs_views[pp]; ck_ = ck_t[pp]
            csb = ck_[:].unsqueeze(2).to_broadcast([32, 2, 8, 128])
            s.op("dve", lambda e, kv_=kv_, csb=csb: e.tensor_tensor(out=kv_, in0=kv_, in1=csb, op=ALU.mult), reads=[kt_, ck_], writes=[kt_])
            s.op("dve", lambda e, kb_=kb_, kv_=kv_: e.tensor_tensor(out=kb_[0:32], in0=kv_[:, 0], in1=kv_[:, 1], op=ALU.add), reads=[kt_], writes=[kb_])
            for g in range(2):
                pa = psA[pac % 3]; pac += 1
                pe_ = pexp[pp * 2 + g]; pm_ = pm[(pp * 2 + g)]
                for hh in range(4):
                    h = 4 * g + hh
                    s.op("pe", lambda e, pa=pa, hh=hh, h=h, kb_=kb_: e.matmul(pa[:, hh * 128:(hh + 1) * 128], lhsT=kb_[:, h, :], rhs=qb[:, h, :], start=True, stop=True), reads=[kb_, qb], writes=[(pa, hh)])
                s.op("act", lambda e, pa=pa, pe_=pe_: e.activation(out=pe_[:].rearrange("p h q -> p (h q)"), in_=pa[:], func=AF.Exp, scale=SC), reads=[pa], writes=[pe_])
                s.op("pool", lambda e, pe_=pe_, pm_=pm_, mt=mt: e.tensor_tensor(out=pm_[:], in0=pe_[:], in1=bc(mt[:], 4), op=ALU.mult), reads=[pe_, mt], writes=[pm_])

        def stageB(kt):
            pp = kt % 3
            vb_ = vb[kt % 4]
            for h in range(8):
                pm_ = pm[pp * 2 + h // 4]
                oa = oacc[h // 2]
                s.op("pe", lambda e, oa=oa, h=h, pm_=pm_, vb_=vb_: e.matmul(oa[:, (h % 2) * 129:(h % 2) * 129 + 129], lhsT=pm_[:, h % 4, :], rhs=vb_[:, h, 0:129], start=False, stop=True),
                     reads=[pm_, vb_], writes=[(oa, h % 2)])

        stageA1(0)
        if nkt > 1:
            stageA1(1)
        for kt in range(nkt):
            stageA2(kt)
            if kt > 0:
                stageB(kt - 1)
            if kt + 2 < nkt:
                stageA1(kt + 2)
        stageB(nkt - 1)
        if debug:
            dbt = s.sb([128, 4, 258], F32, "dbt%d" % i)
            for b in range(4):
                s.op("dve", lambda e, b=b, dbt=dbt: e.tensor_copy(out=dbt[:, b, :], in_=oacc[b][:, 0:258]), reads=[oacc[b]], writes=[(dbt, b)])
            s.dma("sp", dbg3[i], dbt[:], reads=[dbt])
        for b in range(4):
            ov = oacc[b][:, 0:258].rearrange("p (h c) -> p h c", h=2)
            s.op("dve", lambda e, ov=ov, b=b: e.reciprocal(out=rec[:, 2 * b:2 * b + 2, :], in_=ov[:, :, 128:129]), reads=[oacc[b]], writes=[(rec, b)])
            s.op("dve", lambda e, ov=ov, b=b: e.tensor_tensor(out=otile[:, 2 * b:2 * b + 2, :], in0=ov[:, :, 0:128], in1=rec[:, 2 * b:2 * b + 2, :].to_broadcast([128, 2, 128]), op=ALU.mult), reads=[oacc[b], (rec, b)], writes=[(otile, b)])
        s.dma("sp", ob[i].rearrange("p (h d) -> p h d", h=8), otile[:], reads=[otile])
    s.finish()
    return nc


LN_EPS = 1e-5
NORM_EPS = 1e-6
DN_ALPHA = 4 ** 0.25
LIMIT = 7.0
SALPHA = 1.702

def build_lc(Tc, NE=32, D=4096, F=768, upto=5):
    nc = bass.Bass("TRN2", target_bir_lowering=False)
    dt = lambda n, sh: nc.dram_tensor(n, sh, F32, kind="ExternalInput").ap()
    KC = D // 128
    FB = F // 128
    h_d = dt("h", [Tc, D]); oa_d = dt("oa", [Tc, 1536]); ag_d = dt("ag", [Tc, 1536]); ob_d = dt("ob", [Tc, 1024])
    oc_d = dt("oc", [Tc, 1536]); cg_d = dt("cg", [Tc, 1536]); gA_d = dt("gA", [1536]); gC_d = dt("gC", [1536])
    wout_d = dt("wout", [D, D]); l1g = dt("l1g", [D]); l1b = dt("l1b", [D]); l2g = dt("l2g", [D]); l2b = dt("l2b", [D])
    rw_d = dt("rw", [D, NE]); rb_d = dt("rb", [NE])
    NEW = NE if upto >= 4 else 1
    wgu_d = dt("wgu", [NEW, D, 2 * F]); bgu_d = dt("bguT", [128, NE, 2 * FB]); wd_d = dt("wd", [NEW, F, D]); bd_d = dt("bd", [NE, D])
    ident_d = dt("ident", [128, 128])
    hout = nc.dram_tensor("hout", [Tc, D], F32, kind="ExternalOutput").ap()
    s = Sched(nc)
    GT = 256
    NT = GT // 128
    ident = s.sb([128, 128], F32, "identT"); s.dma("sp", ident[:], ident_d, writes=[ident])
    epsT = s.sb([128, 2], F32, "epsT")
    s.op("dve", lambda e: e.memset(epsT[:, 0:1], LN_EPS), writes=[(epsT, 0)])
    s.op("dve", lambda e: e.memset(epsT[:, 1:2], NORM_EPS), writes=[(epsT, 1)])
    rw = s.sb([128, KC, NE], F32, "rwT"); s.dma("sp", rw[:], rw_d.rearrange("(kc p) e -> p kc e", p=128), writes=[rw])
    rwh = s.sb([128, KC, NE], BF16, "rwh"); rwl = s.sb([128, KC, NE], BF16, "rwl")
    s.op("act", lambda e: e.activation(out=rwh[:], in_=rw[:], func=AF.Copy), reads=[rw], writes=[rwh])
    s.op("dve", lambda e: e.tensor_tensor(out=rwl[:], in0=rw[:], in1=rwh[:], op=ALU.subtract), reads=[rw, rwh], writes=[rwl])
    sc4l = s.sb([128, 4, 128], BF16, "sc4l")
    rbB = s.sb([128, NE], F32, "rbB"); s.dma("sp", rbB[:], rb_d.partition_broadcast(128), writes=[rbB])
    bg = s.sb([128, NE, 2 * FB], F32, "bgT"); s.dma("sp", bg[:], bgu_d, writes=[bg])
    s.op("dve", lambda e: e.tensor_scalar(out=bg[:, :, FB:2 * FB], in0=bg[:, :, FB:2 * FB], scalar1=1.0, scalar2=None, op0=ALU.add), reads=[bg], writes=[bg])
    bdb = s.sb([NE, D], BF16, "bdb"); s.dma("pool", bdb[:], bd_d, writes=[bdb])
    hres = [s.sb([128, D], F32, "hres%d" % i) for i in range(NT)]
    hT = s.sb([128, KC, GT], BF16, "hT")
    scr = s.sb([128, 4096], F32, "scr")
    lnb = s.sb([128, 2, D], F32, "lnb")
    WP = 6144
    wpool = [s.sb([128, WP], BF16, "wp%d" % i) for i in range(3)]
    wc = [0]
    actT = s.sb([128, FB, GT], BF16, "actT")
    tmpA = s.sb([128, 512], F32, "tmpA"); tmpB = s.sb([128, 512], F32, "tmpB")
    stats = s.sb([128, D // 512, 6], F32, "stats"); mv = s.sb([128, 2], F32, "mv"); rstd = s.sb([128, 1], F32, "rstd")
    sm = s.sb([128, 64], F32, "sm")
    G = [s.sb([128, NE], F32, "G%d" % i) for i in range(NT)]
    GTt = [s.sb([NE, 128], BF16, "GTt%d" % i) for i in range(NT)]
    lgt = s.sb([128, NE], F32, "lgt"); m8 = s.sb([128, 8], F32, "m8"); ex = s.sb([128, NE], F32, "ex"); msk = s.sb([128, NE], F32, "msk")
    sc4 = s.sb([128, 4, 128], F32, "sc4")
    bank = [s.ps([128, 512], F32, "bank%d" % i) for i in range(8)]

    def nextw():
        w = wpool[wc[0] % 3]; wc[0] += 1
        return w

    def transpose_tile(src, ti, also_router_ps=None):
        for k4 in range(KC // 4):
            pt = bank[k4 % 2]
            for j in range(4):
                kc = k4 * 4 + j
                s.op("pe", lambda e, pt=pt, j=j, kc=kc: e.transpose(pt[:, j * 128:(j + 1) * 128], src[:, kc * 128:(kc + 1) * 128], ident[:]),
                     reads=[src, ident], writes=[(pt, j)])
            dst = hT[:, k4 * 4:(k4 + 1) * 4, ti * 128:(ti + 1) * 128]
            srcv = pt[:].rearrange("p (j t) -> p j t", j=4)
            s.op("act", lambda e, dst=dst, srcv=srcv: e.activation(out=dst, in_=srcv, func=AF.Copy), reads=[pt], writes=[(hT, (ti, k4))])
            if also_router_ps is not None:
                s.op("dve", lambda e, srcv=srcv, dst=dst: e.tensor_tensor(out=sc4l[:], in0=srcv, in1=dst, op=ALU.subtract), reads=[pt, (hT, (ti, k4))], writes=[sc4l])
                for j in range(4):
                    kc = k4 * 4 + j
                    hi = hT[:, kc, ti * 128:(ti + 1) * 128]
                    s.op("pe", lambda e, hi=hi, kc=kc: e.matmul(also_router_ps[:, 0:NE], lhsT=hi, rhs=rwh[:, kc, :], start=(kc == 0), stop=False),
                         reads=[hT, rwh], writes=[also_router_ps])
                    s.op("pe", lambda e, j=j, kc=kc: e.matmul(also_router_ps[:, 0:NE], lhsT=sc4l[:, j, :], rhs=rwh[:, kc, :], start=False, stop=False),
                         reads=[sc4l, rwh], writes=[also_router_ps])
                    s.op("pe", lambda e, hi=hi, kc=kc: e.matmul(also_router_ps[:, 0:NE], lhsT=hi, rhs=rwl[:, kc, :], start=False, stop=(kc == KC - 1)),
                         reads=[hT, rwl], writes=[also_router_ps])

    def layer_norm(xt, gd, bd_):
        s.dma("sp", lnb[:, 0, :], gd.partition_broadcast(128), writes=[(lnb, 0)])
        s.dma("sp", lnb[:, 1, :], bd_.partition_broadcast(128), writes=[(lnb, 1)])
        emit_ln_rows(s, xt, 128, D, LN_EPS, "ln", stats, mv, rstd, epsT)
        s.op("pool", lambda e: e.tensor_tensor(out=xt[:], in0=xt[:], in1=lnb[:, 0, :], op=ALU.mult), reads=[xt, lnb], writes=[xt])
        s.op("dve", lambda e: e.tensor_tensor(out=xt[:], in0=xt[:], in1=lnb[:, 1, :], op=ALU.add), reads=[xt, lnb], writes=[xt])

    for grp in range(Tc // GT):
        for ti in range(NT):
            t0 = grp * GT + ti * 128
            rows = slice(t0, t0 + 128)
            s.dma("sp", hres[ti][:], h_d[rows, :], writes=[hres[ti]])
            mx = scr
            s.dma("sp", mx[:, 0:1536], oa_d[rows, :], writes=[(mx, "a")])
            s.dma("sp", mx[:, 1536:2560], ob_d[rows, :], writes=[(mx, "b")])
            s.dma("sp", mx[:, 2560:4096], oc_d[rows, :], writes=[(mx, "c")])
            gt = lnb[:, 0, 0:3072]; gn = lnb[:, 1, 0:3072]
            s.dma("sp", gt[:, 0:1536], ag_d[rows, :], writes=[(lnb, 0)]); s.dma("sp", gt[:, 1536:3072], cg_d[rows, :], writes=[(lnb, 0)])
            s.dma("sp", gn[:, 0:1536], gA_d.partition_broadcast(128), writes=[(lnb, 1)]); s.dma("sp", gn[:, 1536:3072], gC_d.partition_broadcast(128), writes=[(lnb, 1)])
            s.op("act", lambda e, gt=gt: e.activation(out=gt, in_=gt, func=AF.Silu), reads=[(lnb, 0)], writes=[(lnb, 0)])
            s.op("pool", lambda e, gt=gt, gn=gn: e.tensor_tensor(out=gt, in0=gt, in1=gn, op=ALU.mult), reads=[lnb], writes=[(lnb, 0)])
            xa = mx[:, 0:1536].rearrange("p (h v) -> p h v", v=128)
            sq = lnb[:, 1, 0:1536].rearrange("p (h v) -> p h v", v=128)
            s.op("dve", lambda e, xa=xa, sq=sq: e.tensor_tensor(out=sq, in0=xa, in1=xa, op=ALU.mult), reads=[(mx, "a"), lnb], writes=[(lnb, 1)])
            s.op("dve", lambda e, sq=sq: e.tensor_reduce(out=sm[:, 0:12], in_=sq, op=ALU.add, axis=AX.X), reads=[(lnb, 1)], writes=[sm])
            s.op("act", lambda e: e.activation(out=sm[:, 0:12], in_=sm[:, 0:12], func=AF.Sqrt, bias=epsT[:, 1:2], scale=1.0 / 128), reads=[sm, epsT], writes=[sm])
            s.op("dve", lambda e: e.reciprocal(out=sm[:, 0:12], in_=sm[:, 0:12]), reads=[sm], writes=[sm])
            s.op("dve", lambda e, xa=xa: e.tensor_tensor(out=xa, in0=xa, in1=sm[:, 0:12].unsqueeze(2).to_broadcast([128, 12, 128]), op=ALU.mult), reads=[(mx, "a"), sm], writes=[(mx, "a")])
            xc = mx[:, 2560:4096].rearrange("p (h v) -> p h v", v=256)
            sq2 = lnb[:, 1, 0:1536].rearrange("p (h v) -> p h v", v=256)
            s.op("dve", lambda e, xc=xc: e.tensor_reduce(out=sm[:, 16:22], in_=xc, op=ALU.add, axis=AX.X), reads=[(mx, "c")], writes=[sm])
            s.op("dve", lambda e: e.tensor_scalar(out=sm[:, 16:22], in0=sm[:, 16:22], scalar1=1.0 / 256, scalar2=None, op0=ALU.mult), reads=[sm], writes=[sm])
            s.op("dve", lambda e, xc=xc: e.tensor_tensor(out=xc, in0=xc, in1=sm[:, 16:22].unsqueeze(2).to_broadcast([128, 6, 256]), op=ALU.subtract), reads=[(mx, "c"), sm], writes=[(mx, "c")])
            s.op("dve", lambda e, xc=xc, sq2=sq2: e.tensor_tensor(out=sq2, in0=xc, in1=xc, op=ALU.mult), reads=[(mx, "c"), lnb], writes=[(lnb, 1)])
            s.op("dve", lambda e, sq2=sq2: e.tensor_reduce(out=sm[:, 24:30], in_=sq2, op=ALU.add, axis=AX.X), reads=[(lnb, 1)], writes=[sm])
            s.op("act", lambda e: e.activation(out=sm[:, 24:30], in_=sm[:, 24:30], func=AF.Sqrt, bias=epsT[:, 1:2], scale=1.0 / 256), reads=[sm, epsT], writes=[sm])
            s.op("dve", lambda e: e.reciprocal(out=sm[:, 24:30], in_=sm[:, 24:30]), reads=[sm], writes=[sm])
            s.op("dve", lambda e, xc=xc: e.tensor_tensor(out=xc, in0=xc, in1=sm[:, 24:30].unsqueeze(2).to_broadcast([128, 6, 256]), op=ALU.mult), reads=[(mx, "c"), sm], writes=[(mx, "c")])
            s.op("pool", lambda e, gt=gt: e.tensor_tensor(out=mx[:, 0:1536], in0=mx[:, 0:1536], in1=gt[:, 0:1536], op=ALU.mult), reads=[mx, lnb], writes=[(mx, "a")])
            s.op("pool", lambda e, gt=gt: e.tensor_tensor(out=mx[:, 2560:4096], in0=mx[:, 2560:4096], in1=gt[:, 1536:3072], op=ALU.mult), reads=[mx, lnb], writes=[(mx, "c")])
            transpose_tile(mx, ti)
        CB = 128
        wv = wout_d.rearrange("(kc p) n -> p kc n", p=128)
        for cb in range(D // CB):
            w = nextw()
            wvw = w[:, 0:KC * CB].rearrange("p (kc n) -> p kc n", n=CB)
            for q4 in range(4):
                s.dma("pool", wvw[:, q4 * 8:(q4 + 1) * 8, :], wv[:, q4 * 8:(q4 + 1) * 8, cb * CB:(cb + 1) * CB], writes=[(w, q4)])
            for ti in range(NT):
                pg = bank[2 + (cb * NT + ti) % 4]
                for kc in range(KC):
                    s.op("pe", lambda e, pg=pg, kc=kc, wvw=wvw, ti=ti: e.matmul(pg[:, 0:CB], lhsT=hT[:, kc, ti * 128:(ti + 1) * 128], rhs=wvw[:, kc, :], start=(kc == 0), stop=(kc == KC - 1)),
                         reads=[hT, w], writes=[pg])
                s.op("dve", lambda e, pg=pg, ti=ti, cb=cb: e.scalar_tensor_tensor(out=hres[ti][:, cb * CB:(cb + 1) * CB], in0=hres[ti][:, cb * CB:(cb + 1) * CB], scalar=DN_ALPHA, in1=pg[:, 0:CB], op0=ALU.mult, op1=ALU.add),
                     reads=[pg, hres[ti]], writes=[hres[ti]])
        if upto < 3:
            for ti in range(NT):
                t0 = grp * GT + ti * 128
                s.dma("sp", hout[t0:t0 + 128, :], hres[ti][:], reads=[hres[ti]])
            continue
        for ti in range(NT):
            layer_norm(hres[ti], l1g, l1b)
            rp = bank[6]
            transpose_tile(hres[ti], ti, also_router_ps=rp)
            s.op("dve", lambda e, rp=rp: e.tensor_tensor(out=lgt[:], in0=rp[:, 0:NE], in1=rbB[:], op=ALU.add), reads=[rp, rbB], writes=[lgt])
            s.op("dve", lambda e: e.max(out=m8[:], in_=lgt[:]), reads=[lgt], writes=[m8])
            s.op("dve", lambda e: e.tensor_scalar(out=msk[:], in0=lgt[:], scalar1=m8[:, 3:4], scalar2=None, op0=ALU.is_ge), reads=[lgt, m8], writes=[msk])
            s.op("dve", lambda e: e.tensor_scalar(out=ex[:], in0=lgt[:], scalar1=m8[:, 0:1], scalar2=None, op0=ALU.subtract), reads=[lgt, m8], writes=[ex])
            s.op("act", lambda e: e.activation(out=ex[:], in_=ex[:], func=AF.Exp), reads=[ex], writes=[ex])
            s.op("dve", lambda e: e.tensor_tensor(out=ex[:], in0=ex[:], in1=msk[:], op=ALU.mult), reads=[ex, msk], writes=[ex])
            s.op("dve", lambda e: e.tensor_reduce(out=sm[:, 32:33], in_=ex[:], op=ALU.add, axis=AX.X), reads=[ex], writes=[sm])
            s.op("dve", lambda e: e.reciprocal(out=sm[:, 32:33], in_=sm[:, 32:33]), reads=[sm], writes=[sm])
            s.op("dve", lambda e, ti=ti: e.tensor_scalar(out=G[ti][:], in0=ex[:], scalar1=sm[:, 32:33], scalar2=None, op0=ALU.mult), reads=[ex, sm], writes=[G[ti]])
            tp = bank[7]
            s.op("pe", lambda e, tp=tp, ti=ti: e.transpose(tp[0:NE, 0:128], G[ti][:], ident[:]), reads=[G[ti], ident], writes=[tp])
            s.op("act", lambda e, tp=tp, ti=ti: e.activation(out=GTt[ti][:], in_=tp[0:NE, 0:128], func=AF.Copy), reads=[tp], writes=[GTt[ti]])
            for db in range(D // 512):
                pg = bank[2 + db % 4]
                s.op("pe", lambda e, pg=pg, ti=ti, db=db: e.matmul(pg[:], lhsT=GTt[ti][:], rhs=bdb[:, db * 512:(db + 1) * 512], start=True, stop=True), reads=[GTt[ti], bdb], writes=[pg])
                s.op("dve", lambda e, pg=pg, ti=ti, db=db: e.scalar_tensor_tensor(out=hres[ti][:, db * 512:(db + 1) * 512], in0=hres[ti][:, db * 512:(db + 1) * 512], scalar=DN_ALPHA, in1=pg[:], op0=ALU.mult, op1=ALU.add),
                     reads=[pg, hres[ti]], writes=[hres[ti]])
        if upto < 4:
            for ti in range(NT):
                t0 = grp * GT + ti * 128
                s.dma("sp", hout[t0:t0 + 128, :], hres[ti][:], reads=[hres[ti]])
            continue
        gc = scr[:, 0:FB * GT].rearrange("p (f t) -> p f t", t=GT)
        sg = scr[:, FB * GT:2 * FB * GT].rearrange("p (f t) -> p f t", t=GT)
        for ex_i in range(NE):
            for half in range(2):
                wvv = wgu_d[ex_i].rearrange("(kc p) n -> p kc n", p=128)
                for pc in range(KC // 8):
                    w = nextw()
                    wvw = w[:, 0:8 * F].rearrange("p (kc n) -> p kc n", n=F)
                    s.dma("pool", wvw, wvv[:, pc * 8:(pc + 1) * 8, half * F:(half + 1) * F], writes=[w])
                    for k8 in range(8):
                        kc = pc * 8 + k8
                        for fb in range(FB):
                            s.op("pe", lambda e, fb=fb, kc=kc, k8=k8, wvw=wvw: e.matmul(bank[fb][:, 0:GT], lhsT=wvw[:, k8, fb * 128:(fb + 1) * 128], rhs=hT[:, kc, :], start=(kc == 0), stop=(kc == KC - 1)),
                                 reads=[w, hT], writes=[bank[fb]])
                for fb in range(FB):
                    bcol = bg[:, ex_i, half * FB + fb: half * FB + fb + 1]
                    if half == 0:
                        s.op("dve", lambda e, fb=fb, bcol=bcol: e.tensor_scalar(out=gc[:, fb, :], in0=bank[fb][:, 0:GT], scalar1=bcol, scalar2=LIMIT, op0=ALU.add, op1=ALU.min), reads=[bank[fb], bg], writes=[(scr, ("g", fb))])
                        s.op("act", lambda e, fb=fb: e.activation(out=sg[:, fb, :], in_=gc[:, fb, :], func=AF.Sigmoid, scale=SALPHA), reads=[(scr, ("g", fb))], writes=[(scr, ("s", fb))])
                    else:
                        s.op("dve", lambda e, fb=fb, bcol=bcol: e.tensor_scalar(out=tmpA[:, 0:GT], in0=bank[fb][:, 0:GT], scalar1=bcol, scalar2=LIMIT + 1.0, op0=ALU.add, op1=ALU.min), reads=[bank[fb], bg], writes=[tmpA])
                        s.op("dve", lambda e, fb=fb: e.scalar_tensor_tensor(out=tmpB[:, 0:GT], in0=tmpA[:, 0:GT], scalar=1.0 - LIMIT, in1=gc[:, fb, :], op0=ALU.max, op1=ALU.mult), reads=[tmpA, (scr, ("g", fb))], writes=[tmpB])
                        s.op("pool", lambda e, fb=fb: e.tensor_tensor(out=actT[:, fb, :], in0=tmpB[:, 0:GT], in1=sg[:, fb, :], op=ALU.mult), reads=[tmpB, (scr, ("s", fb))], writes=[(actT, fb)])
            wdv = wd_d[ex_i].rearrange("(fb p) n -> p fb n", p=128)
            for db in range(D // 1024):
                w = nextw()
                wvw = w[:, 0:FB * 1024].rearrange("p (fb n) -> p fb n", n=1024)
                s.dma("pool", wvw, wdv[:, :, db * 1024:(db + 1) * 1024], writes=[w])
                for hb in range(2):
                    for ti in range(NT):
                        pg = bank[6 + (hb * NT + ti) % 2]
                        for fb in range(FB):
                            s.op("pe", lambda e, pg=pg, fb=fb, ti=ti, wvw=wvw, hb=hb: e.matmul(pg[:], lhsT=actT[:, fb, ti * 128:(ti + 1) * 128], rhs=wvw[:, fb, hb * 512:(hb + 1) * 512], start=(fb == 0), stop=(fb == FB - 1)),
                                 reads=[actT, w], writes=[pg])
                        cs = slice(db * 1024 + hb * 512, db * 1024 + (hb + 1) * 512)
                        s.op("dve", lambda e, pg=pg, ti=ti, cs=cs, ex_i=ex_i: e.scalar_tensor_tensor(out=hres[ti][:, cs], in0=pg[:], scalar=G[ti][:, ex_i:ex_i + 1], in1=hres[ti][:, cs], op0=ALU.mult, op1=ALU.add),
                             reads=[pg, G[ti], hres[ti]], writes=[hres[ti]])
        if upto < 5:
            for ti in range(NT):
                t0 = grp * GT + ti * 128
                s.dma("sp", hout[t0:t0 + 128, :], hres[ti][:], reads=[hres[ti]])
            continue
        for ti in range(NT):
            t0 = grp * GT + ti * 128
            layer_norm(hres[ti], l2g, l2b)
            s.dma("sp", hout[t0:t0 + 128, :], hres[ti][:], reads=[hres[ti]])
    s.finish()
    return nc

import numpy as np

A_HEADS, C_HEADS = 12, 6
SC128 = 128.0 ** -0.5
RET_THETA = 10000.0
ROPE_THETA = 500000.0

def ret_lg():
    return np.log(1.0 - np.exp(np.linspace(np.log(1.0 / 32), np.log(1.0 / 512), C_HEADS))).astype(np.float64)

def rope_cs(T, n_rot, theta):
    inv = 1.0 / (theta ** (np.arange(0, n_rot, 2, dtype=np.float32) / n_rot))
    ang = np.arange(T, dtype=np.float32)[:, None] * inv[None, :].astype(np.float32)
    return np.cos(ang).astype(np.float32), np.sin(ang).astype(np.float32)

def lb1_consts(T, TB=1024):
    cos, sin = rope_cs(T, 128, RET_THETA)
    ropeC = np.concatenate([cos.T, cos.T], 0).astype(np.float32)
    ropeS = np.concatenate([-sin.T, sin.T], 0).astype(np.float32)
    rmask = np.ones((128, TB), np.float32); rmask[:, ::64] = 0.0
    return dict(ropeC=np.ascontiguousarray(ropeC), ropeS=np.ascontiguousarray(ropeS), rmask=rmask, identb=np.eye(128, dtype=np.float32))

def lb1_core_inputs(c, layer, aq, af, ai, cq, ck, cv, hgrn_lb, consts):
    T = aq.shape[0]
    lg = ret_lg()
    pos = np.arange(64, dtype=np.float64)
    a_q = np.empty((3, 128, T), np.float32); a_f = np.empty((3, 128, T), np.float32)
    c_q = np.empty((3, 128, T), np.float32); c_qs = np.empty((3, 128, T), np.float32)
    c_k = np.empty((3, 128, T), np.float32); c_ks = np.empty((3, 128, T), np.float32)
    v6 = np.empty((6, T, 64), np.float32)
    lbraw = np.empty((128, 6), np.float32)
    qdec = np.empty((128, 3, 64), np.float32); kdec = np.empty((128, 3, 64), np.float32); cdec = np.empty((128, 3), np.float32)
    mask6 = np.zeros((64, 6, 64), np.float32)
    tri = (pos[None, :] >= pos[:, None])
    for j in range(3):
        uid = 3 * c + j
        hd, half = uid // 2, uid % 2
        a_q[j] = aq[:, hd * 128:(hd + 1) * 128].T
        a_f[j] = af[:, hd * 128:(hd + 1) * 128].T
        v6[j] = ai[:, hd * 128 + half * 64: hd * 128 + half * 64 + 64]
        lbraw[:, j] = hgrn_lb[0, hd * 128:(hd + 1) * 128]
        lbraw[:, 3 + j] = hgrn_lb[1, hd * 128:(hd + 1) * 128]
        mask6[:, j, :] = tri
        hd, qt = uid // 4, uid % 4
        qq = cq[:, hd * 128:(hd + 1) * 128].T; kk = ck[:, hd * 128:(hd + 1) * 128].T
        c_q[j] = qq; c_qs[j] = np.concatenate([qq[64:], qq[:64]], 0)
        c_k[j] = kk; c_ks[j] = np.concatenate([kk[64:], kk[:64]], 0)
        v6[3 + j] = cv[:, hd * 256 + qt * 64: hd * 256 + qt * 64 + 64]
        qdec[:, j, :] = np.exp(lg[hd] * (pos + 1.0))[None, :]
        kdec[:, j, :] = (np.exp(lg[hd] * (63.0 - pos)) * SC128)[None, :]
        cdec[:, j] = np.exp(lg[hd] * 64.0)
        mask6[:, 3 + j, :] = np.where(tri, np.exp(lg[hd] * (pos[None, :] - pos[:, None])), 0.0)
    d = dict(a_q=a_q, a_f=a_f, a_lb=np.full((128, 6), float(layer), np.float32), a_lbraw=lbraw, c_q=c_q, c_qs=c_qs, c_k=c_k, c_ks=c_ks,
             v6=v6, qdec=qdec, kdec=kdec, cdec=cdec, mask6=mask6)
    d.update(consts)
    return d

def lb1_gather(results, T):
    oa = np.empty((T, 1536), np.float32); oc = np.empty((T, 1536), np.float32)
    for c, r in enumerate(results):
        o6 = r["o6"].reshape(T, 6, 64)
        for j in range(3):
            uid = 3 * c + j
            hd, half = uid // 2, uid % 2
            oa[:, hd * 128 + half * 64: hd * 128 + half * 64 + 64] = o6[:, j]
            hd, qt = uid // 4, uid % 4
            oc[:, hd * 256 + qt * 64: hd * 256 + qt * 64 + 64] = o6[:, 3 + j]
    return oa, oc

def lb2_shared(bk, bv, ik):
    T = bk.shape[0]
    NKT = T // 128
    cb, sb_ = rope_cs(T, 32, ROPE_THETA)
    ci, si = rope_cs(T, 16, ROPE_THETA)
    kTt = np.ascontiguousarray(bk.reshape(NKT, 128, 8, 128).transpose(0, 3, 2, 1))
    perm32 = np.r_[16:32, 0:16]
    ksw = np.ascontiguousarray(kTt[:, perm32])
    C32 = np.concatenate([cb, cb], 1); S32 = np.concatenate([-sb_, sb_], 1)
    Ck = np.ascontiguousarray(C32.reshape(NKT, 128, 32).transpose(0, 2, 1)); Sk = np.ascontiguousarray(S32.reshape(NKT, 128, 32).transpose(0, 2, 1))
    Vt = np.zeros((NKT, 128, 8, 132), np.float32); Vt[..., :128] = bv.reshape(NKT, 128, 8, 128); Vt[..., 128] = 1.0
    kiT = np.ascontiguousarray(ik.T)
    perm16 = np.r_[8:16, 0:8]
    kisw = np.ascontiguousarray(kiT[perm16])
    C16 = np.concatenate([ci, ci], 1); S16 = np.concatenate([-si, si], 1)
    krsw = np.ascontiguousarray(np.stack([kTt[:, 0:32], ksw], 2))
    CSk = np.ascontiguousarray(np.stack([Ck, Sk], 2))
    return dict(kTt=kTt, krsw=krsw, CSk=CSk, Vt=Vt, kiT=kiT, kisw=kisw, Cki=np.ascontiguousarray(C16.T), Ski=np.ascontiguousarray(S16.T),
                identb=np.eye(128, dtype=np.float32)), (C32, S32, C16, S16)

def lb2_core_inputs(c, NQ, bq, iq, iw, shared, tabs):
    C32, S32, C16, S16 = tabs
    tiles = [8 * i + c for i in range(NQ)]
    rows = np.concatenate([np.arange(j * 128, (j + 1) * 128) for j in tiles])
    perm32 = np.r_[16:32, 0:16]; perm16 = np.r_[8:16, 0:8]
    qT = np.ascontiguousarray(bq[rows].reshape(NQ, 128, 8, 128).transpose(0, 3, 2, 1))
    qsw = np.ascontiguousarray(qT[:, perm32])
    Cq = np.ascontiguousarray(C32[rows].reshape(NQ, 128, 32).transpose(0, 2, 1)); Sq = np.ascontiguousarray(S32[rows].reshape(NQ, 128, 32).transpose(0, 2, 1))
    qiT = np.ascontiguousarray(iq[rows].reshape(NQ, 128, 16, 64).transpose(0, 3, 2, 1))
    qisw = np.ascontiguousarray(qiT[:, perm16])
    Cqi = np.ascontiguousarray(C16[rows].reshape(NQ, 128, 16).transpose(0, 2, 1)); Sqi = np.ascontiguousarray(S16[rows].reshape(NQ, 128, 16).transpose(0, 2, 1))
    iwc = np.ascontiguousarray(iw[rows].reshape(NQ, 128, 16))
    r = np.arange(128)[:, None]; z = np.arange(1024)[None, :]
    vis = (z < 128 * c + 64 * (r // 64 + 1)).astype(np.float32)
    pen = np.where(vis > 0, np.float32(3.0e38), np.float32(-2.0e30)).astype(np.float32)
    d = dict(qT=qT, qsw=qsw, Cq=Cq, Sq=Sq, qiT=qiT, qisw=qisw, Cqi=Cqi, Sqi=Sqi, iw=iwc, pen=pen)
    d.update(shared)
    return d

def lb2_gather(results, T, NQ):
    ob = np.empty((T, 1024), np.float32)
    for c, r in enumerate(results):
        for i in range(NQ):
            j = 8 * i + c
            ob[j * 128:(j + 1) * 128] = r["ob"][i]
    return ob

from concourse.bass_utils import run_bass_kernel_spmd

_CACHE = {}

def _prog(key, fn):
    if key not in _CACHE:
        _CACHE[key] = fn()
    return _CACHE[key]

PROJ_SIZES = (1536, 1536, 1536, 1536, 1024, 1024, 1024, 1024, 64, 16, 768, 768, 1536, 1536)


def kernel(x, ln_in_g, ln_in_b, w_in, w_out, hgrn_lb, hgrn_norm_g, ret_norm_g, ln1_g, ln1_b,
           router_w, router_b, w_gate_up, b_gate_up, w_down, b_down, ln2_g, ln2_b):
    f32 = lambda a: np.ascontiguousarray(np.asarray(a, dtype=np.float32))
    x = f32(x)[0]
    T, D = x.shape
    L = w_in.shape[0]
    NPROJ = w_in.shape[2]
    eye = np.eye(128, dtype=np.float32)
    offs = np.cumsum((0,) + PROJ_SIZES)
    h = None
    NCQ = 4
    NCOL = NPROJ // NCQ
    TH2 = T // 2
    consts1 = lb1_consts(T)
    LCN = 8
    TcC = T // LCN
    for l in range(L):
        do_ln = (l == 0)
        ncA = _prog(("la", do_ln), lambda: build_la(TH2, D, NCOL, do_ln))
        src = x if do_ln else h
        wl = f32(w_in[l])
        ims = []
        for c in range(8):
            th, cq = c // NCQ, c % NCQ
            d = {"x": np.ascontiguousarray(src[th * TH2:(th + 1) * TH2]), "w": np.ascontiguousarray(wl[:, cq * NCOL:(cq + 1) * NCOL]), "ident": eye}
            if do_ln:
                d["g"] = f32(ln_in_g); d["b"] = f32(ln_in_b)
            ims.append(d)
        res = run_bass_kernel_spmd(ncA, ims, core_ids=list(range(8))).results
        del ims, wl
        p = np.empty((T, NPROJ), np.float32)
        for c in range(8):
            th, cq = c // NCQ, c % NCQ
            p[th * TH2:(th + 1) * TH2, cq * NCOL:(cq + 1) * NCOL] = res[c]["p"]
        if do_ln:
            h = np.concatenate([res[0]["hout"], res[NCQ]["hout"]], 0)
        del res
        aq, af, ai, ag, bq, bk, bv, iq, ik, iw, cq_, ck, cv, cg = [np.ascontiguousarray(p[:, offs[i]:offs[i + 1]]) for i in range(14)]
        del p
        ncB1 = _prog(("lb1", T), lambda: build_lb1(T))
        hl = f32(hgrn_lb)
        ims = [lb1_core_inputs(c, l, aq, af, ai, cq_, ck, cv, hl, consts1) for c in range(8)]
        res = run_bass_kernel_spmd(ncB1, ims, core_ids=list(range(8))).results
        del ims
        oa_raw, oc_raw = lb1_gather(res, T)
        del res, aq, af, ai, cq_, ck, cv
        NQ = T // 128 // 8
        ncB2 = _prog(("lb2", T), lambda: build_lb2(T, NQ))
        shared, tabs = lb2_shared(bk, bv, ik)
        ims = [lb2_core_inputs(c, NQ, bq, iq, iw, shared, tabs) for c in range(8)]
        res = run_bass_kernel_spmd(ncB2, ims, core_ids=list(range(8))).results
        del ims, shared
        ob = lb2_gather(res, T, NQ)
        del res, bq, bk, bv, iq, ik, iw
        ncC = _prog(("lc", TcC), lambda: build_lc(TcC))
        NE = router_w.shape[2]
        bguT = np.ascontiguousarray(f32(b_gate_up[l]).reshape(NE, 12, 128).transpose(2, 0, 1))
        com = dict(gA=f32(hgrn_norm_g[l]).reshape(-1), gC=f32(ret_norm_g[l]).reshape(-1), wout=f32(w_out[l]), l1g=f32(ln1_g[l]), l1b=f32(ln1_b[l]),
                   l2g=f32(ln2_g[l]), l2b=f32(ln2_b[l]), rw=f32(router_w[l]), rb=f32(router_b[l]), wgu=f32(w_gate_up[l]), bguT=bguT,
                   wd=f32(w_down[l]), bd=f32(b_down[l]), ident=eye)
        ims = []
        for c in range(LCN):
            sl = slice(c * TcC, (c + 1) * TcC)
            d = dict(h=np.ascontiguousarray(h[sl]), oa=np.ascontiguousarray(oa_raw[sl]), ag=np.ascontiguousarray(ag[sl]), ob=np.ascontiguousarray(ob[sl]),
                     oc=np.ascontiguousarray(oc_raw[sl]), cg=np.ascontiguousarray(cg[sl]))
            d.update(com)
            ims.append(d)
        res = run_bass_kernel_spmd(ncC, ims, core_ids=list(range(LCN))).results
        del ims, com
        h = np.concatenate([r["hout"] for r in res], 0)
        del res, oa_raw, oc_raw, ob, ag, cg
    return h[None].astype(np.float32)
```

```python
import contextlib
import numpy as np
import concourse.bass as bass
import concourse.mybir as mybir

F32 = mybir.dt.float32
BF16 = mybir.dt.bfloat16
I32 = mybir.dt.int32
AF = mybir.ActivationFunctionType
ALU = mybir.AluOpType
AX = mybir.AxisListType


class _St:
    __slots__ = ("w", "r")

    def __init__(self):
        self.w = None
        self.r = {}


class T:
    def __init__(self, s, t, name):
        self.s = s
        self.t = t
        self.name = name
        self.st = {"*": _St()}
        self.dsem = None
        self.dcnt = 0

    def __getitem__(self, idx):
        return self.t[idx]

    def states(self, key):
        if key is None:
            return list(self.st.values())
        if key not in self.st:
            n = _St()
            n.w = self.st["*"].w
            n.r = dict(self.st["*"].r)
            self.st[key] = n
        return [self.st[key], self.st["*"]]


class Sched:
    ENG = ("pe", "act", "dve", "pool", "sp")

    def __init__(self, nc):
        self.nc = nc
        self.es = contextlib.ExitStack()
        self.prog = {e: [] for e in self.ENG}
        self.cnt = {e: 0 for e in self.ENG}
        self.sem = {}
        self.waited = {e: {} for e in self.ENG}
        self.semobj = {}
        for e in self.ENG:
            self.semobj[e] = self.es.enter_context(nc.semaphore("s_" + e))
        self.ntiles = 0
        self.downer = {}
        self.scope = None
        self.out_tiles = []

    def sb(self, shape, dtype, name):
        t = self.es.enter_context(self.nc.sbuf_tensor(name, list(shape), dtype))
        return T(self, t, name)

    def ps(self, shape, dtype, name):
        t = self.es.enter_context(self.nc.psum_tensor(name, list(shape), dtype))
        return T(self, t, name)

    def _dsem(self, tl):
        if tl.dsem is None:
            key = "d_" + tl.name
            tl.dsem = key
            self.semobj[key] = self.es.enter_context(self.nc.semaphore(key))
            self.downer[key] = tl
        return tl.dsem

    @staticmethod
    def _norm(lst):
        out = []
        for x in lst:
            if isinstance(x, tuple):
                out.append(x)
            else:
                out.append((x, None))
        return out

    def _deps(self, eng, reads, writes):
        deps = {}

        def add(tk):
            if tk is None:
                return
            k, v = tk
            if deps.get(k, 0) < v:
                deps[k] = v

        for tl, key in reads:
            for st in (tl.states(key) if key is not None else tl.states(None)):
                add(st.w)
        for tl, key in writes:
            for st in (tl.states(key) if key is not None else tl.states(None)):
                add(st.w)
                for k, v in st.r.items():
                    add((k, v))
        waits = []
        wd = self.waited[eng]
        for k, v in deps.items():
            if k == eng and eng in ("pe", "sp"):
                continue
            if k in self.downer:
                v = self.downer[k].dcnt
            if wd.get(k, 0) >= v:
                continue
            wd[k] = v
            waits.append((k, v))
        return waits

    def _commit(self, reads, writes, tk):
        k, v = tk
        for tl, key in reads:
            sts = tl.states(key) if key is not None else tl.states(None)
            if key is not None:
                sts = [tl.st[key]]
            for st in sts:
                if st.r.get(k, 0) < v:
                    st.r[k] = v
        for tl, key in writes:
            if key is None:
                tl.st = {"*": _St()}
                tl.st["*"].w = tk
            else:
                st = tl.states(key)[0]
                st.w = tk
                st.r = {}

    def op(self, eng, fn, reads=(), writes=()):
        reads = self._norm(reads)
        writes = self._norm(writes)
        waits = self._deps(eng, reads, writes)
        self.cnt[eng] += 1
        tk = (eng, self.cnt[eng])
        semobj = self.semobj
        so = semobj[eng]

        scope = self.scope
        nc = self.nc

        def run(e, waits=waits, fn=fn, so=so):
            for k, v in waits:
                e.wait_ge(semobj[k], v)
            if scope is None:
                fn(e).then_inc(so, 1)
            else:
                with nc.named_scope(scope):
                    fn(e).then_inc(so, 1)

        self.prog[eng].append(run)
        self._commit(reads, writes, tk)

    def dma(self, q, out, in_, reads=(), writes=(), **kw):
        reads = self._norm(reads)
        writes = self._norm(writes)
        waits = self._deps(q, reads, writes)
        owner = (writes[0][0] if writes else reads[0][0])
        dk = self._dsem(owner)
        owner.dcnt += 16
        tk = (dk, owner.dcnt)
        semobj = self.semobj

        def run(e, waits=waits):
            for k, v in waits:
                e.wait_ge(semobj[k], v)
            e.dma_start(out=out, in_=in_, **kw).then_inc(semobj[dk], 16)

        self.prog[q].append(run)
        self._commit(reads, writes, tk)
        if not writes:
            self.out_tiles.append(owner)

    def collective(self, kind, in_ap, out_ap, reads=(), writes=(), groups=None):
        reads = self._norm(reads)
        writes = self._norm(writes)
        waits = self._deps("pool", reads, writes)
        owner = writes[0][0]
        dk = self._dsem(owner)
        owner.dcnt += 16
        tk = (dk, owner.dcnt)
        semobj = self.semobj
        if groups is None:
            groups = [list(range(8))]

        def run(e, waits=waits):
            for k, v in waits:
                e.wait_ge(semobj[k], v)
            e.collective_compute(kind, (mybir.AluOpType.add if kind in ('AllReduce', 'ReduceScatter') else mybir.AluOpType.bypass), replica_groups=groups, ins=[in_ap], outs=[out_ap]).then_inc(semobj[dk], 16)

        self.prog["pool"].append(run)
        self._commit(reads, writes, tk)

    def finish(self):
        waits = []
        seen = set()
        for tl in self.out_tiles:
            if tl.dsem in seen:
                continue
            seen.add(tl.dsem)
            waits.append((tl.dsem, tl.dcnt))
        semobj = self.semobj

        def run(e):
            for k, v in waits:
                e.wait_ge(semobj[k], v)

        self.prog["sp"].append(run)
        nc = self.nc
        prog = self.prog
        with nc.Block() as block:
            @block.sync
            def _(e):
                for f in prog["sp"]:
                    f(e)

            @block.tensor
            def _(e):
                for f in prog["pe"]:
                    f(e)

            @block.scalar
            def _(e):
                for f in prog["act"]:
                    f(e)

            @block.vector
            def _(e):
                for f in prog["dve"]:
                    f(e)

            @block.gpsimd
            def _(e):
                for f in prog["pool"]:
                    f(e)
        self.es.close()


LN_EPS = 1e-5

def emit_ln_rows(s, xt, P, D, eps, tmp_prefix, stats, mv, rstd, epsT):
    nch = D // 512
    for c in range(nch):
        s.op("dve", lambda e, c=c: e.bn_stats(out=stats[:, c, :], in_=xt[:, c * 512:(c + 1) * 512]),
             reads=[xt], writes=[(stats, c)])
    s.op("dve", lambda e: e.bn_aggr(out=mv[:], in_=stats[:].rearrange("p c s -> p (c s)")), reads=[stats], writes=[mv])
    s.op("act", lambda e: e.activation(out=rstd[:], in_=mv[:, 1:2], func=AF.Sqrt, bias=epsT[:, 0:1], scale=1.0),
         reads=[mv, epsT], writes=[rstd])
    s.op("dve", lambda e: e.reciprocal(out=rstd[:], in_=rstd[:]), reads=[rstd], writes=[rstd])
    s.op("dve", lambda e: e.tensor_scalar(out=xt[:], in0=xt[:], scalar1=mv[:, 0:1], scalar2=rstd[:, 0:1],
                                          op0=ALU.subtract, op1=ALU.mult), reads=[xt, mv, rstd], writes=[xt])


def build_la(Tc, K, N, do_ln):
    nc = bass.Bass("TRN2", target_bir_lowering=False)
    x = nc.dram_tensor("x", [Tc, K], F32, kind="ExternalInput").ap()
    w = nc.dram_tensor("w", [K, N], F32, kind="ExternalInput").ap()
    ident_d = nc.dram_tensor("ident", [128, 128], F32, kind="ExternalInput").ap()
    if do_ln:
        g = nc.dram_tensor("g", [K], F32, kind="ExternalInput").ap()
        b = nc.dram_tensor("b", [K], F32, kind="ExternalInput").ap()
        hout = nc.dram_tensor("hout", [Tc, K], F32, kind="ExternalOutput").ap()
    p = nc.dram_tensor("p", [Tc, N], F32, kind="ExternalOutput").ap()
    s = Sched(nc)
    KC = K // 128
    TH = min(Tc, 1024)
    NTH = TH // 128
    NB = 256
    ident = s.sb([128, 128], F32, "identT")
    s.dma("sp", ident[:], ident_d, writes=[ident])
    if do_ln:
        gB = s.sb([128, K], F32, "gB"); bB = s.sb([128, K], F32, "bB")
        s.dma("sp", gB[:], g.partition_broadcast(128), writes=[gB])
        s.dma("sp", bB[:], b.partition_broadcast(128), writes=[bB])
        stats = s.sb([128, K // 512, 6], F32, "stats"); mv = s.sb([128, 2], F32, "mv"); rstd = s.sb([128, 1], F32, "rstd")
        epsT = s.sb([128, 1], F32, "epsT")
        s.op("dve", lambda e: e.memset(epsT[:], LN_EPS), writes=[epsT])
    xts = [s.sb([128, K], F32, "xt%d" % i) for i in range(2)]
    hT = s.sb([128, KC, TH], BF16, "hT")
    pst = [s.ps([128, 512], F32, "pst%d" % i) for i in range(2)]
    psg = [s.ps([128, 512], F32, "psg%d" % i) for i in range(4)]
    wbs = [s.sb([128, KC, NB], BF16, "wb%d" % i) for i in range(2)]
    outs = [s.sb([128, NB], F32, "ot%d" % i) for i in range(4)]
    nblocks = (N + NB - 1) // NB
    wv = w.rearrange("(kc p) n -> p kc n", p=128)
    tcount = 0
    wcount = 0
    ocount = 0
    for half in range(Tc // TH):
        for ti in range(NTH):
            t0 = half * TH + ti * 128
            xt = xts[tcount % 2]
            s.dma("sp", xt[:], x[t0:t0 + 128, :], writes=[xt])
            if do_ln:
                emit_ln_rows(s, xt, 128, K, LN_EPS, "ln", stats, mv, rstd, epsT)
                s.op("pool", lambda e, xt=xt: e.tensor_tensor(out=xt[:], in0=xt[:], in1=gB[:], op=ALU.mult), reads=[xt, gB], writes=[xt])
                s.op("dve", lambda e, xt=xt: e.tensor_tensor(out=xt[:], in0=xt[:], in1=bB[:], op=ALU.add), reads=[xt, bB], writes=[xt])
                s.dma("sp", hout[t0:t0 + 128, :], xt[:], reads=[xt])
            for k4 in range(KC // 4):
                pt = pst[k4 % 2]
                for j in range(4):
                    kc = k4 * 4 + j
                    s.op("pe", lambda e, pt=pt, j=j, kc=kc, xt=xt: e.transpose(pt[:, j * 128:(j + 1) * 128], xt[:, kc * 128:(kc + 1) * 128], ident[:]),
                         reads=[xt, ident], writes=[(pt, j)])
                eng = "act" if k4 % 2 == 0 else "dve"
                dst = hT[:, k4 * 4:(k4 + 1) * 4, ti * 128:(ti + 1) * 128]
                src = pt[:].rearrange("p (j t) -> p j t", j=4)
                if eng == "act":
                    s.op("act", lambda e, dst=dst, src=src: e.activation(out=dst, in_=src, func=AF.Copy), reads=[pt], writes=[(hT, (ti, k4))])
                else:
                    s.op("dve", lambda e, dst=dst, src=src: e.tensor_copy(out=dst, in_=src), reads=[pt], writes=[(hT, (ti, k4))])
            tcount += 1
        for nb in range(nblocks):
            n0 = nb * NB
            nw = min(NB, N - n0)
            wb = wbs[wcount % 2]; wcount += 1
            for q4 in range(KC // 8):
                s.dma("pool", wb[:, q4 * 8:(q4 + 1) * 8, 0:nw], wv[:, q4 * 8:(q4 + 1) * 8, n0:n0 + nw], writes=[(wb, q4)])
            for ti in range(NTH):
                t0 = half * TH + ti * 128
                pg = psg[ocount % 4]; ot = outs[ocount % 4]; ocount += 1
                for kc in range(KC):
                    s.op("pe", lambda e, pg=pg, kc=kc, wb=wb, ti=ti, nw=nw: e.matmul(pg[:, 0:nw], lhsT=hT[:, kc, ti * 128:(ti + 1) * 128], rhs=wb[:, kc, 0:nw], start=(kc == 0), stop=(kc == KC - 1)),
                         reads=[hT, wb], writes=[pg])
                if ocount % 2 == 0:
                    s.op("act", lambda e, pg=pg, ot=ot, nw=nw: e.activation(out=ot[:, 0:nw], in_=pg[:, 0:nw], func=AF.Copy), reads=[pg], writes=[ot])
                else:
                    s.op("dve", lambda e, pg=pg, ot=ot, nw=nw: e.tensor_copy(out=ot[:, 0:nw], in_=pg[:, 0:nw]), reads=[pg], writes=[ot])
                s.dma("sp", p[t0:t0 + 128, n0:n0 + nw], ot[:, 0:nw], reads=[ot])
    s.finish()
    return nc


def build_lb1(T, TB=1024):
    nc = bass.Bass("TRN2", target_bir_lowering=False)
    C = 64
    NCH = TB // C
    NBLK = T // TB
    dt = lambda n, sh: nc.dram_tensor(n, sh, F32, kind="ExternalInput").ap()
    a_q = dt("a_q", [3, 128, T]); a_f = dt("a_f", [3, 128, T]); a_lb = dt("a_lb", [128, 6])
    a_lbraw = dt("a_lbraw", [128, 6])
    c_q = dt("c_q", [3, 128, T]); c_qs = dt("c_qs", [3, 128, T]); c_k = dt("c_k", [3, 128, T]); c_ks = dt("c_ks", [3, 128, T])
    v6 = dt("v6", [6, T, 64])
    ropeC = dt("ropeC", [128, T]); ropeS = dt("ropeS", [128, T])
    rmask_d = dt("rmask", [128, TB])
    qdec_d = dt("qdec", [128, 3, 64]); kdec_d = dt("kdec", [128, 3, 64]); cdec_d = dt("cdec", [128, 3])
    mask6_d = dt("mask6", [64, 6, 64])
    identb_d = dt("identb", [128, 128])
    o6 = nc.dram_tensor("o6", [T // C, C, 6 * 64], F32, kind="ExternalOutput").ap()
    s = Sched(nc)
    SC = 128.0 ** -0.5
    rmask = s.sb([128, TB], F32, "rmaskT"); s.dma("sp", rmask[:], rmask_d, writes=[rmask])
    qdec = s.sb([128, 3, 64], F32, "qdecT"); s.dma("sp", qdec[:], qdec_d, writes=[qdec])
    kdec = s.sb([128, 3, 64], F32, "kdecT"); s.dma("sp", kdec[:], kdec_d, writes=[kdec])
    cdec = s.sb([128, 3], F32, "cdecT"); s.dma("sp", cdec[:], cdec_d, writes=[cdec])
    mask6 = s.sb([64, 6, 64], F32, "mask6T"); s.dma("sp", mask6[:], mask6_d, writes=[mask6])
    identb = s.sb([128, 128], BF16, "identbT"); s.dma("pool", identb[:], identb_d, writes=[identb])
    lbr = s.sb([128, 6], F32, "lbr"); s.dma("sp", lbr[:], a_lbraw, writes=[lbr])
    lbm = s.sb([128, 6], F32, "lbm"); s.dma("sp", lbm[:], a_lb, writes=[lbm])
    lb = s.sb([128, 3], F32, "lb"); oml = s.sb([128, 3], F32, "oml")
    s.op("dve", lambda e: e.tensor_tensor(out=lb[:], in0=lbr[:, 3:6], in1=lbr[:, 0:3], op=ALU.subtract), reads=[lbr], writes=[lb])
    s.op("act", lambda e: e.activation(out=lb[:], in_=lb[:], func=AF.Sigmoid), reads=[lb], writes=[lb])
    s.op("dve", lambda e: e.tensor_tensor(out=lb[:], in0=lb[:], in1=lbm[:, 0:3], op=ALU.mult), reads=[lb, lbm], writes=[lb])
    s.op("dve", lambda e: e.tensor_scalar(out=oml[:], in0=lb[:], scalar1=-1.0, scalar2=1.0, op0=ALU.mult, op1=ALU.add), reads=[lb], writes=[oml])
    S = s.sb([128, 6, 64], F32, "S"); Sb = s.sb([128, 6, 64], BF16, "Sb")
    s.op("dve", lambda e: e.memset(S[:], 0.0), writes=[S])
    s.op("pool", lambda e: e.memset(Sb[:], 0.0), writes=[Sb])
    d6 = s.sb([128, NCH, 6], F32, "d6")
    Qt = [s.sb([128, TB], BF16, "Qt%d" % u) for u in range(6)]
    Kt = [s.sb([128, TB], BF16, "Kt%d" % u) for u in range(6)]
    Qi = [s.sb([128, TB], BF16, "Qi%d" % u) for u in range(6)]
    Kd = [s.sb([64, NCH, 128], BF16, "Kd%d" % u) for u in range(6)]
    Vb = [s.sb([64, NCH, 64], BF16, "Vb%d" % u) for u in range(6)]
    KdT = s.sb([128, TB], BF16, "KdT")
    sc = [s.sb([128, TB], F32, "sc%d" % i) for i in range(6)]
    rC = s.sb([128, TB], F32, "rC"); rS = s.sb([128, TB], F32, "rS")
    obuf = s.sb([64, NCH, 6 * 64], F32, "obuf")
    attm = [s.sb([64, 6, 64], BF16, "attm%d" % i) for i in range(2)]
    att_ps = [s.ps([64, 512], F32, "attps%d" % i) for i in range(2)]
    o_ps = [s.ps([64, 512], F32, "ops%d" % i) for i in range(2)]
    U_ps = [s.ps([128, 512], F32, "Ups%d" % i) for i in range(2)]
    tr_ps = [s.ps([64, 1024], BF16, "trps%d" % i) for i in range(2)]
    trc = 0

    def ch3(t):
        return t[:].rearrange("p (n c) -> p n c", c=C)

    for blk in range(NBLK):
        tsl = slice(blk * TB, (blk + 1) * TB)
        s.dma("sp", rC[:], ropeC[:, tsl], writes=[rC]); s.dma("sp", rS[:], ropeS[:, tsl], writes=[rS])
        for u in range(6):
            s.dma("pool", Vb[u][:], v6[u, tsl, :].rearrange("(n c) v -> c n v", c=C), writes=[Vb[u]])
            if u < 3:
                q, f, t2, t3, t4, t5 = sc
                s.dma("sp", q[:], a_q[u, :, tsl], writes=[q]); s.dma("sp", f[:], a_f[u, :, tsl], writes=[f])
                s.op("act", lambda e, f=f: e.activation(out=f[:], in_=f[:], func=AF.Sigmoid), reads=[f], writes=[f])
                s.op("dve", lambda e, f=f, u=u: e.tensor_scalar(out=f[:], in0=f[:], scalar1=oml[:, u:u + 1], scalar2=lb[:, u:u + 1], op0=ALU.mult, op1=ALU.add), reads=[f, oml, lb], writes=[f])
                lf = t2
                s.op("act", lambda e, f=f, lf=lf: e.activation(out=lf[:], in_=f[:], func=AF.Ln), reads=[f], writes=[lf])
                kk = f
                s.op("pool", lambda e, f=f: e.tensor_scalar(out=f[:], in0=f[:], scalar1=-1.0, scalar2=1.0, op0=ALU.mult, op1=ALU.add), reads=[f], writes=[f])
                cum = t3
                s.op("dve", lambda e, cum=cum, lf=lf: e.tensor_tensor_scan(out=cum[:], data0=rmask[:], data1=lf[:], initial=0.0, op0=ALU.mult, op1=ALU.add), reads=[rmask, lf], writes=[cum])
                cum3 = ch3(cum)
                mid = cum3[:, :, 31:32].to_broadcast([128, NCH, C])
                last = cum3[:, :, C - 1:C].to_broadcast([128, NCH, C])
                dm = t2
                s.op("pool", lambda e, dm=dm, cum3=cum3, mid=mid: e.tensor_tensor(out=ch3(dm), in0=cum3, in1=mid, op=ALU.subtract), reads=[cum], writes=[dm])
                e1 = t4
                s.op("act", lambda e, e1=e1, dm=dm: e.activation(out=e1[:], in_=dm[:], func=AF.Exp), reads=[dm], writes=[e1])
                s.op("dve", lambda e, u=u, q=q, e1=e1: e.scalar_tensor_tensor(out=Qt[u][:], in0=q[:], scalar=SC, in1=e1[:], op0=ALU.mult, op1=ALU.mult), reads=[q, e1], writes=[Qt[u]])
                e2 = t5
                s.op("act", lambda e, e2=e2, dm=dm: e.activation(out=e2[:], in_=dm[:], func=AF.Exp, scale=-1.0), reads=[dm], writes=[e2])
                s.op("pool", lambda e, u=u, kk=kk, e2=e2: e.tensor_tensor(out=Kt[u][:], in0=kk[:], in1=e2[:], op=ALU.mult), reads=[kk, e2], writes=[Kt[u]])
                e3 = t4
                s.op("act", lambda e, e3=e3, cum=cum: e.activation(out=e3[:], in_=cum[:], func=AF.Exp), reads=[cum], writes=[e3])
                s.op("dve", lambda e, u=u, q=q, e3=e3: e.scalar_tensor_tensor(out=Qi[u][:], in0=q[:], scalar=SC, in1=e3[:], op0=ALU.mult, op1=ALU.mult), reads=[q, e3], writes=[Qi[u]])
                dl = t2
                s.op("pool", lambda e, dl=dl, cum3=cum3, last=last: e.tensor_tensor(out=ch3(dl), in0=last, in1=cum3, op=ALU.subtract), reads=[cum], writes=[dl])
                e4 = t5
                s.op("act", lambda e, e4=e4, dl=dl: e.activation(out=e4[:], in_=dl[:], func=AF.Exp), reads=[dl], writes=[e4])
                s.op("pool", lambda e, kk=kk, e4=e4: e.tensor_tensor(out=KdT[:], in0=kk[:], in1=e4[:], op=ALU.mult), reads=[kk, e4], writes=[KdT])
                s.op("act", lambda e, u=u, cum3=cum3: e.activation(out=d6[:, :, u:u + 1], in_=cum3[:, :, C - 1:C], func=AF.Exp), reads=[cum], writes=[(d6, u)])
            else:
                r = u - 3
                q, qs, k, ks, t4, t5 = sc
                s.dma("sp", q[:], c_q[r, :, tsl], writes=[q]); s.dma("sp", qs[:], c_qs[r, :, tsl], writes=[qs])
                s.dma("sp", k[:], c_k[r, :, tsl], writes=[k]); s.dma("sp", ks[:], c_ks[r, :, tsl], writes=[ks])
                s.op("dve", lambda e, q=q: e.tensor_tensor(out=q[:], in0=q[:], in1=rC[:], op=ALU.mult), reads=[q, rC], writes=[q])
                s.op("pool", lambda e, qs=qs: e.tensor_tensor(out=qs[:], in0=qs[:], in1=rS[:], op=ALU.mult), reads=[qs, rS], writes=[qs])
                s.op("dve", lambda e, q=q, qs=qs: e.tensor_tensor(out=q[:], in0=q[:], in1=qs[:], op=ALU.add), reads=[q, qs], writes=[q])
                s.op("act", lambda e, u=u, q=q: e.activation(out=Qt[u][:], in_=q[:], func=AF.Copy), reads=[q], writes=[Qt[u]])
                qd = qdec[:, r:r + 1, :].to_broadcast([128, NCH, C])
                s.op("pool", lambda e, u=u, q=q, qd=qd: e.tensor_tensor(out=ch3(Qi[u]), in0=ch3(q), in1=qd, op=ALU.mult), reads=[q, qdec], writes=[Qi[u]])
                s.op("dve", lambda e, k=k: e.tensor_tensor(out=k[:], in0=k[:], in1=rC[:], op=ALU.mult), reads=[k, rC], writes=[k])
                s.op("pool", lambda e, ks=ks: e.tensor_tensor(out=ks[:], in0=ks[:], in1=rS[:], op=ALU.mult), reads=[ks, rS], writes=[ks])
                s.op("dve", lambda e, k=k, ks=ks: e.tensor_tensor(out=k[:], in0=k[:], in1=ks[:], op=ALU.add), reads=[k, ks], writes=[k])
                s.op("act", lambda e, u=u, k=k: e.activation(out=Kt[u][:], in_=k[:], func=AF.Copy, scale=SC), reads=[k], writes=[Kt[u]])
                kd = kdec[:, r:r + 1, :].to_broadcast([128, NCH, C])
                s.op("pool", lambda e, k=k, kd=kd: e.tensor_tensor(out=ch3(KdT), in0=ch3(k), in1=kd, op=ALU.mult), reads=[k, kdec], writes=[KdT])
                cd = cdec[:, r:r + 1].to_broadcast([128, NCH])
                s.op("dve", lambda e, u=u, cd=cd: e.tensor_copy(out=d6[:, :, u], in_=cd), reads=[cdec], writes=[(d6, u)])
            for g in range(NCH // 8):
                tp = tr_ps[trc % 2]; trc += 1
                for j in range(8):
                    n = g * 8 + j
                    s.op("pe", lambda e, tp=tp, j=j, n=n: e.transpose(tp[:, j * 128:(j + 1) * 128], KdT[:, n * C:(n + 1) * C], identb[:]),
                         reads=[KdT, identb], writes=[(tp, j)])
                s.op("act", lambda e, tp=tp, g=g, u=u: e.activation(out=Kd[u][:, g * 8:(g + 1) * 8, :], in_=tp[:].rearrange("p (j k) -> p j k", j=8), func=AF.Copy),
                     reads=[tp], writes=[(Kd[u], g)])
        for n in range(NCH):
            gi = blk * NCH + n
            ap_ = att_ps[gi % 2]; op_ = o_ps[gi % 2]; up_ = U_ps[gi % 2]; am = attm[gi % 2]
            csl = slice(n * C, (n + 1) * C)
            for u in range(6):
                s.op("pe", lambda e, u=u, ap_=ap_, csl=csl: e.matmul(ap_[:, u * 64:(u + 1) * 64], lhsT=Kt[u][:, csl], rhs=Qt[u][:, csl], start=True, stop=True),
                     reads=[Kt[u], Qt[u]], writes=[(ap_, u)])
            s.op("dve", lambda e, ap_=ap_, am=am: e.tensor_tensor(out=am[:].rearrange("p u t -> p (u t)"), in0=ap_[:, 0:384], in1=mask6[:].rearrange("p u t -> p (u t)"), op=ALU.mult),
                 reads=[ap_, mask6], writes=[am])
            for u in range(6):
                s.op("pe", lambda e, u=u, up_=up_, n=n: e.matmul(up_[:, u * 64:(u + 1) * 64], lhsT=Kd[u][:, n, :], rhs=Vb[u][:, n, :], start=True, stop=True),
                     reads=[Kd[u], Vb[u]], writes=[(up_, u)])
            for u in range(6):
                s.op("pe", lambda e, u=u, op_=op_, am=am, n=n: e.matmul(op_[:, u * 64:(u + 1) * 64], lhsT=am[:, u, :], rhs=Vb[u][:, n, :], start=True, stop=False),
                     reads=[am, Vb[u]], writes=[(op_, u)])
                s.op("pe", lambda e, u=u, op_=op_, csl=csl: e.matmul(op_[:, u * 64:(u + 1) * 64], lhsT=Qi[u][:, csl], rhs=Sb[:, u, :], start=False, stop=True),
                     reads=[Qi[u], Sb], writes=[(op_, u)])
            s.op("act", lambda e, op_=op_, n=n: e.activation(out=obuf[:, n, :], in_=op_[:, 0:384], func=AF.Copy), reads=[op_], writes=[(obuf, n)])
            dB = d6[:, n, :].unsqueeze(2).to_broadcast([128, 6, 64])
            s.op("dve", lambda e, dB=dB: e.tensor_tensor(out=S[:], in0=S[:], in1=dB, op=ALU.mult), reads=[S, d6], writes=[S])
            s.op("dve", lambda e, up_=up_: e.tensor_tensor(out=S[:].rearrange("p u v -> p (u v)"), in0=S[:].rearrange("p u v -> p (u v)"), in1=up_[:, 0:384], op=ALU.add), reads=[S, up_], writes=[S])
            s.op("act", lambda e: e.activation(out=Sb[:], in_=S[:], func=AF.Copy), reads=[S], writes=[Sb])
        s.dma("sp", o6[blk * NCH:(blk + 1) * NCH].rearrange("n t u -> t n u"), obuf[:], reads=[obuf])
    s.finish()
    return nc


NEG1 = -1.0e30

def build_lb2(T, NQ, debug=False):
    nc = bass.Bass("TRN2", target_bir_lowering=False)
    NKT = T // 128
    dt = lambda n, sh: nc.dram_tensor(n, sh, F32, kind="ExternalInput").ap()
    qT = dt("qT", [NQ, 128, 8, 128]); qsw = dt("qsw", [NQ, 32, 8, 128]); Cq = dt("Cq", [NQ, 32, 128]); Sq = dt("Sq", [NQ, 32, 128])
    kTt = dt("kTt", [NKT, 128, 8, 128]); krsw = dt("krsw", [NKT, 32, 2, 8, 128]); CSk = dt("CSk", [NKT, 32, 2, 128])
    Vt = dt("Vt", [NKT, 128, 8, 132])
    qiT = dt("qiT", [NQ, 64, 16, 128]); qisw = dt("qisw", [NQ, 16, 16, 128]); Cqi = dt("Cqi", [NQ, 16, 128]); Sqi = dt("Sqi", [NQ, 16, 128])
    kiT = dt("kiT", [64, T]); kisw = dt("kisw", [16, T]); Cki = dt("Cki", [16, T]); Ski = dt("Ski", [16, T])
    iw = dt("iw", [NQ, 128, 16])
    pen_d = dt("pen", [128, 1024])
    identb_d = dt("identb", [128, 128])
    ob = nc.dram_tensor("ob", [NQ, 128, 1024], F32, kind="ExternalOutput").ap()
    NMAX = 1024 * NQ
    if debug:
        dbg1 = nc.dram_tensor("dbg1", [NQ, 128, NMAX], F32, kind="ExternalOutput").ap()
        dbg2 = nc.dram_tensor("dbg2", [NQ, 128, NMAX], F32, kind="ExternalOutput").ap()
        dbg3 = nc.dram_tensor("dbg3", [NQ, 128, 4, 258], F32, kind="ExternalOutput").ap()
    s = Sched(nc)
    SC = 128.0 ** -0.5
    assert NMAX <= T
    pen = s.sb([128, 1024], F32, "penT"); s.dma("sp", pen[:], pen_d, writes=[pen])
    identb = s.sb([128, 128], BF16, "identbT"); s.dma("pool", identb[:], identb_d, writes=[identb])
    kiTb = s.sb([64, NMAX], BF16, "kiTb")
    PW = min(512, NMAX)
    stg = s.sb([64, PW], F32, "stg"); stg2 = s.sb([16, 3, PW], F32, "stg2"); stg3 = s.sb([16, 2, PW], F32, "stg3")
    for pc in range(NMAX // PW):
        sl = slice(pc * PW, (pc + 1) * PW)
        s.dma("sp", stg[:], kiT[:, sl], writes=[stg])
        s.dma("sp", stg2[:, 0, :], kisw[:, sl], writes=[(stg2, 0)])
        s.dma("sp", stg2[:, 1, :], Cki[:, sl], writes=[(stg2, 1)])
        s.dma("sp", stg2[:, 2, :], Ski[:, sl], writes=[(stg2, 2)])
        s.op("dve", lambda e: e.tensor_tensor(out=stg3[:, 0, :], in0=stg[0:16, :], in1=stg2[:, 1, :], op=ALU.mult), reads=[stg, stg2], writes=[(stg3, 0)])
        s.op("pool", lambda e: e.tensor_tensor(out=stg3[:, 1, :], in0=stg2[:, 0, :], in1=stg2[:, 2, :], op=ALU.mult), reads=[stg2], writes=[(stg3, 1)])
        s.op("dve", lambda e: e.tensor_tensor(out=stg[0:16, :], in0=stg3[:, 0, :], in1=stg3[:, 1, :], op=ALU.add), reads=[stg3], writes=[stg])
        s.op("act", lambda e, sl=sl: e.activation(out=kiTb[:, sl], in_=stg[:], func=AF.Copy), reads=[stg], writes=[(kiTb, pc)])
    work = s.sb([128, NMAX], F32, "work")
    qf = s.sb([128, 8, 128], F32, "qf"); cq_t = s.sb([32, 2, 128], F32, "cq_t")
    qb = s.sb([128, 8, 128], BF16, "qb")
    qif = s.sb([64, 16, 128], F32, "qif"); cqi_t = s.sb([16, 2, 128], F32, "cqi_t")
    qib = s.sb([64, 16, 128], BF16, "qib")
    rt1 = s.sb([32, 16, 128], F32, "rt1"); rt2 = s.sb([32, 16, 128], F32, "rt2")
    iwt = s.sb([128, 16], F32, "iwt")
    rl = [s.sb([128, 512], F32, "rl%d" % i) for i in range(3)]
    m8 = s.sb([128, 8], F32, "m8")
    mT = [s.sb([128, 128], BF16, "mT%d" % i) for i in range(3)]
    kb16 = [s.sb([128, 8, 128], BF16, "kb16_%d" % i) for i in range(4)]
    krs0 = s.sb([32, 2, 8, 128], F32, "krs0")
    krs_tiles = [krs0, rt1, rt2]
    krs_views = [krs0[:], rt1[:].rearrange("p (a h) q -> p a h q", a=2), rt2[:].rearrange("p (a h) q -> p a h q", a=2)]
    ck_t = [s.sb([32, 2, 128], F32, "ckt%d" % i) for i in range(3)]
    pexp = None
    vb = [s.sb([128, 8, 132], BF16, "vb%d" % i) for i in range(4)]
    pexp = [s.sb([128, 4, 128], BF16, "pexp%d" % i) for i in range(6)]
    pm = [s.sb([128, 4, 128], BF16, "pm%d" % i) for i in range(6)]
    otile = s.sb([128, 8, 128], F32, "otile"); rec = s.sb([128, 8, 1], F32, "rec")
    psA = [s.ps([128, 512], F32, "psA%d" % i) for i in range(3)]
    oacc = [s.ps([128, 512], F32, "oacc%d" % i) for i in range(4)]
    psT = s.ps([128, 3, 128], F32, "psT")
    identf = s.sb([128, 128], F32, "identfT"); s.dma("sp", identf[:], identb_d, writes=[identf])
    pac = 0
    pairc = 0
    zl = s.sb([128, 128], BF16, "zl"); zr = s.sb([128, 512], BF16, "zr")
    s.op("pool", lambda e: e.memset(zl[:], 0.0), writes=[zl])
    s.op("pool", lambda e: e.memset(zr[:], 0.0), writes=[zr])

    def bc(t2d, n):
        return t2d.unsqueeze(1).to_broadcast([t2d.shape[0], n, 128])

    for i in range(NQ):
        N = 1024 * (i + 1)
        s.scope = 'prep%d' % i
        s.dma("sp", qf[:], qT[i], writes=[qf]); s.dma("sp", rt2[:, 0:8, :], qsw[i], writes=[rt2])
        s.dma("sp", cq_t[:, 0, :], Cq[i], writes=[(cq_t, 0)]); s.dma("sp", cq_t[:, 1, :], Sq[i], writes=[(cq_t, 1)])
        s.op("dve", lambda e: e.tensor_tensor(out=rt1[:, 0:8, :], in0=qf[0:32], in1=bc(cq_t[:, 0, :], 8), op=ALU.mult), reads=[qf, cq_t], writes=[rt1])
        s.op("pool", lambda e: e.tensor_tensor(out=rt2[:, 0:8, :], in0=rt2[:, 0:8, :], in1=bc(cq_t[:, 1, :], 8), op=ALU.mult), reads=[rt2, cq_t], writes=[rt2])
        s.op("dve", lambda e: e.tensor_tensor(out=qf[0:32], in0=rt1[:, 0:8, :], in1=rt2[:, 0:8, :], op=ALU.add), reads=[rt1, rt2], writes=[qf])
        s.op("act", lambda e: e.activation(out=qb[:], in_=qf[:], func=AF.Copy), reads=[qf], writes=[qb])
        s.dma("sp", qif[:], qiT[i], writes=[qif]); s.dma("sp", rt2[0:16], qisw[i], writes=[rt2])
        s.dma("sp", cqi_t[:, 0, :], Cqi[i], writes=[(cqi_t, 0)]); s.dma("sp", cqi_t[:, 1, :], Sqi[i], writes=[(cqi_t, 1)])
        s.op("dve", lambda e: e.tensor_tensor(out=rt1[0:16], in0=qif[0:16], in1=bc(cqi_t[:, 0, :], 16), op=ALU.mult), reads=[qif, cqi_t], writes=[rt1])
        s.op("pool", lambda e: e.tensor_tensor(out=rt2[0:16], in0=rt2[0:16], in1=bc(cqi_t[:, 1, :], 16), op=ALU.mult), reads=[rt2, cqi_t], writes=[rt2])
        s.op("dve", lambda e: e.tensor_tensor(out=qif[0:16], in0=rt1[0:16], in1=rt2[0:16], op=ALU.add), reads=[rt1, rt2], writes=[qif])
        s.op("act", lambda e: e.activation(out=qib[:], in_=qif[:], func=AF.Copy), reads=[qif], writes=[qib])
        s.dma("sp", iwt[:], iw[i], writes=[iwt])
        s.scope = 'idx%d' % i
        for kb in range(N // 512):
            ksl = slice(kb * 512, (kb + 1) * 512)
            for h in range(16):
                pa = psA[pac % 3]; r_ = rl[pac % 3]; pac += 1
                s.op("pe", lambda e, pa=pa, h=h, ksl=ksl: e.matmul(pa[:], lhsT=qib[:, h, :], rhs=kiTb[:, ksl], start=True, stop=True), reads=[qib, kiTb], writes=[pa])
                s.op("act", lambda e, pa=pa, r_=r_: e.activation(out=r_[:], in_=pa[:], func=AF.Relu), reads=[pa], writes=[r_])
                if h == 0:
                    s.op("dve", lambda e, r_=r_, ksl=ksl: e.tensor_scalar(out=work[:, ksl], in0=r_[:], scalar1=iwt[:, 0:1], scalar2=None, op0=ALU.mult), reads=[r_, iwt], writes=[(work, kb)])
                else:
                    s.op("dve", lambda e, r_=r_, ksl=ksl, h=h: e.scalar_tensor_tensor(out=work[:, ksl], in0=r_[:], scalar=iwt[:, h:h + 1], in1=work[:, ksl], op0=ALU.mult, op1=ALU.add), reads=[r_, iwt, (work, kb)], writes=[(work, kb)])
        zs = slice(N - 1024, N)
        s.op("dve", lambda e, zs=zs: e.tensor_tensor(out=work[:, zs], in0=work[:, zs], in1=pen[:], op=ALU.min), reads=[work, pen], writes=[work])
        if debug:
            s.dma("sp", dbg1[i, :, 0:N], work[:, 0:N], reads=[work])
        s.scope = 'topk%d' % i
        for r in range(32):
            s.op("dve", lambda e, N=N: e.max(out=m8[:], in_=work[:, 0:N]), reads=[work], writes=[m8])
            s.op("dve", lambda e, N=N: e.match_replace(out=work[:, 0:N], in_to_replace=m8[:], in_values=work[:, 0:N], imm_value=NEG1), reads=[work, m8], writes=[work])
        if debug:
            s.dma("sp", dbg2[i, :, 0:N], work[:, 0:N], reads=[work])
        s.op("pool", lambda e, N=N: e.tensor_scalar(out=work[:, 0:N], in0=work[:, 0:N], scalar1=NEG1, scalar2=None, op0=ALU.is_equal), reads=[work], writes=[work])
        s.scope = 'attn%d' % i
        nkt = N // 128
        for b in range(4):
            s.op("pe", lambda e, b=b: e.matmul(oacc[b][:], lhsT=zl[:], rhs=zr[:], start=True, stop=True), reads=[zl, zr], writes=[oacc[b]])
        def stageA1(kt):
            pp = kt % 3
            kb_ = kb16[kt % 4]; kt_ = krs_tiles[pp]; kv_ = krs_views[pp]; ck_ = ck_t[pp]; vb_ = vb[kt % 4]
            s.dma("pool", kb_[:].rearrange("p h k -> p (h k)"), kTt[kt].rearrange("p h k -> p (h k)"), writes=[kb_])
            s.dma("sp", kv_.rearrange("p a h k -> p (a h k)"), krsw[kt].rearrange("p a h k -> p (a h k)"), writes=[kt_])
            s.dma("sp", ck_[:], CSk[kt], writes=[ck_])
            s.dma("pool", vb_[:].rearrange("p h k -> p (h k)"), Vt[kt].rearrange("p h k -> p (h k)"), writes=[vb_])

        def stageA2(kt):
            nonlocal pac
            pp = kt % 3
            mt = mT[pp]
            s.op("pe", lambda e, kt=kt, pp=pp: e.transpose(psT[:, pp, :], work[:, kt * 128:(kt + 1) * 128], identf[:]), reads=[work, identf], writes=[(psT, pp)])
            s.op("act", lambda e, mt=mt, pp=pp: e.activation(out=mt[:], in_=psT[:, pp, :], func=AF.Copy), reads=[(psT, pp)], writes=[mt])
            kb_ = kb16[kt % 4]; kt_ = krs_tiles[pp]; kv_ = krs_views[pp]; ck_ = ck_t[pp]
            csb = ck_[:].unsqueeze(2).to_broadcast([32, 2, 8, 128])
            s.op("dve", lambda e, kv_=kv_, csb=csb: e.tensor_tensor(out=kv_, in0=kv_, in1=csb, op=ALU.mult), reads=[kt_, ck_], writes=[kt_])
            s.op("dve", lambda e, kb_=kb_, kv_=kv_: e.tensor_tensor(out=kb_[0:32], in0=kv_[:, 0], in1=kv_[:, 1], op=ALU.add), reads=[kt_], writes=[kb_])
            for g in range(2):
                pa = psA[pac % 3]; pac += 1
                pe_ = pexp[pp * 2 + g]; pm_ = pm[(pp * 2 + g)]
                for hh in range(4):
                    h = 4 * g + hh
                    s.op("pe", lambda e, pa=pa, hh=hh, h=h, kb_=kb_: e.matmul(pa[:, hh * 128:(hh + 1) * 128], lhsT=kb_[:, h, :], rhs=qb[:, h, :], start=True, stop=True), reads=[kb_, qb], writes=[(pa, hh)])
                s.op("act", lambda e, pa=pa, pe_=pe_: e.activation(out=pe_[:].rearrange("p h q -> p (h q)"), in_=pa[:], func=AF.Exp, scale=SC), reads=[pa], writes=[pe_])
                s.op("pool", lambda e, pe_=pe_, pm_=pm_, mt=mt: e.tensor_tensor(out=pm_[:], in0=pe_[:], in1=bc(mt[:], 4), op=ALU.mult), reads=[pe_, mt], writes=[pm_])

        def stageB(kt):
            pp = kt % 3
            vb_ = vb[kt % 4]
            for h in range(8):
                pm_ = pm[pp * 2 + h // 4]
                oa = oacc[h // 2]
                s.op("pe", lambda e, oa=oa, h=h, pm_=pm_, vb_=vb_: e.matmul(oa[:, (h % 2) * 129:(h % 2) * 129 + 129], lhsT=pm_[:, h % 4, :], rhs=vb_[:, h, 0:129], start=False, stop=True),
                     reads=[pm_, vb_], writes=[(oa, h % 2)])

        stageA1(0)
        if nkt > 1:
            stageA1(1)
        for kt in range(nkt):
            stageA2(kt)
            if kt > 0:
                stageB(kt - 1)
            if kt + 2 < nkt:
                stageA1(kt + 2)
        stageB(nkt - 1)
        if debug:
            dbt = s.sb([128, 4, 258], F32, "dbt%d" % i)
            for b in range(4):
                s.op("dve", lambda e, b=b, dbt=dbt: e.tensor_copy(out=dbt[:, b, :], in_=oacc[b][:, 0:258]), reads=[oacc[b]], writes=[(dbt, b)])
            s.dma("sp", dbg3[i], dbt[:], reads=[dbt])
        for b in range(4):
            ov = oacc[b][:, 0:258].rearrange("p (h c) -> p h c", h=2)
            s.op("dve", lambda e, ov=ov, b=b: e.reciprocal(out=rec[:, 2 * b:2 * b + 2, :], in_=ov[:, :, 128:129]), reads=[oacc[b]], writes=[(rec, b)])
            s.op("dve", lambda e, ov=ov, b=b: e.tensor_tensor(out=otile[:, 2 * b:2 * b + 2, :], in0=ov[:, :, 0:128], in1=rec[:, 2 * b:2 * b + 2, :].to_broadcast([128, 2, 128]), op=ALU.mult), reads=[oacc[b], (rec, b)], writes=[(otile, b)])
        s.dma("sp", ob[i].rearrange("p (h d) -> p h d", h=8), otile[:], reads=[otile])
    s.finish()
    return nc


LN_EPS = 1e-5
NORM_EPS = 1e-6
DN_ALPHA = 4 ** 0.25
LIMIT = 7.0
SALPHA = 1.702

def build_lc(Tc, NE=32, D=4096, F=768, upto=5):
    nc = bass.Bass("TRN2", target_bir_lowering=False)
    dt = lambda n, sh: nc.dram_tensor(n, sh, F32, kind="ExternalInput").ap()
    KC = D // 128
    FB = F // 128
    h_d = dt("h", [Tc, D]); oa_d = dt("oa", [Tc, 1536]); ag_d = dt("ag", [Tc, 1536]); ob_d = dt("ob", [Tc, 1024])
    oc_d = dt("oc", [Tc, 1536]); cg_d = dt("cg", [Tc, 1536]); gA_d = dt("gA", [1536]); gC_d = dt("gC", [1536])
    wout_d = dt("wout", [D, D]); l1g = dt("l1g", [D]); l1b = dt("l1b", [D]); l2g = dt("l2g", [D]); l2b = dt("l2b", [D])
    rw_d = dt("rw", [D, NE]); rb_d = dt("rb", [NE])
    NEW = NE if upto >= 4 else 1
    wgu_d = dt("wgu", [NEW, D, 2 * F]); bgu_d = dt("bguT", [128, NE, 2 * FB]); wd_d = dt("wd", [NEW, F, D]); bd_d = dt("bd", [NE, D])
    ident_d = dt("ident", [128, 128])
    hout = nc.dram_tensor("hout", [Tc, D], F32, kind="ExternalOutput").ap()
    s = Sched(nc)
    GT = 256
    NT = GT // 128
    ident = s.sb([128, 128], F32, "identT"); s.dma("sp", ident[:], ident_d, writes=[ident])
    epsT = s.sb([128, 2], F32, "epsT")
    s.op("dve", lambda e: e.memset(epsT[:, 0:1], LN_EPS), writes=[(epsT, 0)])
    s.op("dve", lambda e: e.memset(epsT[:, 1:2], NORM_EPS), writes=[(epsT, 1)])
    rw = s.sb([128, KC, NE], F32, "rwT"); s.dma("sp", rw[:], rw_d.rearrange("(kc p) e -> p kc e", p=128), writes=[rw])
    rwh = s.sb([128, KC, NE], BF16, "rwh"); rwl = s.sb([128, KC, NE], BF16, "rwl")
    s.op("act", lambda e: e.activation(out=rwh[:], in_=rw[:], func=AF.Copy), reads=[rw], writes=[rwh])
    s.op("dve", lambda e: e.tensor_tensor(out=rwl[:], in0=rw[:], in1=rwh[:], op=ALU.subtract), reads=[rw, rwh], writes=[rwl])
    sc4l = s.sb([128, 4, 128], BF16, "sc4l")
    rbB = s.sb([128, NE], F32, "rbB"); s.dma("sp", rbB[:], rb_d.partition_broadcast(128), writes=[rbB])
    bg = s.sb([128, NE, 2 * FB], F32, "bgT"); s.dma("sp", bg[:], bgu_d, writes=[bg])
    s.op("dve", lambda e: e.tensor_scalar(out=bg[:, :, FB:2 * FB], in0=bg[:, :, FB:2 * FB], scalar1=1.0, scalar2=None, op0=ALU.add), reads=[bg], writes=[bg])
    bdb = s.sb([NE, D], BF16, "bdb"); s.dma("pool", bdb[:], bd_d, writes=[bdb])
    hres = [s.sb([128, D], F32, "hres%d" % i) for i in range(NT)]
    hT = s.sb([128, KC, GT], BF16, "hT")
    scr = s.sb([128, 4096], F32, "scr")
    lnb = s.sb([128, 2, D], F32, "lnb")
    WP = 6144
    wpool = [s.sb([128, WP], BF16, "wp%d" % i) for i in range(3)]
    wc = [0]
    actT = s.sb([128, FB, GT], BF16, "actT")
    tmpA = s.sb([128, 512], F32, "tmpA"); tmpB = s.sb([128, 512], F32, "tmpB")
    stats = s.sb([128, D // 512, 6], F32, "stats"); mv = s.sb([128, 2], F32, "mv"); rstd = s.sb([128, 1], F32, "rstd")
    sm = s.sb([128, 64], F32, "sm")
    G = [s.sb([128, NE], F32, "G%d" % i) for i in range(NT)]
    GTt = [s.sb([NE, 128], BF16, "GTt%d" % i) for i in range(NT)]
    lgt = s.sb([128, NE], F32, "lgt"); m8 = s.sb([128, 8], F32, "m8"); ex = s.sb([128, NE], F32, "ex"); msk = s.sb([128, NE], F32, "msk")
    sc4 = s.sb([128, 4, 128], F32, "sc4")
    bank = [s.ps([128, 512], F32, "bank%d" % i) for i in range(8)]

    def nextw():
        w = wpool[wc[0] % 3]; wc[0] += 1
        return w

    def transpose_tile(src, ti, also_router_ps=None):
        for k4 in range(KC // 4):
            pt = bank[k4 % 2]
            for j in range(4):
                kc = k4 * 4 + j
                s.op("pe", lambda e, pt=pt, j=j, kc=kc: e.transpose(pt[:, j * 128:(j + 1) * 128], src[:, kc * 128:(kc + 1) * 128], ident[:]),
                     reads=[src, ident], writes=[(pt, j)])
            dst = hT[:, k4 * 4:(k4 + 1) * 4, ti * 128:(ti + 1) * 128]
            srcv = pt[:].rearrange("p (j t) -> p j t", j=4)
            s.op("act", lambda e, dst=dst, srcv=srcv: e.activation(out=dst, in_=srcv, func=AF.Copy), reads=[pt], writes=[(hT, (ti, k4))])
            if also_router_ps is not None:
                s.op("dve", lambda e, srcv=srcv, dst=dst: e.tensor_tensor(out=sc4l[:], in0=srcv, in1=dst, op=ALU.subtract), reads=[pt, (hT, (ti, k4))], writes=[sc4l])
                for j in range(4):
                    kc = k4 * 4 + j
                    hi = hT[:, kc, ti * 128:(ti + 1) * 128]
                    s.op("pe", lambda e, hi=hi, kc=kc: e.matmul(also_router_ps[:, 0:NE], lhsT=hi, rhs=rwh[:, kc, :], start=(kc == 0), stop=False),
                         reads=[hT, rwh], writes=[also_router_ps])
                    s.op("pe", lambda e, j=j, kc=kc: e.matmul(also_router_ps[:, 0:NE], lhsT=sc4l[:, j, :], rhs=rwh[:, kc, :], start=False, stop=False),
                         reads=[sc4l, rwh], writes=[also_router_ps])
                    s.op("pe", lambda e, hi=hi, kc=kc: e.matmul(also_router_ps[:, 0:NE], lhsT=hi, rhs=rwl[:, kc, :], start=False, stop=(kc == KC - 1)),
                         reads=[hT, rwl], writes=[also_router_ps])

    def layer_norm(xt, gd, bd_):
        s.dma("sp", lnb[:, 0, :], gd.partition_broadcast(128), writes=[(lnb, 0)])
        s.dma("sp", lnb[:, 1, :], bd_.partition_broadcast(128), writes=[(lnb, 1)])
        emit_ln_rows(s, xt, 128, D, LN_EPS, "ln", stats, mv, rstd, epsT)
        s.op("pool", lambda e: e.tensor_tensor(out=xt[:], in0=xt[:], in1=lnb[:, 0, :], op=ALU.mult), reads=[xt, lnb], writes=[xt])
        s.op("dve", lambda e: e.tensor_tensor(out=xt[:], in0=xt[:], in1=lnb[:, 1, :], op=ALU.add), reads=[xt, lnb], writes=[xt])

    for grp in range(Tc // GT):
        for ti in range(NT):
            t0 = grp * GT + ti * 128
            rows = slice(t0, t0 + 128)
            s.dma("sp", hres[ti][:], h_d[rows, :], writes=[hres[ti]])
            mx = scr
            s.dma("sp", mx[:, 0:1536], oa_d[rows, :], writes=[(mx, "a")])
            s.dma("sp", mx[:, 1536:2560], ob_d[rows, :], writes=[(mx, "b")])
            s.dma("sp", mx[:, 2560:4096], oc_d[rows, :], writes=[(mx, "c")])
            gt = lnb[:, 0, 0:3072]; gn = lnb[:, 1, 0:3072]
            s.dma("sp", gt[:, 0:1536], ag_d[rows, :], writes=[(lnb, 0)]); s.dma("sp", gt[:, 1536:3072], cg_d[rows, :], writes=[(lnb, 0)])
            s.dma("sp", gn[:, 0:1536], gA_d.partition_broadcast(128), writes=[(lnb, 1)]); s.dma("sp", gn[:, 1536:3072], gC_d.partition_broadcast(128), writes=[(lnb, 1)])
            s.op("act", lambda e, gt=gt: e.activation(out=gt, in_=gt, func=AF.Silu), reads=[(lnb, 0)], writes=[(lnb, 0)])
            s.op("pool", lambda e, gt=gt, gn=gn: e.tensor_tensor(out=gt, in0=gt, in1=gn, op=ALU.mult), reads=[lnb], writes=[(lnb, 0)])
            xa = mx[:, 0:1536].rearrange("p (h v) -> p h v", v=128)
            sq = lnb[:, 1, 0:1536].rearrange("p (h v) -> p h v", v=128)
            s.op("dve", lambda e, xa=xa, sq=sq: e.tensor_tensor(out=sq, in0=xa, in1=xa, op=ALU.mult), reads=[(mx, "a"), lnb], writes=[(lnb, 1)])
            s.op("dve", lambda e, sq=sq: e.tensor_reduce(out=sm[:, 0:12], in_=sq, op=ALU.add, axis=AX.X), reads=[(lnb, 1)], writes=[sm])
            s.op("act", lambda e: e.activation(out=sm[:, 0:12], in_=sm[:, 0:12], func=AF.Sqrt, bias=epsT[:, 1:2], scale=1.0 / 128), reads=[sm, epsT], writes=[sm])
            s.op("dve", lambda e: e.reciprocal(out=sm[:, 0:12], in_=sm[:, 0:12]), reads=[sm], writes=[sm])
            s.op("dve", lambda e, xa=xa: e.tensor_tensor(out=xa, in0=xa, in1=sm[:, 0:12].unsqueeze(2).to_broadcast([128, 12, 128]), op=ALU.mult), reads=[(mx, "a"), sm], writes=[(mx, "a")])
            xc = mx[:, 2560:4096].rearrange("p (h v) -> p h v", v=256)
            sq2 = lnb[:, 1, 0:1536].rearrange("p (h v) -> p h v", v=256)
            s.op("dve", lambda e, xc=xc: e.tensor_reduce(out=sm[:, 16:22], in_=xc, op=ALU.add, axis=AX.X), reads=[(mx, "c")], writes=[sm])
            s.op("dve", lambda e: e.tensor_scalar(out=sm[:, 16:22], in0=sm[:, 16:22], scalar1=1.0 / 256, scalar2=None, op0=ALU.mult), reads=[sm], writes=[sm])
            s.op("dve", lambda e, xc=xc: e.tensor_tensor(out=xc, in0=xc, in1=sm[:, 16:22].unsqueeze(2).to_broadcast([128, 6, 256]), op=ALU.subtract), reads=[(mx, "c"), sm], writes=[(mx, "c")])
            s.op("dve", lambda e, xc=xc, sq2=sq2: e.tensor_tensor(out=sq2, in0=xc, in1=xc, op=ALU.mult), reads=[(mx, "c"), lnb], writes=[(lnb, 1)])
            s.op("dve", lambda e, sq2=sq2: e.tensor_reduce(out=sm[:, 24:30], in_=sq2, op=ALU.add, axis=AX.X), reads=[(lnb, 1)], writes=[sm])
            s.op("act", lambda e: e.activation(out=sm[:, 24:30], in_=sm[:, 24:30], func=AF.Sqrt, bias=epsT[:, 1:2], scale=1.0 / 256), reads=[sm, epsT], writes=[sm])
            s.op("dve", lambda e: e.reciprocal(out=sm[:, 24:30], in_=sm[:, 24:30]), reads=[sm], writes=[sm])
            s.op("dve", lambda e, xc=xc: e.tensor_tensor(out=xc, in0=xc, in1=sm[:, 24:30].unsqueeze(2).to_broadcast([128, 6, 256]), op=ALU.mult), reads=[(mx, "c"), sm], writes=[(mx, "c")])
            s.op("pool", lambda e, gt=gt: e.tensor_tensor(out=mx[:, 0:1536], in0=mx[:, 0:1536], in1=gt[:, 0:1536], op=ALU.mult), reads=[mx, lnb], writes=[(mx, "a")])
            s.op("pool", lambda e, gt=gt: e.tensor_tensor(out=mx[:, 2560:4096], in0=mx[:, 2560:4096], in1=gt[:, 1536:3072], op=ALU.mult), reads=[mx, lnb], writes=[(mx, "c")])
            transpose_tile(mx, ti)
        CB = 128
        wv = wout_d.rearrange("(kc p) n -> p kc n", p=128)
        for cb in range(D // CB):
            w = nextw()
            wvw = w[:, 0:KC * CB].rearrange("p (kc n) -> p kc n", n=CB)
            for q4 in range(4):
                s.dma("pool", wvw[:, q4 * 8:(q4 + 1) * 8, :], wv[:, q4 * 8:(q4 + 1) * 8, cb * CB:(cb + 1) * CB], writes=[(w, q4)])
            for ti in range(NT):
                pg = bank[2 + (cb * NT + ti) % 4]
                for kc in range(KC):
                    s.op("pe", lambda e, pg=pg, kc=kc, wvw=wvw, ti=ti: e.matmul(pg[:, 0:CB], lhsT=hT[:, kc, ti * 128:(ti + 1) * 128], rhs=wvw[:, kc, :], start=(kc == 0), stop=(kc == KC - 1)),
                         reads=[hT, w], writes=[pg])
                s.op("dve", lambda e, pg=pg, ti=ti, cb=cb: e.scalar_tensor_tensor(out=hres[ti][:, cb * CB:(cb + 1) * CB], in0=hres[ti][:, cb * CB:(cb + 1) * CB], scalar=DN_ALPHA, in1=pg[:, 0:CB], op0=ALU.mult, op1=ALU.add),
                     reads=[pg, hres[ti]], writes=[hres[ti]])
        if upto < 3:
            for ti in range(NT):
                t0 = grp * GT + ti * 128
                s.dma("sp", hout[t0:t0 + 128, :], hres[ti][:], reads=[hres[ti]])
            continue
        for ti in range(NT):
            layer_norm(hres[ti], l1g, l1b)
            rp = bank[6]
            transpose_tile(hres[ti], ti, also_router_ps=rp)
            s.op("dve", lambda e, rp=rp: e.tensor_tensor(out=lgt[:], in0=rp[:, 0:NE], in1=rbB[:], op=ALU.add), reads=[rp, rbB], writes=[lgt])
            s.op("dve", lambda e: e.max(out=m8[:], in_=lgt[:]), reads=[lgt], writes=[m8])
            s.op("dve", lambda e: e.tensor_scalar(out=msk[:], in0=lgt[:], scalar1=m8[:, 3:4], scalar2=None, op0=ALU.is_ge), reads=[lgt, m8], writes=[msk])
            s.op("dve", lambda e: e.tensor_scalar(out=ex[:], in0=lgt[:], scalar1=m8[:, 0:1], scalar2=None, op0=ALU.subtract), reads=[lgt, m8], writes=[ex])
            s.op("act", lambda e: e.activation(out=ex[:], in_=ex[:], func=AF.Exp), reads=[ex], writes=[ex])
            s.op("dve", lambda e: e.tensor_tensor(out=ex[:], in0=ex[:], in1=msk[:], op=ALU.mult), reads=[ex, msk], writes=[ex])
            s.op("dve", lambda e: e.tensor_reduce(out=sm[:, 32:33], in_=ex[:], op=ALU.add, axis=AX.X), reads=[ex], writes=[sm])
            s.op("dve", lambda e: e.reciprocal(out=sm[:, 32:33], in_=sm[:, 32:33]), reads=[sm], writes=[sm])
            s.op("dve", lambda e, ti=ti: e.tensor_scalar(out=G[ti][:], in0=ex[:], scalar1=sm[:, 32:33], scalar2=None, op0=ALU.mult), reads=[ex, sm], writes=[G[ti]])
            tp = bank[7]
            s.op("pe", lambda e, tp=tp, ti=ti: e.transpose(tp[0:NE, 0:128], G[ti][:], ident[:]), reads=[G[ti], ident], writes=[tp])
            s.op("act", lambda e, tp=tp, ti=ti: e.activation(out=GTt[ti][:], in_=tp[0:NE, 0:128], func=AF.Copy), reads=[tp], writes=[GTt[ti]])
            for db in range(D // 512):
                pg = bank[2 + db % 4]
                s.op("pe", lambda e, pg=pg, ti=ti, db=db: e.matmul(pg[:], lhsT=GTt[ti][:], rhs=bdb[:, db * 512:(db + 1) * 512], start=True, stop=True), reads=[GTt[ti], bdb], writes=[pg])
                s.op("dve", lambda e, pg=pg, ti=ti, db=db: e.scalar_tensor_tensor(out=hres[ti][:, db * 512:(db + 1) * 512], in0=hres[ti][:, db * 512:(db + 1) * 512], scalar=DN_ALPHA, in1=pg[:], op0=ALU.mult, op1=ALU.add),
                     reads=[pg, hres[ti]], writes=[hres[ti]])
        if upto < 4:
            for ti in range(NT):
                t0 = grp * GT + ti * 128
                s.dma("sp", hout[t0:t0 + 128, :], hres[ti][:], reads=[hres[ti]])
            continue
        gc = scr[:, 0:FB * GT].rearrange("p (f t) -> p f t", t=GT)
        sg = scr[:, FB * GT:2 * FB * GT].rearrange("p (f t) -> p f t", t=GT)
        for ex_i in range(NE):
            for half in range(2):
                wvv = wgu_d[ex_i].rearrange("(kc p) n -> p kc n", p=128)
                for pc in range(KC // 8):
                    w = nextw()
                    wvw = w[:, 0:8 * F].rearrange("p (kc n) -> p kc n", n=F)
                    s.dma("pool", wvw, wvv[:, pc * 8:(pc + 1) * 8, half * F:(half + 1) * F], writes=[w])
                    for k8 in range(8):
                        kc = pc * 8 + k8
                        for fb in range(FB):
                            s.op("pe", lambda e, fb=fb, kc=kc, k8=k8, wvw=wvw: e.matmul(bank[fb][:, 0:GT], lhsT=wvw[:, k8, fb * 128:(fb + 1) * 128], rhs=hT[:, kc, :], start=(kc == 0), stop=(kc == KC - 1)),
                                 reads=[w, hT], writes=[bank[fb]])
                for fb in range(FB):
                    bcol = bg[:, ex_i, half * FB + fb: half * FB + fb + 1]
                    if half == 0:
                        s.op("dve", lambda e, fb=fb, bcol=bcol: e.tensor_scalar(out=gc[:, fb, :], in0=bank[fb][:, 0:GT], scalar1=bcol, scalar2=LIMIT, op0=ALU.add, op1=ALU.min), reads=[bank[fb], bg], writes=[(scr, ("g", fb))])
                        s.op("act", lambda e, fb=fb: e.activation(out=sg[:, fb, :], in_=gc[:, fb, :], func=AF.Sigmoid, scale=SALPHA), reads=[(scr, ("g", fb))], writes=[(scr, ("s", fb))])
                    else:
                        s.op("dve", lambda e, fb=fb, bcol=bcol: e.tensor_scalar(out=tmpA[:, 0:GT], in0=bank[fb][:, 0:GT], scalar1=bcol, scalar2=LIMIT + 1.0, op0=ALU.add, op1=ALU.min), reads=[bank[fb], bg], writes=[tmpA])
                        s.op("dve", lambda e, fb=fb: e.scalar_tensor_tensor(out=tmpB[:, 0:GT], in0=tmpA[:, 0:GT], scalar=1.0 - LIMIT, in1=gc[:, fb, :], op0=ALU.max, op1=ALU.mult), reads=[tmpA, (scr, ("g", fb))], writes=[tmpB])
                        s.op("dve", lambda e, fb=fb: e.tensor_tensor(out=actT[:, fb, :], in0=tmpB[:, 0:GT], in1=sg[:, fb, :], op=ALU.mult), reads=[tmpB, (scr, ("s", fb))], writes=[(actT, fb)])
            wdv = wd_d[ex_i].rearrange("(fb p) n -> p fb n", p=128)
            for db in range(D // 1024):
                w = nextw()
                wvw = w[:, 0:FB * 1024].rearrange("p (fb n) -> p fb n", n=1024)
                s.dma("pool", wvw, wdv[:, :, db * 1024:(db + 1) * 1024], writes=[w])
                for hb in range(2):
                    for ti in range(NT):
                        pg = bank[6 + (hb * NT + ti) % 2]
                        for fb in range(FB):
                            s.op("pe", lambda e, pg=pg, fb=fb, ti=ti, wvw=wvw, hb=hb: e.matmul(pg[:], lhsT=actT[:, fb, ti * 128:(ti + 1) * 128], rhs=wvw[:, fb, hb * 512:(hb + 1) * 512], start=(fb == 0), stop=(fb == FB - 1)),
                                 reads=[actT, w], writes=[pg])
                        cs = slice(db * 1024 + hb * 512, db * 1024 + (hb + 1) * 512)
                        s.op("dve", lambda e, pg=pg, ti=ti, cs=cs, ex_i=ex_i: e.scalar_tensor_tensor(out=hres[ti][:, cs], in0=pg[:], scalar=G[ti][:, ex_i:ex_i + 1], in1=hres[ti][:, cs], op0=ALU.mult, op1=ALU.add),
                             reads=[pg, G[ti], hres[ti]], writes=[hres[ti]])
        if upto < 5:
            for ti in range(NT):
                t0 = grp * GT + ti * 128
                s.dma("sp", hout[t0:t0 + 128, :], hres[ti][:], reads=[hres[ti]])
            continue
        for ti in range(NT):
            t0 = grp * GT + ti * 128
            layer_norm(hres[ti], l2g, l2b)
            s.dma("sp", hout[t0:t0 + 128, :], hres[ti][:], reads=[hres[ti]])
    s.finish()
    return nc

import numpy as np

A_HEADS, C_HEADS = 12, 6
SC128 = 128.0 ** -0.5
RET_THETA = 10000.0
ROPE_THETA = 500000.0

def ret_lg():
    return np.log(1.0 - np.exp(np.linspace(np.log(1.0 / 32), np.log(1.0 / 512), C_HEADS))).astype(np.float64)

def rope_cs(T, n_rot, theta):
    inv = 1.0 / (theta ** (np.arange(0, n_rot, 2, dtype=np.float32) / n_rot))
    ang = np.arange(T, dtype=np.float32)[:, None] * inv[None, :].astype(np.float32)
    return np.cos(ang).astype(np.float32), np.sin(ang).astype(np.float32)

def lb1_consts(T, TB=1024):
    cos, sin = rope_cs(T, 128, RET_THETA)
    ropeC = np.concatenate([cos.T, cos.T], 0).astype(np.float32)
    ropeS = np.concatenate([-sin.T, sin.T], 0).astype(np.float32)
    rmask = np.ones((128, TB), np.float32); rmask[:, ::64] = 0.0
    return dict(ropeC=np.ascontiguousarray(ropeC), ropeS=np.ascontiguousarray(ropeS), rmask=rmask, identb=np.eye(128, dtype=np.float32))

def lb1_core_inputs(c, layer, aq, af, ai, cq, ck, cv, hgrn_lb, consts):
    T = aq.shape[0]
    lg = ret_lg()
    pos = np.arange(64, dtype=np.float64)
    a_q = np.empty((3, 128, T), np.float32); a_f = np.empty((3, 128, T), np.float32)
    c_q = np.empty((3, 128, T), np.float32); c_qs = np.empty((3, 128, T), np.float32)
    c_k = np.empty((3, 128, T), np.float32); c_ks = np.empty((3, 128, T), np.float32)
    v6 = np.empty((6, T, 64), np.float32)
    lbraw = np.empty((128, 6), np.float32)
    qdec = np.empty((128, 3, 64), np.float32); kdec = np.empty((128, 3, 64), np.float32); cdec = np.empty((128, 3), np.float32)
    mask6 = np.zeros((64, 6, 64), np.float32)
    tri = (pos[None, :] >= pos[:, None])
    for j in range(3):
        uid = 3 * c + j
        hd, half = uid // 2, uid % 2
        a_q[j] = aq[:, hd * 128:(hd + 1) * 128].T
        a_f[j] = af[:, hd * 128:(hd + 1) * 128].T
        v6[j] = ai[:, hd * 128 + half * 64: hd * 128 + half * 64 + 64]
        lbraw[:, j] = hgrn_lb[0, hd * 128:(hd + 1) * 128]
        lbraw[:, 3 + j] = hgrn_lb[1, hd * 128:(hd + 1) * 128]
        mask6[:, j, :] = tri
        hd, qt = uid // 4, uid % 4
        qq = cq[:, hd * 128:(hd + 1) * 128].T; kk = ck[:, hd * 128:(hd + 1) * 128].T
        c_q[j] = qq; c_qs[j] = np.concatenate([qq[64:], qq[:64]], 0)
        c_k[j] = kk; c_ks[j] = np.concatenate([kk[64:], kk[:64]], 0)
        v6[3 + j] = cv[:, hd * 256 + qt * 64: hd * 256 + qt * 64 + 64]
        qdec[:, j, :] = np.exp(lg[hd] * (pos + 1.0))[None, :]
        kdec[:, j, :] = (np.exp(lg[hd] * (63.0 - pos)) * SC128)[None, :]
        cdec[:, j] = np.exp(lg[hd] * 64.0)
        mask6[:, 3 + j, :] = np.where(tri, np.exp(lg[hd] * (pos[None, :] - pos[:, None])), 0.0)
    d = dict(a_q=a_q, a_f=a_f, a_lb=np.full((128, 6), float(layer), np.float32), a_lbraw=lbraw, c_q=c_q, c_qs=c_qs, c_k=c_k, c_ks=c_ks,
             v6=v6, qdec=qdec, kdec=kdec, cdec=cdec, mask6=mask6)
    d.update(consts)
    return d

def lb1_gather(results, T):
    oa = np.empty((T, 1536), np.float32); oc = np.empty((T, 1536), np.float32)
    for c, r in enumerate(results):
        o6 = r["o6"].reshape(T, 6, 64)
        for j in range(3):
            uid = 3 * c + j
            hd, half = uid // 2, uid % 2
            oa[:, hd * 128 + half * 64: hd * 128 + half * 64 + 64] = o6[:, j]
            hd, qt = uid // 4, uid % 4
            oc[:, hd * 256 + qt * 64: hd * 256 + qt * 64 + 64] = o6[:, 3 + j]
    return oa, oc

def lb2_shared(bk, bv, ik):
    T = bk.shape[0]
    NKT = T // 128
    cb, sb_ = rope_cs(T, 32, ROPE_THETA)
    ci, si = rope_cs(T, 16, ROPE_THETA)
    kTt = np.ascontiguousarray(bk.reshape(NKT, 128, 8, 128).transpose(0, 3, 2, 1))
    perm32 = np.r_[16:32, 0:16]
    ksw = np.ascontiguousarray(kTt[:, perm32])
    C32 = np.concatenate([cb, cb], 1); S32 = np.concatenate([-sb_, sb_], 1)
    Ck = np.ascontiguousarray(C32.reshape(NKT, 128, 32).transpose(0, 2, 1)); Sk = np.ascontiguousarray(S32.reshape(NKT, 128, 32).transpose(0, 2, 1))
    Vt = np.zeros((NKT, 128, 8, 132), np.float32); Vt[..., :128] = bv.reshape(NKT, 128, 8, 128); Vt[..., 128] = 1.0
    kiT = np.ascontiguousarray(ik.T)
    perm16 = np.r_[8:16, 0:8]
    kisw = np.ascontiguousarray(kiT[perm16])
    C16 = np.concatenate([ci, ci], 1); S16 = np.concatenate([-si, si], 1)
    krsw = np.ascontiguousarray(np.stack([kTt[:, 0:32], ksw], 2))
    CSk = np.ascontiguousarray(np.stack([Ck, Sk], 2))
    return dict(kTt=kTt, krsw=krsw, CSk=CSk, Vt=Vt, kiT=kiT, kisw=kisw, Cki=np.ascontiguousarray(C16.T), Ski=np.ascontiguousarray(S16.T),
                identb=np.eye(128, dtype=np.float32)), (C32, S32, C16, S16)

def lb2_core_inputs(c, NQ, bq, iq, iw, shared, tabs):
    C32, S32, C16, S16 = tabs
    tiles = [8 * i + c for i in range(NQ)]
    rows = np.concatenate([np.arange(j * 128, (j + 1) * 128) for j in tiles])
    perm32 = np.r_[16:32, 0:16]; perm16 = np.r_[8:16, 0:8]
    qT = np.ascontiguousarray(bq[rows].reshape(NQ, 128, 8, 128).transpose(0, 3, 2, 1))
    qsw = np.ascontiguousarray(qT[:, perm32])
    Cq = np.ascontiguousarray(C32[rows].reshape(NQ, 128, 32).transpose(0, 2, 1)); Sq = np.ascontiguousarray(S32[rows].reshape(NQ, 128, 32).transpose(0, 2, 1))
    qiT = np.ascontiguousarray(iq[rows].reshape(NQ, 128, 16, 64).transpose(0, 3, 2, 1))
    qisw = np.ascontiguousarray(qiT[:, perm16])
    Cqi = np.ascontiguousarray(C16[rows].reshape(NQ, 128, 16).transpose(0, 2, 1)); Sqi = np.ascontiguousarray(S16[rows].reshape(NQ, 128, 16).transpose(0, 2, 1))
    iwc = np.ascontiguousarray(iw[rows].reshape(NQ, 128, 16))
    r = np.arange(128)[:, None]; z = np.arange(1024)[None, :]
    vis = (z < 128 * c + 64 * (r // 64 + 1)).astype(np.float32)
    pen = np.where(vis > 0, np.float32(3.0e38), np.float32(-2.0e30)).astype(np.float32)
    d = dict(qT=qT, qsw=qsw, Cq=Cq, Sq=Sq, qiT=qiT, qisw=qisw, Cqi=Cqi, Sqi=Sqi, iw=iwc, pen=pen)
    d.update(shared)
    return d

def lb2_gather(results, T, NQ):
    ob = np.empty((T, 1024), np.float32)
    for c, r in enumerate(results):
        for i in range(NQ):
            j = 8 * i + c
            ob[j * 128:(j + 1) * 128] = r["ob"][i]
    return ob

from concourse.bass_utils import run_bass_kernel_spmd

_CACHE = {}

def _prog(key, fn):
    if key not in _CACHE:
        _CACHE[key] = fn()
    return _CACHE[key]

PROJ_SIZES = (1536, 1536, 1536, 1536, 1024, 1024, 1024, 1024, 64, 16, 768, 768, 1536, 1536)


def kernel(x, ln_in_g, ln_in_b, w_in, w_out, hgrn_lb, hgrn_norm_g, ret_norm_g, ln1_g, ln1_b,
           router_w, router_b, w_gate_up, b_gate_up, w_down, b_down, ln2_g, ln2_b):
    f32 = lambda a: np.ascontiguousarray(np.asarray(a, dtype=np.float32))
    x = f32(x)[0]
    T, D = x.shape
    L = w_in.shape[0]
    NPROJ = w_in.shape[2]
    eye = np.eye(128, dtype=np.float32)
    offs = np.cumsum((0,) + PROJ_SIZES)
    h = None
    NCQ = 4
    NCOL = NPROJ // NCQ
    TH2 = T // 2
    consts1 = lb1_consts(T)
    LCN = 8
    TcC = T // LCN
    for l in range(L):
        do_ln = (l == 0)
        ncA = _prog(("la", do_ln), lambda: build_la(TH2, D, NCOL, do_ln))
        src = x if do_ln else h
        wl = f32(w_in[l])
        ims = []
        for c in range(8):
            th, cq = c // NCQ, c % NCQ
            d = {"x": np.ascontiguousarray(src[th * TH2:(th + 1) * TH2]), "w": np.ascontiguousarray(wl[:, cq * NCOL:(cq + 1) * NCOL]), "ident": eye}
            if do_ln:
                d["g"] = f32(ln_in_g); d["b"] = f32(ln_in_b)
            ims.append(d)
        res = run_bass_kernel_spmd(ncA, ims, core_ids=list(range(8))).results
        del ims, wl
        p = np.empty((T, NPROJ), np.float32)
        for c in range(8):
            th, cq = c // NCQ, c % NCQ
            p[th * TH2:(th + 1) * TH2, cq * NCOL:(cq + 1) * NCOL] = res[c]["p"]
        if do_ln:
            h = np.concatenate([res[0]["hout"], res[NCQ]["hout"]], 0)
        del res
        aq, af, ai, ag, bq, bk, bv, iq, ik, iw, cq_, ck, cv, cg = [np.ascontiguousarray(p[:, offs[i]:offs[i + 1]]) for i in range(14)]
        del p
        ncB1 = _prog(("lb1", T), lambda: build_lb1(T))
        hl = f32(hgrn_lb)
        ims = [lb1_core_inputs(c, l, aq, af, ai, cq_, ck, cv, hl, consts1) for c in range(8)]
        res = run_bass_kernel_spmd(ncB1, ims, core_ids=list(range(8))).results
        del ims
        oa_raw, oc_raw = lb1_gather(res, T)
        del res, aq, af, ai, cq_, ck, cv
        NQ = T // 128 // 8
        ncB2 = _prog(("lb2", T), lambda: build_lb2(T, NQ))
        shared, tabs = lb2_shared(bk, bv, ik)
        ims = [lb2_core_inputs(c, NQ, bq, iq, iw, shared, tabs) for c in range(8)]
        res = run_bass_kernel_spmd(ncB2, ims, core_ids=list(range(8))).results
        del ims, shared
        ob = lb2_gather(res, T, NQ)
        del res, bq, bk, bv, iq, ik, iw
        ncC = _prog(("lc", TcC), lambda: build_lc(TcC))
        NE = router_w.shape[2]
        bguT = np.ascontiguousarray(f32(b_gate_up[l]).reshape(NE, 12, 128).transpose(2, 0, 1))
        com = dict(gA=f32(hgrn_norm_g[l]).reshape(-1), gC=f32(ret_norm_g[l]).reshape(-1), wout=f32(w_out[l]), l1g=f32(ln1_g[l]), l1b=f32(ln1_b[l]),
                   l2g=f32(ln2_g[l]), l2b=f32(ln2_b[l]), rw=f32(router_w[l]), rb=f32(router_b[l]), wgu=f32(w_gate_up[l]), bguT=bguT,
                   wd=f32(w_down[l]), bd=f32(b_down[l]), ident=eye)
        ims = []
        for c in range(LCN):
            sl = slice(c * TcC, (c + 1) * TcC)
            d = dict(h=np.ascontiguousarray(h[sl]), oa=np.ascontiguousarray(oa_raw[sl]), ag=np.ascontiguousarray(ag[sl]), ob=np.ascontiguousarray(ob[sl]),
                     oc=np.ascontiguousarray(oc_raw[sl]), cg=np.ascontiguousarray(cg[sl]))
            d.update(com)
            ims.append(d)
        res = run_bass_kernel_spmd(ncC, ims, core_ids=list(range(LCN))).results
        del ims, com
        h = np.concatenate([r["hout"] for r in res], 0)
        del res, oa_raw, oc_raw, ob, ag, cg
    return h[None].astype(np.float32)
```
